# Optimizing a Trainium2 kernel written in Bass

```python
import math
import functools
import jax
import jax.numpy as jnp
from jax import lax
import numpy as np

D_MODEL = 1024
BATCH = 4
SEQ = 4096
DEPTH = 4

CTX_LEN = 256
GRID_W = 64
Q_BLOCK = 128
CHUNK = 128
ROPE_BASE = 10000.0
EPS = 1e-5
DEEPNORM_ALPHA = (2 * DEPTH) ** 0.25
DEEPNORM_BETA = (8 * DEPTH) ** -0.25
N_EVEN = (DEPTH + 1) // 2
N_ODD = DEPTH // 2
HA = 4
DKA = 64
DVA = 2 * DKA
HB = 4
DKB = 128
DVB = 128
CONV_W = 3
HC = 4
DKC = 64
DVC = 128
HD = 8
HKV = 2
DHD = 64
EVEN_SPLITS = (HA * 2 * DKA, HA * 2 * DKA, HA * DVA, HB * DKB, HB * DKB, HB * DVB, HB * DVB, 4 * HB)
ODD_SPLITS = (HC * DKC, HC * DKC, HC * DVC, HC * DVC, HD * DHD, HKV * DHD, HKV * DHD)
EVEN_IN = sum(EVEN_SPLITS)
ODD_IN = sum(ODD_SPLITS)
EVEN_OUT = HA * DVA + HB * DVB
ODD_OUT = HC * DVC + HD * DHD
N_GROUPS = 4
EXP_PER_GROUP = 8
N_EXPERTS = N_GROUPS * EXP_PER_GROUP
TOP_K = 2
D_EXPERT = 512
MOE_BLOCK = 128

kernel_name = "hybrid_diff_mlstm_retention_gqa_hmoe_prefix"

F32 = jnp.float32


def split_cols(p, sizes):
    return jnp.split(p, np.cumsum(sizes)[:-1].tolist(), axis=-1)


def to_heads(x, n_heads):
    b, t, _ = x.shape
    return x.reshape(b, t, n_heads, -1).transpose(0, 2, 1, 3)


def from_heads(x):
    b, h, t, d = x.shape
    return x.transpose(0, 2, 1, 3).reshape(b, t, h * d)


def layer_norm(x, g, b):
    xf = x.astype(F32)
    mu = xf.mean(-1, keepdims=True)
    var = jnp.square(xf - mu).mean(-1, keepdims=True)
    return ((xf - mu) * lax.rsqrt(var + EPS) * g + b).astype(x.dtype)


def rms_norm(x, g=None):
    xf = x.astype(F32)
    y = xf * lax.rsqrt(jnp.square(xf).mean(-1, keepdims=True) + EPS)
    if g is not None:
        y = y * g
    return y.astype(x.dtype)


def head_layer_norm(x, g, n_heads):
    b, t, w = x.shape
    xf = x.astype(F32).reshape(b, t, n_heads, w // n_heads)
    mu = xf.mean(-1, keepdims=True)
    var = jnp.square(xf - mu).mean(-1, keepdims=True)
    y = ((xf - mu) * lax.rsqrt(var + EPS)).reshape(b, t, w) * g
    return y.astype(x.dtype)


def axial_rope_tables(n_tokens, dim):
    n_rows = n_tokens // GRID_W
    row = jnp.repeat(jnp.arange(n_rows, dtype=F32), GRID_W)
    col = jnp.tile(jnp.arange(GRID_W, dtype=F32), n_rows)
    quarter = dim // 4
    inv_freq = ROPE_BASE ** (-jnp.arange(quarter, dtype=F32) / quarter)
    ang = jnp.concatenate([row[:, None] * inv_freq, col[:, None] * inv_freq], -1)
    return jnp.cos(ang), jnp.sin(ang)


def apply_rope(x, cos, sin):
    half = x.shape[-1] // 2
    x1, x2 = x[..., :half], x[..., half:]
    cos = cos.astype(x.dtype)
    sin = sin.astype(x.dtype)
    return jnp.concatenate([x1 * cos - x2 * sin, x1 * sin + x2 * cos], -1)


def map_query_blocks(block_fn, qs, t_axis):
    t = qs[0].shape[t_axis]
    nb = t // Q_BLOCK

    def split(q):
        q = q.reshape(q.shape[:t_axis] + (nb, Q_BLOCK) + q.shape[t_axis + 1:])
        return jnp.moveaxis(q, t_axis, 0)

    out = lax.map(lambda blk: block_fn(*blk), tuple(split(q) for q in qs))
    out = jnp.moveaxis(out, 0, t_axis)
    return out.reshape(out.shape[:t_axis] + (t,) + out.shape[t_axis + 2:])


def diff_attention(q1, q2, k1, k2, v, lam):
    scale = DKA ** -0.5

    def block(q1b, q2b):
        p1 = jax.nn.softmax(jnp.einsum('bhqd,bhkd->bhqk', q1b, k1).astype(F32) * scale, -1)
        p2 = jax.nn.softmax(jnp.einsum('bhqd,bhkd->bhqk', q2b, k2).astype(F32) * scale, -1)
        return jnp.einsum('bhqk,bhkd->bhqd', (p1 - lam * p2).astype(v.dtype), v)

    return map_query_blocks(block, (q1, q2), 2)


def gqa_attention(q, k, v):
    scale = DHD ** -0.5

    def block(qb):
        s = jnp.einsum('bkgqd,bksd->bkgqs', qb, k).astype(F32) * scale
        p = jax.nn.softmax(s, -1)
        return jnp.einsum('bkgqs,bksd->bkgqd', p.astype(v.dtype), v)

    return map_query_blocks(block, (q,), 3)


def short_conv(x, w, b):
    pad = CONV_W // 2
    t = x.shape[1]
    xp = jnp.pad(x, ((0, 0), (pad, pad), (0, 0)))
    y = b
    for j in range(CONV_W):
        y = y + xp[:, j:j + t] * w[j]
    return y


def to_chunks(a):
    nc = a.shape[2] // CHUNK
    return jnp.moveaxis(a.reshape(a.shape[:2] + (nc, CHUNK) + a.shape[3:]), 2, 0)


def from_chunks(h):
    h = jnp.moveaxis(h, 0, 2)
    return h.reshape(h.shape[:2] + (h.shape[2] * h.shape[3],) + h.shape[4:])


def mlstm_scan(inputs, state, with_out):
    causal = jnp.tril(jnp.ones((CHUNK, CHUNK), bool))

    def body(carry, inp):
        c_mat, n_vec, m = carry
        qc, kc, vc, lic, lfc = inp
        b = jnp.cumsum(lfc, -1)
        b_end = b[..., -1]
        log_end = b_end[..., None] - b + lic
        m_new = jnp.maximum(b_end + m, log_end.max(-1))
        carry_w = jnp.exp(b_end + m - m_new)
        wk = jnp.exp(log_end - m_new[..., None])
        c_new = carry_w[..., None, None] * c_mat + jnp.einsum('bhs,bhse,bhsd->bhed', wk, vc, kc)
        n_new = carry_w[..., None] * n_vec + jnp.einsum('bhs,bhsd->bhd', wk, kc)
        if not with_out:
            return (c_new, n_new, m_new), None
        log_intra = jnp.where(causal, b[..., :, None] - b[..., None, :] + lic[..., None, :], -jnp.inf)
        log_inter = b + m[..., None]
        m_t = jnp.maximum(log_inter, log_intra.max(-1))
        w_intra = jnp.exp(log_intra - m_t[..., None])
        w_inter = jnp.exp(log_inter - m_t)
        s = jnp.einsum('bhtd,bhsd->bhts', qc, kc) * w_intra
        num = w_inter[..., None] * jnp.einsum('bhed,bhtd->bhte', c_mat, qc) + jnp.einsum('bhts,bhse->bhte', s, vc)
        den = w_inter * jnp.einsum('bhd,bhtd->bht', n_vec, qc) + s.sum(-1)
        h = num / jnp.maximum(jnp.abs(den), jnp.exp(-m_t))[..., None]
        return (c_new, n_new, m_new), h

    state, h = lax.scan(body, state, tuple(to_chunks(a) for a in inputs))
    return state, (from_chunks(h) if with_out else None)


def retention_scan(inputs, state, with_out, log_decay):
    pos = jnp.arange(CHUNK, dtype=F32)
    lag = pos[:, None] - pos[None, :]
    lg = log_decay[:, None, None]
    intra = jnp.where(lag >= 0, jnp.exp(jnp.maximum(lag, 0.0) * lg), 0.0)
    q_decay = jnp.exp((pos + 1.0) * log_decay[:, None])[..., None]
    k_decay = jnp.exp((CHUNK - 1.0 - pos) * log_decay[:, None])[..., None]
    chunk_decay = jnp.exp(CHUNK * log_decay)[:, None, None]

    def body(s_mat, inp):
        qc, kc, vc = inp
        s_new = chunk_decay * s_mat + jnp.einsum('bhsd,bhse->bhde', kc * k_decay, vc)
        if not with_out:
            return s_new, None
        scores = jnp.einsum('bhtd,bhsd->bhts', qc, kc) * intra
        o = jnp.einsum('bhts,bhse->bhte', scores, vc) + jnp.einsum('bhtd,bhde->bhte', qc * q_decay, s_mat)
        return s_new, o

    state, o = lax.scan(body, state, tuple(to_chunks(a) for a in inputs))
    return state, (from_chunks(o) if with_out else None)


def time_flip(a, reverse):
    return jnp.flip(a, axis=2) if reverse else a


def bidir_prefix_scan(scan_fns, ctx_dirs, lat_dirs, state0, with_ctx_out):
    h_ctx, h_lat = [], []
    for d in range(2):
        rev = d == 1
        st, oc = scan_fns[d](tuple(time_flip(a, rev) for a in ctx_dirs[d]), state0, with_ctx_out)
        _, ol = scan_fns[d](tuple(time_flip(a, rev) for a in lat_dirs[d]), st, True)
        h_lat.append(time_flip(ol, rev))
        if with_ctx_out:
            h_ctx.append(time_flip(oc, rev))
    return (h_ctx[0] + h_ctx[1] if with_ctx_out else None), h_lat[0] + h_lat[1]


def even_mixer(h_lat, h_ctx, w_in, w_out, lam_p, subln_g, conv_w, conv_b, gate_b, mnorm_g, layer_idx, with_ctx):
    bsz, t_lat, _ = h_lat.shape
    pl = split_cols(h_lat @ w_in, EVEN_SPLITS)
    pc = split_cols(h_ctx @ w_in, EVEN_SPLITS)
    lam_init = 0.8 - 0.6 * math.exp(-0.3 * layer_idx)
    lp = lam_p.astype(F32)
    lam = jnp.exp(jnp.sum(lp[0] * lp[1])) - jnp.exp(jnp.sum(lp[2] * lp[3])) + lam_init
    cos, sin = axial_rope_tables(t_lat, DKA)

    def diff_pair(a):
        b, t, _ = a.shape
        a = a.reshape(b, t, HA, 2, DKA).transpose(3, 0, 2, 1, 4)
        return a[0], a[1]

    def diff_post(o):
        return from_heads(rms_norm(o, subln_g) * (1.0 - lam_init))

    k1_c, k2_c = diff_pair(pc[1])
    v_c = to_heads(pc[2], HA)
    q1_l, q2_l = [apply_rope(a, cos, sin) for a in diff_pair(pl[0])]
    k1_l, k2_l = [apply_rope(a, cos, sin) for a in diff_pair(pl[1])]
    a_l = diff_post(diff_attention(q1_l, q2_l,
                                   jnp.concatenate([k1_c, k1_l], 2),
                                   jnp.concatenate([k2_c, k2_l], 2),
                                   jnp.concatenate([v_c, to_heads(pl[2], HA)], 2), lam))

    def mlstm_dirs(p):
        b, t, _ = p[3].shape
        qk = jax.nn.silu(short_conv(jnp.concatenate([p[3], p[4]], -1), conv_w, conv_b))
        q = to_heads(qk[..., :HB * DKB], HB).astype(F32)
        k = to_heads(qk[..., HB * DKB:], HB).astype(F32) * DKB ** -0.5
        v = to_heads(p[5], HB).astype(F32)
        g = (p[7].reshape(b, t, 4, HB).astype(F32) + gate_b).transpose(2, 0, 3, 1)
        return [(q, k, v, g[0], jax.nn.log_sigmoid(g[1])), (q, k, v, g[2], jax.nn.log_sigmoid(g[3]))]

    state0 = (jnp.zeros((bsz, HB, DVB, DKB), F32), jnp.zeros((bsz, HB, DKB), F32), jnp.zeros((bsz, HB), F32))
    m_c, m_l = bidir_prefix_scan([mlstm_scan, mlstm_scan], mlstm_dirs(pc), mlstm_dirs(pl), state0, with_ctx)

    def mlstm_post(h_sum, p):
        y = from_heads(h_sum).astype(p[6].dtype) * jax.nn.sigmoid(p[6])
        return head_layer_norm(y, mnorm_g, HB)

    out_l = jnp.concatenate([a_l, mlstm_post(m_l, pl)], -1) @ w_out
    out_c = None
    if with_ctx:
        q1_c, q2_c = diff_pair(pc[0])
        a_c = diff_post(diff_attention(q1_c, q2_c, k1_c, k2_c, v_c, lam))
        out_c = jnp.concatenate([a_c, mlstm_post(m_c, pc)], -1) @ w_out
    return out_l, out_c


def odd_mixer(h_lat, h_ctx, w_in, w_out, decay_logit, qk_g, with_ctx):
    bsz, t_lat, _ = h_lat.shape
    pl = split_cols(h_lat @ w_in, ODD_SPLITS)
    pc = split_cols(h_ctx @ w_in, ODD_SPLITS)
    log_decay = jax.nn.log_sigmoid(decay_logit.astype(F32))

    def ret_inputs(p):
        q = to_heads(p[0], HC).astype(F32)
        k = to_heads(p[1], HC).astype(F32) * DKC ** -0.5
        v = to_heads(p[2], HC).astype(F32)
        return (q, k, v)

    ret_c, ret_l = ret_inputs(pc), ret_inputs(pl)
    scan_fns = [functools.partial(retention_scan, log_decay=log_decay[d]) for d in range(2)]
    s0 = jnp.zeros((bsz, HC, DKC, DVC), F32)
    r_c, r_l = bidir_prefix_scan(scan_fns, [ret_c, ret_c], [ret_l, ret_l], s0, with_ctx)

    def ret_post(r, p):
        return from_heads(rms_norm(r)).astype(p[3].dtype) * jax.nn.silu(p[3])

    cos, sin = axial_rope_tables(t_lat, DHD)

    def group_q(q):
        b, h, t, d = q.shape
        return q.reshape(b, HKV, HD // HKV, t, d)

    def ungroup(o):
        b, k, g, t, d = o.shape
        return from_heads(o.reshape(b, k * g, t, d))

    k_c = rms_norm(to_heads(pc[5], HKV), qk_g[1])
    v_c = to_heads(pc[6], HKV)
    q_l = apply_rope(rms_norm(to_heads(pl[4], HD), qk_g[0]), cos, sin)
    k_l = apply_rope(rms_norm(to_heads(pl[5], HKV), qk_g[1]), cos, sin)
    a_l = ungroup(gqa_attention(group_q(q_l), jnp.concatenate([k_c, k_l], 2),
                                jnp.concatenate([v_c, to_heads(pl[6], HKV)], 2)))
    out_l = jnp.concatenate([ret_post(r_l, pl), a_l], -1) @ w_out
    out_c = None
    if with_ctx:
        q_c = rms_norm(to_heads(pc[4], HD), qk_g[0])
        a_c = ungroup(gqa_attention(group_q(q_c), k_c, v_c))
        out_c = jnp.concatenate([ret_post(r_c, pc), a_c], -1) @ w_out
    return out_l, out_c


def hier_moe(h, w_group, b_group, w_router, b_router, w_gate, w_up, w_down):
    n, d = h.shape
    hf = h.astype(F32)
    g_logits = hf @ w_group + b_group
    g_prob = jax.nn.softmax(g_logits, -1)
    g_idx = jnp.argmax(g_logits, -1)
    g_w = jnp.take_along_axis(g_prob, g_idx[:, None], -1)
    e_logits = (hf @ w_router + b_router).reshape(n, N_GROUPS, EXP_PER_GROUP)
    e_logits = jnp.take_along_axis(e_logits, g_idx[:, None, None], 1)[:, 0]
    top_v, top_i = lax.top_k(e_logits, TOP_K)
    weights = (jax.nn.softmax(top_v, -1) * g_w).reshape(-1)
    expert = (g_idx[:, None] * EXP_PER_GROUP + top_i).reshape(-1)
    token = jnp.repeat(jnp.arange(n), TOP_K)
    order = jnp.argsort(expert)
    s_exp, s_tok, s_w = expert[order], token[order], weights[order]
    counts = jnp.bincount(expert, length=N_EXPERTS)
    padded = (counts + MOE_BLOCK - 1) // MOE_BLOCK * MOE_BLOCK
    padded_end = jnp.cumsum(padded)
    dest = (padded_end - padded)[s_exp] + jnp.arange(n * TOP_K) - (jnp.cumsum(counts) - counts)[s_exp]
    n_blocks = -(-(n * TOP_K) // MOE_BLOCK) + N_EXPERTS
    buf = jnp.zeros((n_blocks * MOE_BLOCK, d), h.dtype).at[dest].set(h[s_tok])
    block_exp = jnp.minimum(jnp.searchsorted(padded_end, jnp.arange(n_blocks) * MOE_BLOCK, side='right'),
                            N_EXPERTS - 1)

    def expert_block(args):
        xb, e = args
        return (jax.nn.silu(xb @ w_gate[e]) * (xb @ w_up[e])) @ w_down[e]

    y = lax.map(expert_block, (buf.reshape(n_blocks, MOE_BLOCK, d), block_exp)).reshape(-1, d)
    out = jax.ops.segment_sum(y[dest].astype(F32) * s_w[:, None], s_tok, num_segments=n)
    return out.astype(h.dtype)


def setup_inputs(seed: int = 0) -> dict:
    key = jax.random.key(seed)
    keys = iter(jax.random.split(key, 40))

    def nrm(shape, scale):
        return jax.random.normal(next(keys), shape, F32) * scale

    d = D_MODEL
    forget_base = jnp.zeros((4, HB), F32).at[1].set(jnp.linspace(3.0, 6.0, HB)).at[3].set(jnp.linspace(3.0, 6.0, HB))
    decay_base = jnp.log(2.0 ** (5.0 + jnp.arange(HC, dtype=F32)) - 1.0)
    return {
        "x": nrm((BATCH, SEQ, d), 1.0),
        "c": nrm((BATCH, d), 1.0),
        "ctx": nrm((BATCH, CTX_LEN, d), 1.0),
        "c_ctx": nrm((d,), 1.0),
        "w_mod": nrm((DEPTH, d, 6 * d), 0.5 * d ** -0.5),
        "b_mod": nrm((DEPTH, 6 * d), 0.02),
        "ln_g": 1.0 + nrm((DEPTH, 2, d), 0.02),
        "ln_b": nrm((DEPTH, 2, d), 0.02),
        "ev_w_in": nrm((N_EVEN, d, EVEN_IN), d ** -0.5),
        "ev_w_out": nrm((N_EVEN, EVEN_OUT, d), DEEPNORM_BETA * EVEN_OUT ** -0.5),
        "ev_lambda": nrm((N_EVEN, 4, DKA), 0.1),
        "ev_subln_g": 1.0 + nrm((N_EVEN, DVA), 0.02),
        "ev_conv_w": nrm((N_EVEN, CONV_W, 2 * HB * DKB), CONV_W ** -0.5),
        "ev_conv_b": nrm((N_EVEN, 2 * HB * DKB), 0.02),
        "ev_gate_b": forget_base + nrm((N_EVEN, 4, HB), 0.1),
        "ev_mnorm_g": 1.0 + nrm((N_EVEN, HB * DVB), 0.02),
        "od_w_in": nrm((N_ODD, d, ODD_IN), d ** -0.5),
        "od_w_out": nrm((N_ODD, ODD_OUT, d), DEEPNORM_BETA * ODD_OUT ** -0.5),
        "od_decay": decay_base + nrm((N_ODD, 2, HC), 0.1),
        "od_qk_g": 1.0 + nrm((N_ODD, 2, DHD), 0.02),
        "moe_w_group": nrm((DEPTH, d, N_GROUPS), d ** -0.5),
        "moe_b_group": nrm((DEPTH, N_GROUPS), 0.01),
        "moe_w_router": nrm((DEPTH, d, N_EXPERTS), d ** -0.5),
        "moe_b_router": nrm((DEPTH, N_EXPERTS), 0.01),
        "moe_w_gate": nrm((DEPTH, N_EXPERTS, d, D_EXPERT), d ** -0.5),
        "moe_w_up": nrm((DEPTH, N_EXPERTS, d, D_EXPERT), d ** -0.5),
        "moe_w_down": nrm((DEPTH, N_EXPERTS, D_EXPERT, d), DEEPNORM_BETA * D_EXPERT ** -0.5),
    }


def reference(x, c, ctx, c_ctx, w_mod, b_mod, ln_g, ln_b, ev_w_in, ev_w_out, ev_lambda, ev_subln_g,
              ev_conv_w, ev_conv_b, ev_gate_b, ev_mnorm_g, od_w_in, od_w_out, od_decay, od_qk_g,
              moe_w_group, moe_b_group, moe_w_router, moe_b_router, moe_w_gate, moe_w_up, moe_w_down):
    c_act = jax.nn.silu(c)
    cc_act = jax.nn.silu(c_ctx)
    x_lat, x_ctx = x, ctx
    for layer in range(DEPTH):
        with_ctx = layer < DEPTH - 1
        i = layer // 2
        mod_l = [m[:, None, :] for m in jnp.split(c_act @ w_mod[layer] + b_mod[layer], 6, -1)]
        mod_c = jnp.split(cc_act @ w_mod[layer] + b_mod[layer], 6, -1)
        h_l = x_lat * (1.0 + mod_l[1]) + mod_l[0]
        h_c = x_ctx * (1.0 + mod_c[1]) + mod_c[0]
        if layer % 2 == 0:
            o_l, o_c = even_mixer(h_l, h_c, ev_w_in[i], ev_w_out[i], ev_lambda[i], ev_subln_g[i], ev_conv_w[i],
                                  ev_conv_b[i], ev_gate_b[i], ev_mnorm_g[i], layer, with_ctx)
        else:
            o_l, o_c = odd_mixer(h_l, h_c, od_w_in[i], od_w_out[i], od_decay[i], od_qk_g[i], with_ctx)
        x_lat = layer_norm(DEEPNORM_ALPHA * x_lat + mod_l[2] * o_l, ln_g[layer, 0], ln_b[layer, 0])
        h_l = x_lat * (1.0 + mod_l[4]) + mod_l[3]
        moe_p = (moe_w_group[layer], moe_b_group[layer], moe_w_router[layer], moe_b_router[layer],
                 moe_w_gate[layer], moe_w_up[layer], moe_w_down[layer])
        if with_ctx:
            x_ctx = layer_norm(DEEPNORM_ALPHA * x_ctx + mod_c[2] * o_c, ln_g[layer, 0], ln_b[layer, 0])
            h_c = x_ctx * (1.0 + mod_c[4]) + mod_c[3]
            n_c = h_c.shape[0] * h_c.shape[1]
            y = hier_moe(jnp.concatenate([h_c.reshape(n_c, -1), h_l.reshape(-1, h_l.shape[-1])], 0), *moe_p)
            x_ctx = layer_norm(DEEPNORM_ALPHA * x_ctx + mod_c[5] * y[:n_c].reshape(h_c.shape),
                               ln_g[layer, 1], ln_b[layer, 1])
            y_l = y[n_c:].reshape(h_l.shape)
        else:
            y_l = hier_moe(h_l.reshape(-1, h_l.shape[-1]), *moe_p).reshape(h_l.shape)
        x_lat = layer_norm(DEEPNORM_ALPHA * x_lat + mod_l[5] * y_l, ln_g[layer, 1], ln_b[layer, 1])
    return x_lat
```

```python
import math
from contextlib import ExitStack
import numpy as np
import concourse.bass as bass
import concourse.mybir as mybir
from concourse.bass_utils import run_bass_kernel_spmd

F32 = mybir.dt.float32
BF16 = mybir.dt.bfloat16
I32 = mybir.dt.int32
AF = mybir.ActivationFunctionType
ALU = mybir.AluOpType
AX = mybir.AxisListType

D = 1024
KT = 8
DEPTH = 4
EPS = 1e-5
ALPHA = (2 * DEPTH) ** 0.25
EVEN_IN = 3600
ODD_IN = 2304
NEXP = 32
NEG_BIG = -1.0e30


def _k(r):
    if isinstance(r, (str, tuple, int)):
        return r
    return r.name


class Op:
    __slots__ = ("eng", "fn", "reads", "writes", "is_dma", "deps", "needs_inc", "tok", "seq", "dsem", "barrier")

    def __init__(self, eng, fn, reads, writes, is_dma):
        self.eng = eng
        self.fn = fn
        self.reads = reads
        self.writes = writes
        self.is_dma = is_dma
        self.deps = []
        self.needs_inc = False
        self.tok = None
        self.seq = -1
        self.dsem = -1
        self.barrier = False


class Prog:
    COMPUTE = ("pe", "act", "dve", "pool")
    ALL = ("pe", "act", "dve", "pool", "sp")

    def __init__(self, nc, n_dma_sems=20):
        self.nc = nc
        self.ops = []
        self.engs = {"pe": nc.tensor, "act": nc.scalar, "dve": nc.vector, "pool": nc.gpsimd, "sp": nc.sync}
        self.n_dma_sems = n_dma_sems
        self._n = 0

    def op(self, eng, fn, reads=(), writes=()):
        o = Op(eng, fn, tuple(_k(r) for r in reads), tuple(_k(w) for w in writes), False)
        self.ops.append(o)
        return o

    def dma(self, q, out, in_, reads=(), writes=(), **kw):
        o = Op(q, lambda e: e.dma_start(out=out, in_=in_, **kw), tuple(_k(r) for r in reads),
               tuple(_k(w) for w in writes), True)
        self.ops.append(o)
        return o

    def barrier(self):
        for e in self.ALL:
            o = Op(e, lambda en: en.nop(), (), (), False)
            o.barrier = True
            self.ops.append(o)

    def emit(self):
        nc = self.nc
        state = {}
        eng_seq = {e: 0 for e in self.engs}
        last_op = {e: None for e in self.COMPUTE}
        waited_c = {e: {p: -1 for p in self.COMPUTE} for e in self.engs}
        waited_d = {e: set() for e in self.engs}
        dma_q_count = {"sp": 0, "pool": 0, "act": 0}
        dma_last_on_sem = {}
        nbar = 0
        for o in self.ops:
            o.seq = eng_seq[o.eng]
            eng_seq[o.eng] += 1
            deps = []
            if o.barrier:
                for p in self.COMPUTE:
                    if last_op[p] is not None and p != o.eng:
                        deps.append(last_op[p])
                    elif last_op[p] is not None and p == o.eng and p != "pe":
                        deps.append(last_op[p])
                deps.extend(dma_last_on_sem.values())
                nbar += 1
                if nbar % len(self.ALL) == 0:
                    state = {}
            else:
                for r in o.reads:
                    st = state.get(r)
                    if st is not None and st[0] is not None:
                        deps.append(st[0])
                for w in o.writes:
                    st = state.get(w)
                    if st is not None:
                        if st[0] is not None:
                            deps.append(st[0])
                        deps.extend(st[1])
            if o.is_dma:
                k = dma_q_count[o.eng]
                dma_q_count[o.eng] += 1
                o.dsem = (o.eng, k % self.n_dma_sems)
                prev = dma_last_on_sem.get(o.dsem)
                if prev is not None:
                    deps.append(prev)
                dma_last_on_sem[o.dsem] = o
            final = []
            for d in deps:
                if d is o:
                    continue
                if d.is_dma:
                    if d in waited_d[o.eng]:
                        continue
                    waited_d[o.eng].add(d)
                    final.append(d)
                else:
                    if d.eng == "pe" and o.eng == "pe" and not o.is_dma:
                        continue
                    if waited_c[o.eng][d.eng] >= d.seq:
                        continue
                    waited_c[o.eng][d.eng] = d.seq
                    final.append(d)
            best = {}
            dm = []
            for d in final:
                if d.is_dma:
                    dm.append(d)
                elif d.eng not in best or best[d.eng].seq < d.seq:
                    best[d.eng] = d
            o.deps = dm + list(best.values())
            for d in o.deps:
                d.needs_inc = True
            if not o.barrier:
                for r in o.reads:
                    st = state.setdefault(r, [None, []])
                    st[1].append(o)
                for w in o.writes:
                    state[w] = [o, []]
            if not o.is_dma and o.eng in last_op and not o.barrier:
                last_op[o.eng] = o
        self._sem_ctx = []

        def mk(name):
            cm = nc.semaphore(name)
            s = cm.__enter__()
            self._sem_ctx.append(cm)
            return s

        sems = {e: mk(f"s_{e}") for e in self.COMPUTE}
        dsems = {}
        for q in ("sp", "pool", "act"):
            for i in range(min(self.n_dma_sems, dma_q_count[q])):
                dsems[(q, i)] = mk(f"d_{q}{i}")
        cnt = {e: 0 for e in self.COMPUTE}
        dcnt = {}
        n_wait = 0
        for o in self.ops:
            e = self.engs[o.eng]
            for d in o.deps:
                s, v = d.tok
                e.wait_ge(s, v)
                n_wait += 1
            inst = o.fn(e)
            if o.is_dma:
                s = dsems[o.dsem]
                dcnt[o.dsem] = dcnt.get(o.dsem, 0) + 16
                inst.then_inc(s, 16)
                o.tok = (s, dcnt[o.dsem])
            elif o.needs_inc:
                cnt[o.eng] += 1
                inst.then_inc(sems[o.eng], 1)
                o.tok = (sems[o.eng], cnt[o.eng])
        self.stats = dict(n_ops=len(self.ops), n_wait=n_wait, cnt=dict(cnt))
        return self.stats

    def final_wait(self, resources):
        self.op("sp", lambda e: e.nop(), reads=tuple(resources))


class Rot:
    def __init__(self, tiles):
        self.tiles = tiles
        self.i = 0

    def next(self):
        t = self.tiles[self.i % len(self.tiles)]
        self.i += 1
        return t


class Builder:
    def __init__(self, TC, TL, debug=()):
        self.TC, self.TL = TC, TL
        self.T = TC + TL
        self.NT = self.T // 128
        self.NTC = TC // 128
        self.debug = set(debug)
        nc = self.nc = bass.Bass("TRN2", target_bir_lowering=False)
        self.P = Prog(nc)
        arena_bytes = 196608
        ar = nc.alloc_sbuf_tensor("arena", [128, arena_bytes], mybir.dt.uint8)
        self.abase = nc.lookup_mloc(ar).addr
        self.aend = self.abase + arena_bytes
        self.ptop = self.abase
        self.top = self.abase
        self._n = 0
        self.inputs = {}
        self.outputs = {}
        self.psf = [nc.alloc_psum_tensor(f"psf{i}", [128, 512], F32) for i in range(6)]
        self.psb = [nc.alloc_psum_tensor(f"psb{i}", [128, 1024], BF16) for i in range(2)]

    def sb(self, shape, dt, persistent=False, name=None):
        self._n += 1
        esz = {F32: 4, BF16: 2, I32: 4}[dt]
        nbytes = int(np.prod(shape[1:])) * esz
        nbytes = (nbytes + 63) // 64 * 64
        if persistent:
            assert self.top == self.ptop, "persistent alloc only at phase boundary"
            off = self.ptop
            self.ptop += nbytes
            self.top = self.ptop
        else:
            off = self.top
            self.top += nbytes
        assert self.top <= self.aend, f"SBUF arena overflow {self.top - self.abase}"
        return self.nc.alloc_sbuf_tensor_at(name or f"t{self._n}", list(shape), dt, offset=off)

    def new_phase(self):
        self.P.barrier()
        self.top = self.ptop

    def din(self, name, shape, dt=F32):
        t = self.nc.dram_tensor(name, list(shape), dt, kind="ExternalInput")
        self.inputs[name] = t
        return t.ap()

    def dout(self, name, shape, dt=F32):
        t = self.nc.dram_tensor(name, list(shape), dt, kind="ExternalOutput")
        self.outputs[name] = t
        return t.ap()

    def dscr(self, name, shape, dt):
        kind = "ExternalOutput" if name in self.debug else "Internal"
        t = self.nc.dram_tensor(name, list(shape), dt, kind=kind)
        if name in self.debug:
            self.outputs[name] = t
        return t.ap()

    def V(self, fn, *a, r=(), w=(), **kw):
        return self.P.op("dve", lambda e: getattr(e, fn)(*a, **kw), r, w)

    def A(self, fn, *a, r=(), w=(), **kw):
        return self.P.op("act", lambda e: getattr(e, fn)(*a, **kw), r, w)

    def G(self, fn, *a, r=(), w=(), **kw):
        return self.P.op("pool", lambda e: getattr(e, fn)(*a, **kw), r, w)

    def mm(self, out, lhsT, rhs, start=True, stop=True, r=(), w=(), skip=False):
        return self.P.op("pe", lambda e: e.matmul(out, lhsT, rhs, start=start, stop=stop, skip_group_check=skip), r, w)

    def tr(self, out, in_, ident, r=(), w=()):
        return self.P.op("pe", lambda e: e.transpose(out, in_, ident), r, w)

    def dma(self, q, out, in_, r=(), w=(), **kw):
        return self.P.dma(q, out, in_, r, w, **kw)

    def consts(self):
        iot = self.iot = self.sb([128, 128], F32, True)
        self.G("iota", iot[:], [[1, 128]], base=0, channel_multiplier=-1, allow_small_or_imprecise_dtypes=True, w=[iot])
        def cmp(op, dt):
            t = self.sb([128, 128], dt, True)
            self.V("tensor_single_scalar", t[:], iot[:], 0.0, op, r=[iot], w=[t])
            return t
        self.ident_f = cmp(ALU.is_equal, F32)
        self.ident_b = cmp(ALU.is_equal, BF16)
        self.triU_f = cmp(ALU.is_ge, F32)
        self.triL_f = cmp(ALU.is_le, F32)
        self.sU_f = cmp(ALU.is_gt, F32)
        self.sU_b = cmp(ALU.is_gt, BF16)
        self.ones_f = self.sb([128, 128], F32, True)
        self.V("memset", self.ones_f[:], 1.0, w=[self.ones_f])
        self.ones_b = self.sb([128, 128], BF16, True)
        self.V("memset", self.ones_b[:], 1.0, w=[self.ones_b])
        self.pidx = self.sb([128, 1], F32, True)
        self.G("iota", self.pidx[:], [[0, 1]], base=0, channel_multiplier=1, allow_small_or_imprecise_dtypes=True, w=[self.pidx])

    def bcast_row(self, col_ap_fn, out_tile, r=()):
        for half in range(2):
            ps = self.psf[4 + half]
            for k4 in range(4):
                kt = half * 4 + k4
                tmp = self.sb([128, 128], F32)
                self.V("tensor_scalar", tmp[:], self.ones_f[:], col_ap_fn(kt), None, ALU.mult, r=[self.ones_f, *r], w=[tmp])
                self.mm(ps[:, k4 * 128:(k4 + 1) * 128], tmp[:], self.ident_f[:], r=[tmp, self.ident_f], w=[ps])
            self.A("copy", out_tile[:, half * 512:(half + 1) * 512], ps[:], r=[ps], w=[out_tile])

    def declare_io(self):
        T = self.T
        self.xin = self.din("xin", [T, D])
        self.cT = self.din("cT", [128, 16])
        self.w_mod = self.din("w_mod", [DEPTH, D, 6 * D])
        self.bmodT = self.din("bmodT", [DEPTH, 128, 48])
        self.ln_g = self.din("ln_g", [DEPTH * 2, D])
        self.ln_b = self.din("ln_b", [DEPTH * 2, D])
        self.ev_w_in = self.din("ev_w_in", [2, D, EVEN_IN])
        self.ev_w_out = self.din("ev_w_out", [2, D, D])
        self.ev_lambda = self.din("ev_lambda", [2, 256])
        self.ev_subln_g = self.din("ev_subln_g", [2, 128])
        self.convwT = self.din("convwT", [2, 128, 24])
        self.convbT = self.din("convbT", [2, 128, 8])
        self.ev_gate_b = self.din("ev_gate_b", [2, 16])
        self.ev_mnorm_g = self.din("ev_mnorm_g", [2, 512])
        self.od_w_in = self.din("od_w_in", [2, D, ODD_IN])
        self.od_w_out = self.din("od_w_out", [2, D, D])
        self.od_decay = self.din("od_decay", [2, 8])
        self.od_qk_g = self.din("od_qk_g", [2, 128])
        self.wr = self.din("wr", [DEPTH, D, 36])
        self.br = self.din("br", [DEPTH, 36])
        self.wexp = [self.din(f"wexp{j}", [DEPTH * NEXP * 128, 2048]) for j in range(6)]
        self.rope_cos = self.din("rope_cos", [self.TL, 32])
        self.rope_sin = self.din("rope_sin", [self.TL, 32])
        self.yout = self.dout("yout", [self.TL, D])
        self.X = self.dscr("X", [T, D], F32)
        self.cat = self.dscr("cat", [T, D], BF16)
        self.catT = self.dscr("catT", [8, 128, T], BF16)
        self.qTA = self.dscr("qTA", [4, 128, T], BF16)
        self.kTA = self.dscr("kTA", [4, 128, T], BF16)
        self.vA = self.dscr("vA", [T, 512], BF16)
        self.qkraw = self.dscr("qkraw", [8, 128, T], BF16)
        self.vB = self.dscr("vB", [T, 512], BF16)
        self.ogB = self.dscr("ogB", [T, 512], F32)
        self.gB = self.dscr("gB", [T, 16], F32)
        self.hB = self.dscr("hB", [T, 512], F32)
        self.NB = self.NT * 2 + NEXP
        self.H2 = self.dscr("H2", [T, D], BF16)
        self.XB = self.dscr("XB", [self.NB * 128, D], BF16)
        self.YB = self.dscr("YB", [self.NB * 128, D], F32)

    def phase_mods(self):
        cT = self.sb([128, 16], F32)
        self.dma("sp", cT[:], self.cT[:, :], w=[cT])
        cact = self.sb([128, 16], F32)
        self.A("activation", cact[:], cT[:], AF.Silu, r=[cT], w=[cact])
        wts = Rot([self.sb([128, 8, 768], F32) for _ in range(2)])
        bm = self.sb([128, DEPTH, 48], F32)
        for l in range(DEPTH):
            self.dma("sp", bm[:, l, :], self.bmodT[l], w=[bm])
        for l in range(DEPTH):
            ps = self.psf[l % 2]
            for cc in range(8):
                wt = wts.next()
                src = self.w_mod[l].rearrange("(kt p) n -> p kt n", p=128)[:, :, cc * 768:(cc + 1) * 768]
                self.dma("sp", wt[:], src, w=[wt])
                for mi in range(6):
                    m = cc * 6 + mi
                    for kt in range(KT):
                        self.mm(ps[:, m * 2:m * 2 + 2], wt[:, kt, mi * 128:(mi + 1) * 128], cact[:, kt * 2:kt * 2 + 2],
                                start=(kt == 0), stop=(kt == KT - 1), r=[wt, cact], w=[ps])
            self.V("tensor_tensor", self.modT[l][:], ps[:, 0:96].rearrange("p (m r) -> p m r", r=2),
                   bm[:, l, :].unsqueeze(2).broadcast_to([128, 48, 2]), ALU.add, r=[ps, bm], w=[self.modT[l]])

    def mod_col(self, l, idx, r):
        return lambda kt: self.modT[l][:, idx * 8 + kt, r:r + 1]

    def load_weight_bf16(self, dst, src2d, ncols):
        step = 2048
        for kt in range(KT):
            c0 = 0
            while c0 < ncols:
                c1 = min(ncols, c0 + step)
                self.dma("pool", dst[:, kt, c0:c1], src2d[kt * 128:(kt + 1) * 128, c0:c1], w=[dst])
                c0 = c1

    def x_src(self, l):
        return self.xin if l == 0 else self.X

    def make_hT(self, l, ti, xt, hT, s1p, sh, pst):
        r = 1 if ti < self.NTC else 0
        for kt in range(KT):
            ps = pst[kt // 4]
            self.tr(ps[:, (kt % 4) * 128:(kt % 4 + 1) * 128], xt[:, kt * 128:(kt + 1) * 128], self.ident_f[:],
                    r=[xt, self.ident_f], w=[ps])
        for kt in range(KT):
            ps = pst[kt // 4]
            src = ps[:, (kt % 4) * 128:(kt % 4 + 1) * 128]
            if kt % 2 == 0:
                self.V("tensor_scalar", hT[:, kt, :], src, s1p[:, kt, r:r + 1], sh(kt, r), ALU.mult, ALU.add,
                       r=[ps, s1p, self.modT[l]], w=[hT])
            else:
                self.A("activation", hT[:, kt, :], src, AF.Identity, bias=sh(kt, r), scale=s1p[:, kt, r:r + 1],
                       r=[ps, s1p, self.modT[l]], w=[hT])

    def setup_persistent(self):
        self.consts()
        self.modT = [self.sb([128, 48, 2], F32, True) for _ in range(DEPTH)]
        self.nmax = self.sb([128, 16], F32, True)
        self.wts = self.sb([128, self.NT, 2], F32, True)
        self.Msel = self.sb([128, self.NT, 2, 32], F32, True)
        NTL = self.TL // 128
        self.cos = self.sb([128, NTL, 32], F32, True)
        self.sin = self.sb([128, NTL, 32], F32, True)
        self.dma("sp", self.cos[:], self.rope_cos.rearrange("(n p) f -> p n f", p=128), w=[self.cos])
        self.dma("sp", self.sin[:], self.rope_sin.rearrange("(n p) f -> p n f", p=128), w=[self.sin])
        zt = self.sb([128, D], BF16)
        self.V("memset", zt[:], 0.0, w=[zt])
        for b in range(self.NB):
            self.dma("sp", self.XB[b * 128:(b + 1) * 128, :], zt[:], r=[zt], w=["XB"])

    def rope(self, ps, o, lt, tmps):
        G = o.shape[1]
        if not hasattr(o, "ap"):
            pass
        pst = ps
        p3 = pst[:, 0:G * 64].rearrange("p (g d) -> p g d", d=64)
        x1, x2 = p3[:, :, 0:32], p3[:, :, 32:64]
        cb = self.cos[:, lt, :].unsqueeze(1).broadcast_to([128, G, 32])
        sbn = self.sin[:, lt, :].unsqueeze(1).broadcast_to([128, G, 32])
        ta, tb = tmps
        rs = [self.cos, self.sin]
        self.V("tensor_tensor", ta[:, 0:G, :], x1, cb, ALU.mult, r=[pst, *rs], w=[ta])
        self.V("tensor_tensor", tb[:, 0:G, :], x2, sbn, ALU.mult, r=[pst, *rs], w=[tb])
        self.V("tensor_tensor", o[:, :, 0:32], ta[:, 0:G, :], tb[:, 0:G, :], ALU.subtract, r=[ta, tb], w=[o])
        self.V("tensor_tensor", ta[:, 0:G, :], x1, sbn, ALU.mult, r=[pst, *rs], w=[ta])
        self.V("tensor_tensor", tb[:, 0:G, :], x2, cb, ALU.mult, r=[pst, *rs], w=[tb])
        self.V("tensor_tensor", o[:, :, 32:64], ta[:, 0:G, :], tb[:, 0:G, :], ALU.add, r=[ta, tb], w=[o])

    def proj_even(self, l):
        i = l // 2
        NT, NTC = self.NT, self.NTC
        self.new_phase()
        wb = self.sb([128, KT, EVEN_IN], BF16)
        self.load_weight_bf16(wb, self.ev_w_in[i], EVEN_IN)
        s1p = self.sb([128, 8, 2], F32)
        self.V("tensor_scalar", s1p[:], self.modT[l][:, 8:16, :], 1.0, None, ALU.add, r=[self.modT[l]], w=[s1p])
        sh = lambda kt, r: self.modT[l][:, kt, r:r + 1]
        gb = self.sb([128, 16], F32)
        self.dma("sp", gb[:], self.ev_gate_b[i:i + 1, :].partition_broadcast(128), w=[gb])
        self.V("memset", self.nmax[:], 0.0, w=[self.nmax])
        xts = Rot([self.sb([128, D], F32) for _ in range(2)])
        hTs = Rot([self.sb([128, KT, 128], BF16) for _ in range(2)])
        os_ = Rot([self.sb([128, 8, 64], F32) for _ in range(2)])
        ta, tb = self.sb([128, 8, 32], F32), self.sb([128, 8, 32], F32)
        sq = self.sb([128, 8, 64], F32)
        nrm = self.sb([128, 8], F32)
        obs = Rot([self.sb([128, 512], BF16) for _ in range(2)])
        stg = Rot([self.sb([128, 4, 128], BF16) for _ in range(2)])
        vbs = Rot([self.sb([128, 512], BF16) for _ in range(2)])
        og = self.sb([128, 512], F32)
        g = self.sb([128, 16], F32)
        e4 = self.sb([128, 4], F32)
        stg2 = self.sb([128, 8, 128], BF16)
        rawbs = Rot([self.sb([128, 512], BF16) for _ in range(2)])
        psf, psb = self.psf, self.psb
        xsrc = self.x_src(l)
        for ti in range(NT):
            t0 = ti * 128
            is_ctx = ti < NTC
            xt = xts.next()
            self.dma("sp", xt[:], xsrc[t0:t0 + 128, :], r=["X"], w=[xt])
            hT = hTs.next()
            self.make_hT(l, ti, xt, hT, s1p, sh, [psf[0], psf[1]])
            for qi, (c0, dst, dname) in enumerate(((0, self.qTA, "qTA"), (512, self.kTA, "kTA"))):
                ps = psf[2 + qi]
                for kt in range(KT):
                    self.mm(ps[:, :], hT[:, kt, :], wb[:, kt, c0:c0 + 512], start=(kt == 0), stop=(kt == KT - 1),
                            r=[hT, wb], w=[ps])
                o = os_.next()
                if is_ctx:
                    self.A("copy", o[:].rearrange("p g d -> p (g d)"), ps[:, :], r=[ps], w=[o])
                else:
                    self.rope(ps, o, ti - NTC, (ta, tb))
                self.V("tensor_tensor", sq[:], o[:], o[:], ALU.mult, r=[o], w=[sq])
                self.V("tensor_reduce", nrm[:], sq[:], axis=AX.X, op=ALU.add, r=[sq], w=[nrm])
                self.V("tensor_tensor", self.nmax[:, qi * 8:qi * 8 + 8], self.nmax[:, qi * 8:qi * 8 + 8], nrm[:], ALU.max,
                       r=[nrm, self.nmax], w=[self.nmax])
                ob = obs.next()
                self.A("activation", ob[:], o[:].rearrange("p g d -> p (g d)"), AF.Copy, scale=(0.125 if qi == 0 else 1.0),
                       r=[o], w=[ob])
                pb = psb[qi]
                for pr in range(4):
                    self.tr(pb[:, pr * 128:(pr + 1) * 128], ob[:, pr * 128:(pr + 1) * 128], self.ident_b[:],
                            r=[ob, self.ident_b], w=[pb])
                st = stg.next()
                self.V("tensor_copy", st[:].rearrange("p a b -> p (a b)"), pb[:, 0:512], r=[pb], w=[st])
                self.dma("sp", dst.rearrange("h p t -> p h t")[:, :, t0:t0 + 128], st[:], r=[st], w=[dname])
            for vi, (c0, dst, dname) in enumerate(((1024, self.vA, "vA"), (2560, self.vB, "vB"))):
                ps = psf[4 + vi]
                for kt in range(KT):
                    self.mm(ps[:, :], hT[:, kt, :], wb[:, kt, c0:c0 + 512], start=(kt == 0), stop=(kt == KT - 1),
                            r=[hT, wb], w=[ps])
                vb = vbs.next()
                self.A("copy", vb[:], ps[:, :], r=[ps], w=[vb])
                self.dma("sp", dst[t0:t0 + 128, :], vb[:], r=[vb], w=[dname])
            ps = psf[4]
            for kt in range(KT):
                self.mm(ps[:, :], hT[:, kt, :], wb[:, kt, 3072:3584], start=(kt == 0), stop=(kt == KT - 1), r=[hT, wb], w=[ps])
            self.A("activation", og[:], ps[:, :], AF.Sigmoid, r=[ps], w=[og])
            self.dma("sp", self.ogB[t0:t0 + 128, :], og[:], r=[og], w=["ogB"])
            ps = psf[5]
            for kt in range(KT):
                self.mm(ps[:, 0:16], hT[:, kt, :], wb[:, kt, 3584:3600], start=(kt == 0), stop=(kt == KT - 1), r=[hT, wb], w=[ps])
            self.V("tensor_tensor", g[:], ps[:, 0:16], gb[:], ALU.add, r=[ps, gb], w=[g])
            for off in (4, 12):
                self.A("activation", e4[:], g[:, off:off + 4], AF.Exp, scale=-1.0, r=[g], w=[e4])
                self.A("activation", e4[:], e4[:], AF.Ln, bias=1.0, r=[e4], w=[e4])
                self.V("tensor_scalar", g[:, off:off + 4], e4[:], -1.0, None, ALU.mult, r=[e4], w=[g])
            self.dma("sp", self.gB[t0:t0 + 128, :], g[:], r=[g], w=["gB"])
            for half in range(2):
                ps = psf[2 + half]
                c0 = 1536 + half * 512
                for kt in range(KT):
                    self.mm(ps[:, :], hT[:, kt, :], wb[:, kt, c0:c0 + 512], start=(kt == 0), stop=(kt == KT - 1), r=[hT, wb], w=[ps])
                rb_ = rawbs.next()
                if half == 0:
                    self.A("copy", rb_[:], ps[:, :], r=[ps], w=[rb_])
                else:
                    self.V("tensor_copy", rb_[:], ps[:, :], r=[ps], w=[rb_])
                for cc in range(4):
                    self.tr(psb[half][:, cc * 128:(cc + 1) * 128], rb_[:, cc * 128:(cc + 1) * 128], self.ident_b[:],
                            r=[rb_, self.ident_b], w=[psb[half]])
                if half == 0:
                    self.V("tensor_copy", stg2[:, 0:4, :].rearrange("p a b -> p (a b)"), psb[0][:, 0:512], r=[psb[0]], w=[stg2])
                else:
                    self.A("copy", stg2[:, 4:8, :].rearrange("p a b -> p (a b)"), psb[1][:, 0:512], r=[psb[1]], w=[stg2])
            self.dma("sp", self.qkraw.rearrange("c p t -> p c t")[:, :, t0:t0 + 128], stg2[:], r=[stg2], w=["qkraw"])

    def compute_negm(self, scale):
        psf = self.psf
        self.tr(psf[0][0:16, 0:128], self.nmax[:, 0:16], self.ident_f[:], r=[self.nmax, self.ident_f], w=[psf[0]])
        mx = self.sb([16, 1], F32)
        self.V("tensor_reduce", mx[:], psf[0][0:16, 0:128], axis=AX.X, op=ALU.max, r=[psf[0]], w=[mx])
        dg = self.sb([16, 16], F32)
        self.V("tensor_scalar", dg[:], self.ident_f[0:16, 0:16], mx[:, 0:1], None, ALU.mult, r=[mx, self.ident_f], w=[dg])
        self.mm(psf[1][:, 0:16], self.ones_f[0:16, 0:128], dg[:], r=[self.ones_f, dg], w=[psf[1]])
        mxb = self.sb([128, 16], F32)
        self.V("tensor_copy", mxb[:], psf[1][:, 0:16], r=[psf[1]], w=[mxb])
        negm = self.sb([128, 8], F32)
        self.V("tensor_tensor", negm[:], mxb[:, 0:8], mxb[:, 8:16], ALU.mult, r=[mxb], w=[negm])
        self.A("activation", negm[:], negm[:], AF.Sqrt, r=[negm], w=[negm])
        self.V("tensor_scalar", negm[:], negm[:], -float(scale), None, ALU.mult, r=[negm], w=[negm])
        return negm

    def qblocks(self):
        blocks = []
        q0 = 0
        while q0 < self.TC:
            nq = min(512, self.TC - q0)
            blocks.append((q0, nq, list(range(self.NTC))))
            q0 += nq
        while q0 < self.T:
            nq = min(512, self.T - q0)
            blocks.append((q0, nq, list(range(self.NT))))
            q0 += nq
        return blocks

    def run_attn_jobs(self, jobs, pTs, st_rot):
        steps = [(job, ki, kt) for job in jobs for ki, kt in enumerate(job["ktiles"])]
        recs = {}

        def emit_S(idx):
            job, ki, kt = steps[idx]
            nq, kr = job["nq"], job["krow"]
            st = st_rot.next()
            self.mm(st[:, 0:nq], job["kT"][kr[0]:kr[1], kt * 128:(kt + 1) * 128], job["qT"][kr[0]:kr[1], job["q0"]:job["q0"] + nq],
                    r=[job["kT"], job["qT"]], w=[st])
            pT = pTs.next()
            self.A("activation", pT[:, 0:nq], st[:, 0:nq], AF.Exp, bias=job["negm_col"], scale=1.0, r=[st, job["ndep"]], w=[pT])
            recs[idx] = pT

        def emit_PV(idx):
            job, ki, kt = steps[idx]
            pT = recs.pop(idx)
            nq, dv = job["nq"], job["dv"]
            nsub = nq // 128
            first, last = ki == 0, ki == len(job["ktiles"]) - 1
            o_ps, s_ps = job["o_ps"], job["s_ps"]
            self.mm(o_ps[0:dv, 0:nq], job["v_fn"](kt), pT[:, 0:nq], start=first, stop=last, r=[pT, job["vdep"]], w=[o_ps])
            if job["sum_mode"] == "bc":
                self.mm(s_ps[:, 0:nq], self.ones_b[:, :], pT[:, 0:nq], start=first, stop=last, r=[pT, self.ones_b], w=[s_ps])
            if last:
                job["post"]()

        if not steps:
            return
        emit_S(0)
        for idx in range(len(steps)):
            if idx + 1 < len(steps):
                emit_S(idx + 1)
            emit_PV(idx)

    def rowsum_to_col(self, s_ps, nq, srow):
        nsub = nq // 128
        self.V("tensor_copy", srow[0:1, 0:nq], s_ps[0:1, 0:nq], r=[s_ps], w=[srow])
        for sub in range(nsub):
            self.mm(s_ps[:, sub:sub + 1], srow[0:1, sub * 128:(sub + 1) * 128], self.ones_f[0:1, 0:1], start=(sub == 0), stop=True,
                    r=[srow, self.ones_f], w=[s_ps], skip=True)

    def attnA(self, l):
        i = l // 2
        T, NT = self.T, self.NT
        lam_init = 0.8 - 0.6 * math.exp(-0.3 * l)
        self.new_phase()
        psf = self.psf
        negm = self.compute_negm(0.125)
        lrow = self.sb([128, 256], F32)
        self.dma("sp", lrow[:], self.ev_lambda[i:i + 1, :].partition_broadcast(128), w=[lrow])
        lp = self.sb([128, 2, 64], F32)
        l4 = lrow[:].rearrange("p (a d) -> p a d", d=64)
        self.V("tensor_tensor", lp[:, 0, :], l4[:, 0, :], l4[:, 1, :], ALU.mult, r=[lrow], w=[lp])
        self.V("tensor_tensor", lp[:, 1, :], l4[:, 2, :], l4[:, 3, :], ALU.mult, r=[lrow, lp], w=[lp])
        ls = self.sb([128, 2], F32)
        self.V("tensor_reduce", ls[:], lp[:], axis=AX.X, op=ALU.add, r=[lp], w=[ls])
        self.A("activation", ls[:], ls[:], AF.Exp, r=[ls], w=[ls])
        neglam = self.sb([128, 1], F32)
        self.V("tensor_tensor", neglam[:], ls[:, 1:2], ls[:, 0:1], ALU.subtract, r=[ls], w=[neglam])
        self.V("tensor_scalar", neglam[:], neglam[:], -lam_init, None, ALU.add, r=[neglam], w=[neglam])
        subg = self.sb([128, 128], F32)
        self.dma("sp", subg[:], self.ev_subln_g[i:i + 1, :].partition_broadcast(128), w=[subg])
        self.V("tensor_scalar", subg[:], subg[:], 1.0 - lam_init, None, ALU.mult, r=[subg], w=[subg])
        kTs = Rot([self.sb([128, T], BF16) for _ in range(2)])
        qTs = Rot([self.sb([128, T], BF16) for _ in range(2)])
        vhs = Rot([self.sb([128, NT, 128], BF16) for _ in range(2)])
        pTs = Rot([self.sb([128, 512], BF16) for _ in range(3)])
        st_rot = Rot([psf[4], psf[5]])
        r1 = self.sb([128, 512], F32)
        r2 = self.sb([128, 512], F32)
        t1 = self.sb([128, 512], F32)
        t2 = self.sb([128, 512], F32)
        outb = Rot([self.sb([128, 512], BF16) for _ in range(2)])
        subgc = self.sb([128, 1], F32)
        self.dma("sp", subgc[:], self.ev_subln_g[i:i + 1, :].rearrange("o d -> d o"), w=[subgc])
        self.V("tensor_scalar", subgc[:], subgc[:], 1.0 - lam_init, None, ALU.mult, r=[subgc], w=[subgc])
        srows = Rot([self.sb([1, 512], F32) for _ in range(2)])
        jobs = []
        for h in range(4):
            kT, qT, vh = kTs.next(), qTs.next(), vhs.next()
            def load(h=h, kT=kT, qT=qT, vh=vh):
                self.dma("sp", kT[:], self.kTA[h], r=["kTA"], w=[kT])
                self.dma("sp", qT[:], self.qTA[h], r=["qTA"], w=[qT])
                self.dma("sp", vh[:], self.vA.rearrange("(n p) f -> p n f", p=128)[:, :, h * 128:(h + 1) * 128], r=["vA"], w=[vh])
            load()
            for (q0, nq, ktiles) in self.qblocks():
                nsub = nq // 128

                def post(h=h, q0=q0, nq=nq, nsub=nsub):
                    V = self.V
                    V("reciprocal", r1[:, 0:nq], psf[1][:, 0:nq], r=[psf[1]], w=[r1])
                    V("reciprocal", r2[:, 0:nq], psf[3][:, 0:nq], r=[psf[3]], w=[r2])
                    V("tensor_tensor", t1[:, 0:nq], psf[0][:, 0:nq], r1[:, 0:nq], ALU.mult, r=[psf[0], r1], w=[t1])
                    V("scalar_tensor_tensor", t2[:, 0:nq], psf[2][:, 0:nq], neglam[:, 0:1], r2[:, 0:nq], ALU.mult, ALU.mult,
                      r=[psf[2], neglam, r2], w=[t2])
                    self.G("tensor_tensor", t1[:, 0:nq], t1[:, 0:nq], t2[:, 0:nq], ALU.add, r=[t1, t2], w=[t1])
                    self.G("tensor_tensor", t2[:, 0:nq], t1[:, 0:nq], t1[:, 0:nq], ALU.mult, r=[t1, t2], w=[t2])
                    self.mm(psf[1][:, 0:nq], self.ones_f[:, :], t2[:, 0:nq], r=[self.ones_f, t2], w=[psf[1]])
                    V("tensor_scalar", r1[:, 0:nq], psf[1][:, 0:nq], 1.0 / 128, EPS, ALU.mult, ALU.add, r=[psf[1], r1], w=[r1])
                    self.A("activation", r1[:, 0:nq], r1[:, 0:nq], AF.Sqrt, r=[r1], w=[r1])
                    V("reciprocal", r1[:, 0:nq], r1[:, 0:nq], r=[r1], w=[r1])
                    V("tensor_tensor", t1[:, 0:nq], t1[:, 0:nq], r1[:, 0:nq], ALU.mult, r=[t1, r1], w=[t1])
                    ob = outb.next()
                    self.A("activation", ob[:, 0:nq], t1[:, 0:nq], AF.Copy, scale=subgc[:, 0:1], r=[t1, subgc], w=[ob])
                    self.dma("sp", self.catT[h][:, q0:q0 + nq], ob[:, 0:nq], r=[ob], w=["catT"])

                for c in range(2):
                    jobs.append(dict(kT=kT, qT=qT, krow=(c * 64, (c + 1) * 64), v_fn=(lambda kt, vh=vh: vh[:, kt, :]), vdep=vh, dv=128,
                                     q0=q0, nq=nq, ktiles=ktiles, negm_col=negm[:, h * 2 + c:h * 2 + c + 1], ndep=negm,
                                     o_ps=psf[c * 2], s_ps=psf[c * 2 + 1], sum_mode="bc", post=(post if c == 1 else (lambda: None))))
            self.run_attn_jobs(jobs, pTs, st_rot)
            jobs = []


def rope_tables(TL):
    n_rows = TL // 64
    row = np.repeat(np.arange(n_rows, dtype=np.float32), 64)
    col = np.tile(np.arange(64, dtype=np.float32), n_rows)
    quarter = 16
    inv_freq = (10000.0 ** (-np.arange(quarter, dtype=np.float32) / quarter)).astype(np.float32)
    ang = np.concatenate([row[:, None] * inv_freq, col[:, None] * inv_freq], -1).astype(np.float32)
    return np.cos(ang).astype(np.float32), np.sin(ang).astype(np.float32)


_SHARED_CACHE = {}


def prep_shared(inp, TL):
    f = np.float32
    sh = {}
    sh["w_mod"] = np.ascontiguousarray(inp["w_mod"], f)
    sh["bmodT"] = np.ascontiguousarray(inp["b_mod"].reshape(DEPTH, 48, 128).transpose(0, 2, 1), f)
    sh["ln_g"] = np.ascontiguousarray(inp["ln_g"].reshape(DEPTH * 2, D), f)
    sh["ln_b"] = np.ascontiguousarray(inp["ln_b"].reshape(DEPTH * 2, D), f)
    sh["ev_w_in"] = np.ascontiguousarray(inp["ev_w_in"], f)
    sh["ev_w_out"] = np.ascontiguousarray(inp["ev_w_out"], f)
    sh["ev_lambda"] = np.ascontiguousarray(inp["ev_lambda"].reshape(2, 256), f)
    sh["ev_subln_g"] = np.ascontiguousarray(inp["ev_subln_g"], f)
    cw = inp["ev_conv_w"]
    sh["convwT"] = np.ascontiguousarray(cw.reshape(2, 3, 8, 128).transpose(0, 3, 2, 1).reshape(2, 128, 24), f)
    sh["convbT"] = np.ascontiguousarray(inp["ev_conv_b"].reshape(2, 8, 128).transpose(0, 2, 1), f)
    sh["ev_gate_b"] = np.ascontiguousarray(inp["ev_gate_b"].reshape(2, 16), f)
    sh["ev_mnorm_g"] = np.ascontiguousarray(inp["ev_mnorm_g"], f)
    sh["od_w_in"] = np.ascontiguousarray(inp["od_w_in"], f)
    sh["od_w_out"] = np.ascontiguousarray(inp["od_w_out"], f)
    sh["od_decay"] = np.ascontiguousarray(inp["od_decay"].reshape(2, 8), f)
    sh["od_qk_g"] = np.ascontiguousarray(inp["od_qk_g"].reshape(2, 128), f)
    sh["wr"] = np.ascontiguousarray(np.concatenate([inp["moe_w_group"], inp["moe_w_router"]], -1), f)
    sh["br"] = np.ascontiguousarray(np.concatenate([inp["moe_b_group"], inp["moe_b_router"]], -1), f)
    wg = inp["moe_w_gate"].reshape(DEPTH, NEXP, 8, 128, 512)
    wu = inp["moe_w_up"].reshape(DEPTH, NEXP, 8, 128, 512)
    wd = inp["moe_w_down"].reshape(DEPTH, NEXP, 4, 128, 1024)
    def lay(w, k0, k1):
        return np.ascontiguousarray(w[:, :, k0:k1].transpose(0, 1, 3, 2, 4).reshape(DEPTH * NEXP * 128, 2048), f)
    sh["wexp0"] = lay(wg, 0, 4)
    sh["wexp1"] = lay(wg, 4, 8)
    sh["wexp2"] = lay(wu, 0, 4)
    sh["wexp3"] = lay(wu, 4, 8)
    sh["wexp4"] = lay(wd, 0, 2)
    sh["wexp5"] = lay(wd, 2, 4)
    c, s = rope_tables(TL)
    sh["rope_cos"], sh["rope_sin"] = c, s
    return sh


def prep_core(inp, b):
    f = np.float32
    d = {}
    d["xin"] = np.ascontiguousarray(np.concatenate([inp["ctx"][b], inp["x"][b]], 0), f)
    c2 = np.stack([inp["c"][b], inp["c_ctx"]], 0)
    d["cT"] = np.ascontiguousarray(c2.reshape(2, 8, 128).transpose(2, 1, 0).reshape(128, 16), f)
    return d


def _mlstm(self, l):
    i = l // 2
    T, NT, NTC = self.T, self.NT, self.NTC
    self.new_phase()
    psf, psb = self.psf, self.psb
    V, A, G, mm = self.V, self.A, self.G, self.mm
    lnscale = -0.5 * math.log(128.0)
    qT = self.sb([128, 4, T], BF16)
    kT = self.sb([128, 4, T], BF16)
    cw = self.sb([128, 24], F32)
    cb = self.sb([128, 8], F32)
    self.dma("sp", cw[:], self.convwT[i], w=[cw])
    self.dma("sp", cb[:], self.convbT[i], w=[cb])
    PIECE = 1024
    rbs = Rot([self.sb([128, PIECE + 2], BF16) for _ in range(2)])
    accs = Rot([self.sb([128, PIECE], F32) for _ in range(2)])
    for cc in range(8):
        dstT = qT if cc < 4 else kT
        for (s0, s1) in ((0, self.TC), (self.TC, T)):
            a = s0
            while a < s1:
                b = min(s1, a + PIECE)
                n = b - a
                rb = rbs.next()
                lo = a - 1 if a > s0 else a
                hi = b + 1 if b < s1 else b
                if a == s0:
                    V("memset", rb[:, 0:1], 0.0, w=[rb])
                if b == s1:
                    V("memset", rb[:, n + 1:n + 2], 0.0, w=[rb])
                self.dma("sp", rb[:, 1 - (a - lo):1 + n + (hi - b)], self.qkraw[cc][:, lo:hi], r=["qkraw"], w=[rb])
                acc = accs.next()
                V("tensor_scalar", acc[:, 0:n], rb[:, 0:n], cw[:, cc * 3:cc * 3 + 1], None, ALU.mult, r=[rb, cw], w=[acc])
                V("scalar_tensor_tensor", acc[:, 0:n], rb[:, 1:n + 1], cw[:, cc * 3 + 1:cc * 3 + 2], acc[:, 0:n], ALU.mult, ALU.add,
                  r=[rb, cw, acc], w=[acc])
                V("scalar_tensor_tensor", acc[:, 0:n], rb[:, 2:n + 2], cw[:, cc * 3 + 2:cc * 3 + 3], acc[:, 0:n], ALU.mult, ALU.add,
                  r=[rb, cw, acc], w=[acc])
                A("activation", dstT[:, cc % 4, a:b], acc[:, 0:n], AF.Silu, bias=cb[:, cc:cc + 1], r=[acc, cb], w=[dstT])
                a = b
    vp = self.sb([128, NT, 4, 129], BF16)
    V("memset", vp[:, :, :, 128:129], 1.0, w=[vp])
    for n in range(NT):
        self.dma("sp", vp[:, n, :, 0:128], self.vB[n * 128:(n + 1) * 128, :].rearrange("p (h d) -> p h d", d=128), r=["vB"], w=[vp])
    gt = self.sb([128, NT, 16], F32)
    self.dma("sp", gt[:], self.gB.rearrange("(n p) g -> p n g", p=128), r=["gB"], w=[gt])
    mg = self.sb([128, 512], F32)
    self.dma("sp", mg[:], self.ev_mnorm_g[i:i + 1, :].partition_broadcast(128), w=[mg])
    S32 = self.sb([128, 4, 129], F32)
    Sb = self.sb([128, 4, 129], BF16)
    lfbcs = Rot([self.sb([128, 128], F32) for _ in range(4)])
    colterm = self.sb([128, 4], F32)
    X = self.sb([128, 4, 128], F32)
    At = self.sb([128, 4, 128], BF16)
    eB = self.sb([128, 4, 128], F32)
    qeb = self.sb([128, 4, 128], BF16)
    wk = self.sb([128, 4], F32)
    kw = self.sb([128, 4, 128], BF16)
    dn = self.sb([128, 4], F32)
    hout = self.sb([128, 4, 128], F32)
    hbs = Rot([self.sb([128, 512], F32) for _ in range(2)])
    ogs = Rot([self.sb([128, 512], F32) for _ in range(2)])
    st4 = self.sb([128, 4], F32)
    sq = self.sb([128, 4, 128], F32)
    catb = Rot([self.sb([128, 512], BF16) for _ in range(2)])
    BR, KQ, ND0, ND1 = psf[0], psf[2], psf[3], psf[4]
    BR3 = BR[:, :].rearrange("p (h t) -> p h t", t=128)
    KTp = psb[0]
    UPs = [psf[5][:, 0:258], psf[1][:, 128:386]]
    UPt = [psf[5], psf[1]]
    NDs = [ND0, ND1]
    for direction in ("bwd", "fwd"):
        fwd = direction == "fwd"
        V("memset", S32[:], 0.0, w=[S32])
        V("memset", Sb[:], 0.0, w=[Sb])
        order = list(range(NT)) if fwd else (list(range(NTC - 1, -1, -1)) + list(range(NT - 1, NTC - 1, -1)))
        tri = self.triU_f if fwd else self.triL_f
        e_idx = 127 if fwd else 0
        lic, lfc = (0, 4) if fwd else (8, 12)
        for c in order:
            tok0, tok1 = c * 128, (c + 1) * 128
            for h in range(4):
                lfbc = lfbcs.next()
                V("tensor_scalar", lfbc[:], self.ones_f[:], gt[:, c, lfc + h:lfc + h + 1], None, ALU.mult, r=[self.ones_f, gt], w=[lfbc])
                mm(BR[:, h * 128:(h + 1) * 128], lfbc[:], tri[:], r=[lfbc, tri], w=[BR])
            mm(psf[1][:, 0:4], tri[:], gt[:, c, lfc:lfc + 4], r=[tri, gt], w=[psf[1]])
            V("scalar_tensor_tensor", colterm[:], gt[:, c, lic:lic + 4], lnscale, psf[1][:, 0:4], ALU.add, ALU.subtract,
              r=[gt, psf[1]], w=[colterm])
            V("tensor_tensor", X[:], BR3, colterm[:].unsqueeze(2).broadcast_to([128, 4, 128]), ALU.add, r=[BR, colterm], w=[X])
            A("activation", X[:], X[:], AF.Exp, r=[X], w=[X])
            G("tensor_tensor", X[:], X[:], tri[:].unsqueeze(1).broadcast_to([128, 4, 128]), ALU.mult, r=[X, tri], w=[X])
            for h in range(4):
                mm(KQ[:, h * 128:(h + 1) * 128], kT[:, h, tok0:tok1], qT[:, h, tok0:tok1], r=[kT, qT], w=[KQ])
            V("tensor_tensor", At[:], KQ[:, :].rearrange("p (h t) -> p h t", t=128), X[:], ALU.mult, r=[KQ, X], w=[At])
            A("activation", eB[:], BR3, AF.Exp, r=[BR], w=[eB])
            G("tensor_tensor", qeb[:], qT[:, :, tok0:tok1], eB[:], ALU.mult, r=[qT, eB], w=[qeb])
            V("tensor_tensor", wk[:], colterm[:], BR3[:, :, e_idx], ALU.add, r=[colterm, BR], w=[wk])
            A("activation", wk[:], wk[:], AF.Exp, r=[wk], w=[wk])
            for h in range(4):
                self.tr(KTp[:, h * 128:(h + 1) * 128], kT[:, h, tok0:tok1], self.ident_b[:], r=[kT, self.ident_b], w=[KTp])
            V("tensor_tensor", kw[:], KTp[:, 0:512].rearrange("p (h d) -> p h d", d=128), wk[:].unsqueeze(2).broadcast_to([128, 4, 128]),
              ALU.mult, r=[KTp, wk], w=[kw])
            for h in range(4):
                nd = NDs[h // 2][:, (h % 2) * 129:(h % 2 + 1) * 129]
                mm(nd, At[:, h, :], vp[:, c, h, :], start=(h % 2 == 0), stop=False, r=[At, vp], w=[NDs[h // 2]], skip=True)
                mm(nd, qeb[:, h, :], Sb[:, h, :], start=False, stop=True, r=[qeb, Sb], w=[NDs[h // 2]], skip=True)
            for b2 in range(2):
                nd3 = NDs[b2][:, 0:258].rearrange("p (j e) -> p j e", e=129)
                A("activation", dn[:, b2 * 2:b2 * 2 + 2], nd3[:, :, 128], AF.Abs, r=[NDs[b2]], w=[dn])
            V("tensor_scalar", dn[:], dn[:], 1.0, None, ALU.max, r=[dn], w=[dn])
            V("reciprocal", dn[:], dn[:], r=[dn], w=[dn])
            for b2 in range(2):
                nd3 = NDs[b2][:, 0:258].rearrange("p (j e) -> p j e", e=129)
                V("tensor_tensor", hout[:, b2 * 2:b2 * 2 + 2, :], nd3[:, :, 0:128],
                  dn[:, b2 * 2:b2 * 2 + 2].unsqueeze(2).broadcast_to([128, 2, 128]), ALU.mult, r=[NDs[b2], dn], w=[hout])
            hflat = hout[:].rearrange("p h d -> p (h d)")
            if not fwd:
                self.dma("sp", self.hB[tok0:tok1, :], hflat, r=[hout], w=["hB"])
            else:
                hb, og = hbs.next(), ogs.next()
                self.dma("sp", hb[:], self.hB[tok0:tok1, :], r=["hB"], w=[hb])
                self.dma("sp", og[:], self.ogB[tok0:tok1, :], r=["ogB"], w=[og])
                V("tensor_tensor", hflat, hflat, hb[:], ALU.add, r=[hout, hb], w=[hout])
                V("tensor_tensor", hflat, hflat, og[:], ALU.mult, r=[hout, og], w=[hout])
                V("tensor_reduce", st4[:], hout[:], axis=AX.X, op=ALU.add, r=[hout], w=[st4])
                V("tensor_scalar", st4[:], st4[:], 1.0 / 128, None, ALU.mult, r=[st4], w=[st4])
                V("tensor_tensor", hout[:], hout[:], st4[:].unsqueeze(2).broadcast_to([128, 4, 128]), ALU.subtract, r=[hout, st4], w=[hout])
                G("tensor_tensor", sq[:], hout[:], hout[:], ALU.mult, r=[hout], w=[sq])
                V("tensor_reduce", st4[:], sq[:], axis=AX.X, op=ALU.add, r=[sq], w=[st4])
                V("tensor_scalar", st4[:], st4[:], 1.0 / 128, EPS, ALU.mult, ALU.add, r=[st4], w=[st4])
                A("activation", st4[:], st4[:], AF.Sqrt, r=[st4], w=[st4])
                V("reciprocal", st4[:], st4[:], r=[st4], w=[st4])
                V("tensor_tensor", hout[:], hout[:], st4[:].unsqueeze(2).broadcast_to([128, 4, 128]), ALU.mult, r=[hout, st4], w=[hout])
                cbt = catb.next()
                V("tensor_tensor", cbt[:], hflat, mg[:], ALU.mult, r=[hout, mg], w=[cbt])
                self.dma("sp", self.cat[tok0:tok1, 512:1024], cbt[:], r=[cbt], w=["cat"])
            for h in range(4):
                up = UPs[h // 2][:, (h % 2) * 129:(h % 2 + 1) * 129]
                mm(up, kw[:, h, :], vp[:, c, h, :], start=(h % 2 == 0), stop=True, r=[kw, vp], w=[UPt[h // 2]], skip=True)
            for h in range(4):
                up = UPs[h // 2][:, (h % 2) * 129:(h % 2 + 1) * 129]
                V("scalar_tensor_tensor", S32[:, h, :], S32[:, h, :], eB[:, h, e_idx:e_idx + 1], up, ALU.mult, ALU.add,
                  r=[S32, eB, UPt[h // 2]], w=[S32])
            A("copy", Sb[:], S32[:], r=[S32], w=[Sb])


Builder.mlstm = _mlstm


def _ln_tile(self, z, st, junk, lng, lnb, out):
    V, A = self.V, self.A
    V("tensor_reduce", st[:, 0:1], z[:], axis=AX.X, op=ALU.add, r=[z], w=[st])
    V("tensor_scalar", st[:, 0:1], st[:, 0:1], 1.0 / D, None, ALU.mult, r=[st], w=[st])
    V("tensor_scalar", z[:], z[:], st[:, 0:1], None, ALU.subtract, r=[z, st], w=[z])
    V("memset", st[:, 1:2], 0.0, r=[st], w=[st])
    A("activation", junk[:], z[:], AF.Square, accum_out=st[:, 1:2], r=[z, st], w=[junk, st])
    V("tensor_scalar", st[:, 1:2], st[:, 1:2], 1.0 / D, EPS, ALU.mult, ALU.add, r=[st], w=[st])
    A("activation", st[:, 1:2], st[:, 1:2], AF.Sqrt, r=[st], w=[st])
    V("reciprocal", st[:, 1:2], st[:, 1:2], r=[st], w=[st])
    V("scalar_tensor_tensor", out[:], z[:], st[:, 1:2], lng[:], ALU.mult, ALU.mult, r=[z, st, lng], w=[out])
    V("tensor_tensor", out[:], out[:], lnb[:], ALU.add, r=[out, lnb], w=[out])


Builder.ln_tile = _ln_tile


def _outproj(self, l, w_out_ap):
    T, NT, NTC = self.T, self.NT, self.NTC
    self.new_phase()
    psf, psb = self.psf, self.psb
    V, A, G, mm = self.V, self.A, self.G, self.mm
    mT = self.modT[l]
    wo = self.sb([128, KT, D], BF16)
    self.load_weight_bf16(wo, w_out_ap, D)
    s4p = self.sb([128, 8, 2], F32)
    V("tensor_scalar", s4p[:], mT[:, 32:40, :], 1.0, None, ALU.add, r=[mT], w=[s4p])
    g2, s4b, sh4b = [], [], []
    for r in range(2):
        t = self.sb([128, D], F32); self.bcast_row(self.mod_col(l, 2, r), t, r=[mT]); g2.append(t)
        t = self.sb([128, D], F32); self.bcast_row(lambda kt, r=r: s4p[:, kt, r:r + 1], t, r=[s4p]); s4b.append(t)
        t = self.sb([128, D], F32); self.bcast_row(self.mod_col(l, 3, r), t, r=[mT]); sh4b.append(t)
    lng = self.sb([128, D], F32)
    lnb = self.sb([128, D], F32)
    self.dma("sp", lng[:], self.ln_g[l * 2:l * 2 + 1, :].partition_broadcast(128), w=[lng])
    self.dma("sp", lnb[:], self.ln_b[l * 2:l * 2 + 1, :].partition_broadcast(128), w=[lnb])
    wrt = self.sb([128, KT, 36], F32)
    self.dma("sp", wrt[:], self.wr[l].rearrange("(kt p) n -> p kt n", p=128), w=[wrt])
    brb = self.sb([128, 36], F32)
    self.dma("sp", brb[:], self.br[l:l + 1, :].partition_broadcast(128), w=[brb])
    catts = Rot([self.sb([128, D], BF16) for _ in range(2)])
    catTs = Rot([self.sb([128, KT, 128], BF16) for _ in range(2)])
    xts = Rot([self.sb([128, D], F32) for _ in range(2)])
    zs = Rot([self.sb([128, D], F32) for _ in range(2)])
    xms = Rot([self.sb([128, D], F32) for _ in range(2)])
    h2s = Rot([self.sb([128, D], F32) for _ in range(2)])
    h2bs = Rot([self.sb([128, D], BF16) for _ in range(2)])
    h2T = self.sb([128, KT, 128], F32)
    junk = self.sb([128, D], F32)
    st = self.sb([128, 2], F32)
    lg = self.sb([128, 36], F32)
    sm = self.sb([128, 16], F32)
    g1h = self.sb([128, 4], F32)
    ge = self.sb([128, 4], F32)
    tmp48 = self.sb([128, 4, 8], F32)
    esel = self.sb([128, 8], F32)
    oh1 = self.sb([128, 8], F32)
    oh2 = self.sb([128, 8], F32)
    msk = self.sb([128, 8], F32)
    xsrc = self.x_src(l)
    for ti in range(NT):
        t0 = ti * 128
        r = 1 if ti < NTC else 0
        fm0 = 0 if l % 2 == 0 else 4
        tm0 = 4 - fm0
        catt = catts.next()
        self.dma("sp", catt[:, 0:512], self.cat[t0:t0 + 128, tm0 * 128:tm0 * 128 + 512], r=["cat"], w=[catt])
        catT = catTs.next()
        self.dma("sp", catT[:, fm0:fm0 + 4, :], self.catT[fm0:fm0 + 4].rearrange("c p t -> p c t")[:, :, t0:t0 + 128], r=["catT"], w=[catT])
        for k4 in range(4):
            self.tr(psb[0][:, k4 * 128:(k4 + 1) * 128], catt[:, k4 * 128:(k4 + 1) * 128], self.ident_b[:], r=[catt, self.ident_b], w=[psb[0]])
        A("copy", catT[:, tm0:tm0 + 4, :].rearrange("p a b -> p (a b)"), psb[0][:, 0:512], r=[psb[0]], w=[catT])
        for half in range(2):
            for kt in range(KT):
                mm(psf[half][:, :], catT[:, kt, :], wo[:, kt, half * 512:(half + 1) * 512], start=(kt == 0), stop=(kt == KT - 1),
                   r=[catT, wo], w=[psf[half]])
        xt = xts.next()
        self.dma("sp", xt[:], xsrc[t0:t0 + 128, :], r=["X"], w=[xt])
        z = zs.next()
        for half in range(2):
            V("tensor_tensor", z[:, half * 512:(half + 1) * 512], psf[half][:, :], g2[r][:, half * 512:(half + 1) * 512], ALU.mult,
              r=[psf[half], g2[r]], w=[z])
        V("scalar_tensor_tensor", z[:], xt[:], ALPHA, z[:], ALU.mult, ALU.add, r=[xt, z], w=[z])
        xm = xms.next()
        self.ln_tile(z, st, junk, lng, lnb, xm)
        self.dma("sp", self.X[t0:t0 + 128, :], xm[:], r=[xm], w=["X"])
        h2 = h2s.next()
        V("tensor_tensor", h2[:], xm[:], s4b[r][:], ALU.mult, r=[xm, s4b[r]], w=[h2])
        G("tensor_tensor", h2[:], h2[:], sh4b[r][:], ALU.add, r=[h2, sh4b[r]], w=[h2])
        h2b = h2bs.next()
        A("copy", h2b[:], h2[:], r=[h2], w=[h2b])
        self.dma("sp", self.H2[t0:t0 + 128, :], h2b[:], r=[h2b], w=["H2"])
        for kt in range(KT):
            self.tr(psf[2 + kt // 4][:, (kt % 4) * 128:(kt % 4 + 1) * 128], h2[:, kt * 128:(kt + 1) * 128], self.ident_f[:],
                    r=[h2, self.ident_f], w=[psf[2 + kt // 4]])
        A("copy", h2T[:, 0:4, :].rearrange("p a b -> p (a b)"), psf[2][:, :], r=[psf[2]], w=[h2T])
        V("tensor_copy", h2T[:, 4:8, :].rearrange("p a b -> p (a b)"), psf[3][:, :], r=[psf[3]], w=[h2T])
        for kt in range(KT):
            mm(psf[4][:, 0:36], h2T[:, kt, :], wrt[:, kt, :], start=(kt == 0), stop=(kt == KT - 1), r=[h2T, wrt], w=[psf[4]])
        V("tensor_tensor", lg[:], psf[4][:, 0:36], brb[:], ALU.add, r=[psf[4], brb], w=[lg])
        V("tensor_reduce", sm[:, 0:1], lg[:, 0:4], axis=AX.X, op=ALU.max, r=[lg], w=[sm])
        V("tensor_scalar", g1h[:], lg[:, 0:4], sm[:, 0:1], None, ALU.is_equal, r=[lg, sm], w=[g1h])
        V("tensor_scalar", sm[:, 1:2], sm[:, 0:1], -1.0, None, ALU.mult, r=[sm], w=[sm])
        V("memset", sm[:, 2:3], 0.0, r=[sm], w=[sm])
        A("activation", ge[:], lg[:, 0:4], AF.Exp, bias=sm[:, 1:2], accum_out=sm[:, 2:3], r=[lg, sm], w=[ge, sm])
        V("reciprocal", sm[:, 3:4], sm[:, 2:3], r=[sm], w=[sm])
        V("tensor_tensor", tmp48[:], lg[:, 4:36].rearrange("p (g e) -> p g e", e=8), g1h[:].unsqueeze(2).broadcast_to([128, 4, 8]),
          ALU.mult, r=[lg, g1h], w=[tmp48])
        V("tensor_reduce", esel[:], tmp48[:].rearrange("p g e -> p e g"), axis=AX.X, op=ALU.add, r=[tmp48], w=[esel])
        V("tensor_reduce", sm[:, 4:5], esel[:], axis=AX.X, op=ALU.max, r=[esel], w=[sm])
        V("tensor_scalar", oh1[:], esel[:], sm[:, 4:5], None, ALU.is_equal, r=[esel, sm], w=[oh1])
        V("scalar_tensor_tensor", msk[:], oh1[:], NEG_BIG, esel[:], ALU.mult, ALU.add, r=[oh1, esel], w=[msk])
        V("tensor_reduce", sm[:, 5:6], msk[:], axis=AX.X, op=ALU.max, r=[msk], w=[sm])
        V("tensor_scalar", oh2[:], msk[:], sm[:, 5:6], None, ALU.is_equal, r=[msk, sm], w=[oh2])
        V("tensor_tensor", sm[:, 6:7], sm[:, 5:6], sm[:, 4:5], ALU.subtract, r=[sm], w=[sm])
        A("activation", sm[:, 6:7], sm[:, 6:7], AF.Exp, r=[sm], w=[sm])
        V("tensor_scalar", sm[:, 7:8], sm[:, 6:7], 1.0, None, ALU.add, r=[sm], w=[sm])
        V("reciprocal", sm[:, 7:8], sm[:, 7:8], r=[sm], w=[sm])
        V("tensor_tensor", sm[:, 8:9], sm[:, 6:7], sm[:, 7:8], ALU.mult, r=[sm], w=[sm])
        V("tensor_scalar", self.wts[:, ti, 0:1], sm[:, 7:8], sm[:, 3:4], None, ALU.mult, r=[sm], w=[self.wts])
        V("tensor_scalar", self.wts[:, ti, 1:2], sm[:, 8:9], sm[:, 3:4], None, ALU.mult, r=[sm, self.wts], w=[self.wts])
        for k, oh in ((0, oh1), (1, oh2)):
            V("tensor_tensor", self.Msel[:, ti, k, :].rearrange("p (g e) -> p g e", e=8), g1h[:].unsqueeze(2).broadcast_to([128, 4, 8]),
              oh[:].unsqueeze(1).broadcast_to([128, 4, 8]), ALU.mult, r=[g1h, oh, self.Msel], w=[self.Msel])


Builder.outproj = _outproj


def _moe(self, l, last):
    T, NT, NTC = self.T, self.NT, self.NTC
    NB = self.NB
    self.new_phase()
    psf, psb = self.psf, self.psb
    V, A, G, mm = self.V, self.A, self.G, self.mm
    mT = self.modT[l]
    Msel, wts = self.Msel, self.wts
    Mb = self.sb([128, NT, 32], BF16)
    V("tensor_tensor", Mb[:], Msel[:, :, 0, :], Msel[:, :, 1, :], ALU.add, r=[Msel], w=[Mb])
    for ti in range(NT):
        mm(psf[0][0:32, 0:1], Mb[:, ti, :], self.ones_b[:, 0:1], start=(ti == 0), stop=(ti == NT - 1), r=[Mb, self.ones_b], w=[psf[0]])
    cc = self.sb([32, 4], F32)
    V("tensor_scalar", cc[:, 0:1], psf[0][0:32, 0:1], 1.0 / 128, 0.49609375, ALU.mult, ALU.add, r=[psf[0]], w=[cc])
    nbi = self.sb([32, 1], I32)
    V("tensor_copy", nbi[:], cc[:, 0:1], r=[cc], w=[nbi])
    V("tensor_copy", cc[:, 1:2], nbi[:], r=[nbi, cc], w=[cc])
    V("tensor_scalar", cc[:, 2:3], cc[:, 1:2], 128.0, None, ALU.mult, r=[cc], w=[cc])
    pbc = self.sb([32, 128], F32)
    V("tensor_scalar", pbc[:], self.ones_f[0:32, :], cc[:, 2:3], None, ALU.mult, r=[self.ones_f, cc], w=[pbc])
    mm(psf[1][:, 0:32], pbc[:], self.sU_f[0:32, 0:32], r=[pbc, self.sU_f], w=[psf[1]])
    mm(psf[1][:, 32:64], pbc[:], self.ident_f[0:32, 0:32], start=False, r=[pbc, self.ident_f], w=[psf[1]], skip=True)
    stb = self.sb([128, 64], F32)
    V("tensor_copy", stb[:], psf[1][:, 0:64], r=[psf[1]], w=[stb])
    endb = self.sb([128, 32], F32)
    V("tensor_tensor", endb[:], stb[:, 0:32], stb[:, 32:64], ALU.add, r=[stb], w=[endb])
    tot = self.sb([128, 32], F32)
    V("memset", tot[:], 0.0, w=[tot])
    sr = self.sb([128, 32], F32)
    pr = self.sb([128, 32], F32)
    destf = self.sb([128, NT, 2], F32)
    for ti in range(NT):
        mm(psf[2][:, 0:32], self.sU_b[:], Mb[:, ti, :], r=[self.sU_b, Mb], w=[psf[2]])
        mm(psf[3][:, 0:32], self.ones_b[:], Mb[:, ti, :], r=[self.ones_b, Mb], w=[psf[3]])
        V("tensor_tensor", sr[:], psf[2][:, 0:32], tot[:], ALU.add, r=[psf[2], tot], w=[sr])
        V("tensor_tensor", sr[:], sr[:], stb[:, 0:32], ALU.add, r=[sr, stb], w=[sr])
        V("tensor_tensor", tot[:], tot[:], psf[3][:, 0:32], ALU.add, r=[tot, psf[3]], w=[tot])
        for k in range(2):
            V("tensor_tensor", pr[:], Msel[:, ti, k, :], sr[:], ALU.mult, r=[Msel, sr], w=[pr])
            V("tensor_reduce", destf[:, ti, k:k + 1], pr[:], axis=AX.X, op=ALU.add, r=[pr, destf], w=[destf])
    desti = self.sb([128, NT, 2], I32)
    V("tensor_copy", desti[:], destf[:], r=[destf], w=[desti])
    bst = self.sb([128, NB], F32)
    G("iota", bst[:], [[128, NB]], base=0, channel_multiplier=0, allow_small_or_imprecise_dtypes=True, w=[bst])
    cmp = self.sb([128, NB, 32], F32)
    V("tensor_tensor", cmp[:], endb[:].unsqueeze(1).broadcast_to([128, NB, 32]), bst[:].unsqueeze(2).broadcast_to([128, NB, 32]),
      ALU.is_le, r=[endb, bst], w=[cmp])
    bex = self.sb([128, NB], F32)
    V("tensor_reduce", bex[:], cmp[:], axis=AX.X, op=ALU.add, r=[cmp], w=[bex])
    V("tensor_scalar", bex[:], bex[:], float(NEXP - 1), float(l * NEXP), ALU.min, ALU.add, r=[bex], w=[bex])
    V("tensor_scalar", bex[:], bex[:], 128.0, self.pidx[:, 0:1], ALU.mult, ALU.add, r=[bex, self.pidx], w=[bex])
    widx = self.sb([128, NB], I32)
    V("tensor_copy", widx[:], bex[:], r=[bex], w=[widx])
    h2ts = Rot([self.sb([128, D], BF16) for _ in range(2)])
    for ti in range(NT):
        h2t = h2ts.next()
        self.dma("sp", h2t[:], self.H2[ti * 128:(ti + 1) * 128, :], r=["H2"], w=[h2t])
        for k in range(2):
            idx = desti[:, ti, k:k + 1]
            o = self.P.op("pool", lambda g, idx=idx, h2t=h2t: g.indirect_dma_start(
                out=self.XB[:, :], out_offset=bass.IndirectOffsetOnAxis(ap=idx, axis=0), in_=h2t[:], in_offset=None),
                reads=[desti, h2t, "XB"], writes=["XB"])
            o.is_dma = True
    wts6 = [Rot([self.sb([128, 2048], BF16) for _ in range(2)]) for _ in range(6)]
    xbs = Rot([self.sb([128, D], BF16) for _ in range(2)])
    xTs = Rot([self.sb([128, KT, 128], BF16) for _ in range(2)])
    sg = self.sb([128, 512], F32)
    hms = Rot([self.sb([128, 512], BF16) for _ in range(2)])
    hTs = Rot([self.sb([128, 4, 128], BF16) for _ in range(2)])
    ybs = Rot([self.sb([128, D], F32) for _ in range(2)])
    for b in range(NB):
        w6 = []
        for j in range(6):
            wt = wts6[j].next()
            idx = widx[:, b:b + 1]
            o = self.P.op("pool", lambda g, idx=idx, wt=wt, j=j: g.indirect_dma_start(
                out=wt[:], out_offset=None, in_=self.wexp[j][:, :], in_offset=bass.IndirectOffsetOnAxis(ap=idx, axis=0)),
                reads=[widx], writes=[wt])
            o.is_dma = True
            w6.append(wt)
        xb = xbs.next()
        self.dma("sp", xb[:], self.XB[b * 128:(b + 1) * 128, :], r=["XB"], w=[xb])
        for kt in range(KT):
            self.tr(psb[0][:, kt * 128:(kt + 1) * 128], xb[:, kt * 128:(kt + 1) * 128], self.ident_b[:], r=[xb, self.ident_b], w=[psb[0]])
        xT = xTs.next()
        A("copy", xT[:, 0:4, :].rearrange("p a b -> p (a b)"), psb[0][:, 0:512], r=[psb[0]], w=[xT])
        V("tensor_copy", xT[:, 4:8, :].rearrange("p a b -> p (a b)"), psb[0][:, 512:1024], r=[psb[0]], w=[xT])
        for gi in range(2):
            for kt in range(KT):
                wsrc = w6[gi * 2 + kt // 4]
                c0 = (kt % 4) * 512
                mm(psf[gi][:, :], xT[:, kt, :], wsrc[:, c0:c0 + 512], start=(kt == 0), stop=(kt == KT - 1), r=[xT, wsrc], w=[psf[gi]])
        A("activation", sg[:], psf[0][:, :], AF.Silu, r=[psf[0]], w=[sg])
        hm = hms.next()
        V("tensor_tensor", hm[:], sg[:], psf[1][:, :], ALU.mult, r=[sg, psf[1]], w=[hm])
        for m in range(4):
            self.tr(psb[1][:, m * 128:(m + 1) * 128], hm[:, m * 128:(m + 1) * 128], self.ident_b[:], r=[hm, self.ident_b], w=[psb[1]])
        hT = hTs.next()
        A("copy", hT[:].rearrange("p a b -> p (a b)"), psb[1][:, 0:512], r=[psb[1]], w=[hT])
        for half in range(2):
            for m in range(4):
                wsrc = w6[4 + m // 2]
                c0 = (m % 2) * 1024 + half * 512
                mm(psf[2 + half][:, :], hT[:, m, :], wsrc[:, c0:c0 + 512], start=(m == 0), stop=(m == 3), r=[hT, wsrc], w=[psf[2 + half]])
        yb = ybs.next()
        A("copy", yb[:, 0:512], psf[2][:, :], r=[psf[2]], w=[yb])
        V("tensor_copy", yb[:, 512:1024], psf[3][:, :], r=[psf[3]], w=[yb])
        self.dma("sp", self.YB[b * 128:(b + 1) * 128, :], yb[:], r=[yb], w=["YB"])
    g5 = []
    for r in range(2):
        t = self.sb([128, D], F32); self.bcast_row(self.mod_col(l, 5, r), t, r=[mT]); g5.append(t)
    lng = self.sb([128, D], F32)
    lnb = self.sb([128, D], F32)
    self.dma("sp", lng[:], self.ln_g[l * 2 + 1:l * 2 + 2, :].partition_broadcast(128), w=[lng])
    self.dma("sp", lnb[:], self.ln_b[l * 2 + 1:l * 2 + 2, :].partition_broadcast(128), w=[lnb])
    y0s = Rot([self.sb([128, D], F32) for _ in range(2)])
    y1s = Rot([self.sb([128, D], F32) for _ in range(2)])
    xts = Rot([self.sb([128, D], F32) for _ in range(2)])
    xns = Rot([self.sb([128, D], F32) for _ in range(2)])
    junk = self.sb([128, D], F32)
    st = self.sb([128, 2], F32)
    for ti in range(NT):
        if last and ti < NTC:
            continue
        r = 1 if ti < NTC else 0
        ys = []
        for k, rot in ((0, y0s), (1, y1s)):
            yk = rot.next()
            idx = desti[:, ti, k:k + 1]
            o = self.P.op("pool", lambda g, idx=idx, yk=yk: g.indirect_dma_start(
                out=yk[:], out_offset=None, in_=self.YB[:, :], in_offset=bass.IndirectOffsetOnAxis(ap=idx, axis=0)),
                reads=[desti, "YB"], writes=[yk])
            o.is_dma = True
            ys.append(yk)
        xt = xts.next()
        self.dma("sp", xt[:], self.X[ti * 128:(ti + 1) * 128, :], r=["X"], w=[xt])
        y0, y1 = ys
        V("tensor_scalar", y0[:], y0[:], wts[:, ti, 0:1], None, ALU.mult, r=[y0, wts], w=[y0])
        V("scalar_tensor_tensor", y0[:], y1[:], wts[:, ti, 1:2], y0[:], ALU.mult, ALU.add, r=[y1, wts, y0], w=[y0])
        G("tensor_tensor", y0[:], y0[:], g5[r][:], ALU.mult, r=[y0, g5[r]], w=[y0])
        V("scalar_tensor_tensor", y0[:], xt[:], ALPHA, y0[:], ALU.mult, ALU.add, r=[xt, y0], w=[y0])
        xn = xns.next()
        self.ln_tile(y0, st, junk, lng, lnb, xn)
        if last:
            lt = ti - NTC
            self.dma("sp", self.yout[lt * 128:(lt + 1) * 128, :], xn[:], r=[xn], w=["yout"])
        else:
            self.dma("sp", self.X[ti * 128:(ti + 1) * 128, :], xn[:], r=[xn, "X"], w=["X"])


Builder.moe = _moe


def _proj_odd(self, l):
    i = l // 2
    NT, NTC = self.NT, self.NTC
    self.new_phase()
    psf, psb = self.psf, self.psb
    V, A, G, mm = self.V, self.A, self.G, self.mm
    wb = self.sb([128, KT, ODD_IN], BF16)
    self.load_weight_bf16(wb, self.od_w_in[i], ODD_IN)
    s1p = self.sb([128, 8, 2], F32)
    V("tensor_scalar", s1p[:], self.modT[l][:, 8:16, :], 1.0, None, ALU.add, r=[self.modT[l]], w=[s1p])
    sh = lambda kt, r: self.modT[l][:, kt, r:r + 1]
    qkg = self.sb([128, 128], F32)
    self.dma("sp", qkg[:], self.od_qk_g[i:i + 1, :].partition_broadcast(128), w=[qkg])
    V("memset", self.nmax[:], 0.0, w=[self.nmax])
    xts = Rot([self.sb([128, D], F32) for _ in range(2)])
    hTs = Rot([self.sb([128, KT, 128], BF16) for _ in range(2)])
    sqt = self.sb([128, 8, 64], F32)
    xn = self.sb([128, 8 * 64], F32)
    o = self.sb([128, 8, 64], F32)
    ta, tb = self.sb([128, 8, 32], F32), self.sb([128, 8, 32], F32)
    ss = self.sb([128, 8], F32)
    nrm = self.sb([128, 8], F32)
    obs = Rot([self.sb([128, 512], BF16) for _ in range(2)])
    stg = Rot([self.sb([128, 4, 128], BF16) for _ in range(2)])
    vbs = Rot([self.sb([128, 512], BF16) for _ in range(2)])
    og = self.sb([128, 512], F32)
    stg2 = self.sb([128, 4, 128], BF16)
    xsrc = self.x_src(l)

    def norm_rope(ps, c0, Gn, gcol, is_ctx, lt, nm_off, scale):
        p3 = ps[:, c0:c0 + Gn * 64].rearrange("p (g d) -> p g d", d=64)
        A("activation", sqt[:, 0:Gn, :], p3, AF.Square, r=[ps], w=[sqt])
        V("tensor_reduce", ss[:, 0:Gn], sqt[:, 0:Gn, :], axis=AX.X, op=ALU.add, r=[sqt], w=[ss])
        V("tensor_scalar", ss[:, 0:Gn], ss[:, 0:Gn], 1.0 / 64, EPS, ALU.mult, ALU.add, r=[ss], w=[ss])
        A("activation", ss[:, 0:Gn], ss[:, 0:Gn], AF.Sqrt, r=[ss], w=[ss])
        V("reciprocal", ss[:, 0:Gn], ss[:, 0:Gn], r=[ss], w=[ss])
        x3 = xn[:, 0:Gn * 64].rearrange("p (g d) -> p g d", d=64)
        V("tensor_tensor", x3, p3, ss[:, 0:Gn].unsqueeze(2).broadcast_to([128, Gn, 64]), ALU.mult, r=[ps, ss], w=[xn])
        G("tensor_tensor", x3, x3, qkg[:, gcol:gcol + 64].unsqueeze(1).broadcast_to([128, Gn, 64]), ALU.mult, r=[xn, qkg], w=[xn])
        if is_ctx:
            V("tensor_copy", o[:, 0:Gn, :], x3, r=[xn], w=[o])
        else:
            self.rope(xn, o[:, 0:Gn, :], lt, (ta, tb))
        V("tensor_tensor", sqt[:, 0:Gn, :], o[:, 0:Gn, :], o[:, 0:Gn, :], ALU.mult, r=[o], w=[sqt])
        V("tensor_reduce", nrm[:, 0:Gn], sqt[:, 0:Gn, :], axis=AX.X, op=ALU.add, r=[sqt], w=[nrm])
        V("tensor_tensor", self.nmax[:, nm_off:nm_off + Gn], self.nmax[:, nm_off:nm_off + Gn], nrm[:, 0:Gn], ALU.max,
          r=[nrm, self.nmax], w=[self.nmax])
        ob = obs.next()
        A("activation", ob[:, 0:Gn * 64], o[:, 0:Gn, :].rearrange("p g d -> p (g d)"), AF.Copy, scale=scale, r=[o], w=[ob])
        return ob

    for ti in range(NT):
        t0 = ti * 128
        is_ctx = ti < NTC
        xt = xts.next()
        self.dma("sp", xt[:], xsrc[t0:t0 + 128, :], r=["X"], w=[xt])
        hT = hTs.next()
        self.make_hT(l, ti, xt, hT, s1p, sh, [psf[0], psf[1]])

        def tokmajor(ps, c0, n):
            for kt in range(KT):
                mm(ps[:, 0:n], hT[:, kt, :], wb[:, kt, c0:c0 + n], start=(kt == 0), stop=(kt == KT - 1), r=[hT, wb], w=[ps])
        tokmajor(psf[2], 512, 512)
        vb = vbs.next()
        A("copy", vb[:], psf[2][:, :], r=[psf[2]], w=[vb])
        self.dma("sp", self.vB[t0:t0 + 128, :], vb[:], r=[vb], w=["vB"])
        tokmajor(psf[3], 1024, 512)
        A("activation", og[:], psf[3][:, :], AF.Silu, r=[psf[3]], w=[og])
        self.dma("sp", self.ogB[t0:t0 + 128, :], og[:], r=[og], w=["ogB"])
        tokmajor(psf[4], 1536, 512)
        ob = norm_rope(psf[4], 0, 8, 0, is_ctx, ti - NTC, 0, 0.125)
        for pr in range(4):
            self.tr(psb[0][:, pr * 128:(pr + 1) * 128], ob[:, pr * 128:(pr + 1) * 128], self.ident_b[:], r=[ob, self.ident_b], w=[psb[0]])
        st = stg.next()
        V("tensor_copy", st[:].rearrange("p a b -> p (a b)"), psb[0][:, 0:512], r=[psb[0]], w=[st])
        self.dma("sp", self.qTA.rearrange("h p t -> p h t")[:, :, t0:t0 + 128], st[:], r=[st], w=["qTA"])
        tokmajor(psf[5], 2048, 256)
        ob = norm_rope(psf[5], 0, 2, 64, is_ctx, ti - NTC, 8, 1.0)
        self.tr(psb[1][:, 0:128], ob[:, 0:128], self.ident_b[:], r=[ob, self.ident_b], w=[psb[1]])
        st = stg.next()
        V("tensor_copy", st[:, 0, :], psb[1][:, 0:128], r=[psb[1]], w=[st])
        self.dma("sp", self.kTA[0][:, t0:t0 + 128], st[:, 0, :], r=[st], w=["kTA"])
        vb = vbs.next()
        A("copy", vb[:, 0:128], psf[5][:, 128:256], r=[psf[5]], w=[vb])
        self.dma("sp", self.vA[t0:t0 + 128, 0:128], vb[:, 0:128], r=[vb], w=["vA"])
        tokmajor(psf[2], 0, 512)
        rb_ = obs.next()
        A("copy", rb_[:], psf[2][:, :], r=[psf[2]], w=[rb_])
        for cc in range(4):
            self.tr(psb[0][:, cc * 128:(cc + 1) * 128], rb_[:, cc * 128:(cc + 1) * 128], self.ident_b[:], r=[rb_, self.ident_b], w=[psb[0]])
        V("tensor_copy", stg2[:].rearrange("p a b -> p (a b)"), psb[0][:, 0:512], r=[psb[0]], w=[stg2])
        self.dma("sp", self.qkraw.rearrange("c p t -> p c t")[:, 0:4, t0:t0 + 128], stg2[:], r=[stg2], w=["qkraw"])


Builder.proj_odd = _proj_odd


def _retention(self, l):
    i = l // 2
    T, NT, NTC = self.T, self.NT, self.NTC
    self.new_phase()
    psf, psb = self.psf, self.psb
    V, A, G, mm = self.V, self.A, self.G, self.mm
    qT = self.sb([64, 4, T], BF16)
    kT = self.sb([64, 4, T], BF16)
    for h in range(4):
        self.dma("sp", qT[:, h, :], self.qkraw[h // 2][(h % 2) * 64:(h % 2 + 1) * 64, :], r=["qkraw"], w=[qT])
        self.dma("sp", kT[:, h, :], self.qkraw[2 + h // 2][(h % 2) * 64:(h % 2 + 1) * 64, :], r=["qkraw"], w=[kT])
    vp = self.sb([128, NT, 512], BF16)
    self.dma("sp", vp[:], self.vB.rearrange("(n p) f -> p n f", p=128), r=["vB"], w=[vp])
    ld = self.sb([128, 8], F32)
    self.dma("sp", ld[:], self.od_decay[i:i + 1, :].partition_broadcast(128), w=[ld])
    A("activation", ld[:], ld[:], AF.Exp, scale=-1.0, r=[ld], w=[ld])
    A("activation", ld[:], ld[:], AF.Ln, bias=1.0, r=[ld], w=[ld])
    V("tensor_scalar", ld[:], ld[:], -1.0, None, ALU.mult, r=[ld], w=[ld])
    lagp = self.sb([128, 128], F32)
    lagn = self.sb([128, 128], F32)
    V("tensor_scalar", lagp[:], self.iot[:], 0.0, None, ALU.max, r=[self.iot], w=[lagp])
    V("tensor_scalar", lagn[:], self.iot[:], -1.0, 0.0, ALU.mult, ALU.max, r=[self.iot], w=[lagn])
    rowf = self.sb([128, 128], F32)
    rowb = self.sb([128, 128], F32)
    G("iota", rowf[:], [[1, 128]], base=1, channel_multiplier=0, allow_small_or_imprecise_dtypes=True, w=[rowf])
    G("iota", rowb[:], [[-1, 128]], base=128, channel_multiplier=0, allow_small_or_imprecise_dtypes=True, w=[rowb])
    colf = self.sb([128, 1], F32)
    V("tensor_scalar", colf[:], self.pidx[:], -1.0, 127.0, ALU.mult, ALU.add, r=[self.pidx], w=[colf])
    Dm = self.sb([128, 2, 4, 128], F32)
    qd = self.sb([64, 2, 4, 128], F32)
    kd = self.sb([128, 2, 4], F32)
    cd = self.sb([128, 2, 4], F32)
    for d in range(2):
        for h in range(4):
            c = ld[:, d * 4 + h:d * 4 + h + 1]
            A("activation", Dm[:, d, h, :], (lagp if d == 0 else lagn)[:], AF.Exp, scale=c, r=[lagp, lagn, ld], w=[Dm])
            V("scalar_tensor_tensor", Dm[:, d, h, :], Dm[:, d, h, :], 0.125, (self.triU_f if d == 0 else self.triL_f)[:], ALU.mult, ALU.mult,
              r=[Dm, self.triU_f, self.triL_f], w=[Dm])
            A("activation", qd[:, d, h, :], (rowf if d == 0 else rowb)[0:64, :], AF.Exp, scale=ld[0:64, d * 4 + h:d * 4 + h + 1],
              r=[rowf, rowb, ld], w=[qd])
            A("activation", kd[:, d, h:h + 1], (colf if d == 0 else self.pidx)[:], AF.Exp, scale=c, r=[colf, self.pidx, ld], w=[kd])
    V("tensor_scalar", kd[:], kd[:], 0.125, None, ALU.mult, r=[kd], w=[kd])
    A("activation", cd[:].rearrange("p a b -> p (a b)"), ld[:], AF.Exp, scale=128.0, r=[ld], w=[cd])
    S32 = self.sb([64, 4, 128], F32)
    Sb = self.sb([64, 4, 128], BF16)
    At = self.sb([128, 4, 128], BF16)
    qeb = self.sb([64, 4, 128], BF16)
    kw = self.sb([128, 4, 64], BF16)
    hout = self.sb([128, 4, 128], F32)
    hbs = Rot([self.sb([128, 512], F32) for _ in range(2)])
    ogs = Rot([self.sb([128, 512], F32) for _ in range(2)])
    st4 = self.sb([128, 4], F32)
    sq = self.sb([128, 4, 128], F32)
    catb = Rot([self.sb([128, 512], BF16) for _ in range(2)])
    KQ, OP, UP, KTp = psf[0], psf[1], psf[2], psb[0]
    for direction in ("bwd", "fwd"):
        fwd = direction == "fwd"
        d = 0 if fwd else 1
        V("memset", S32[:], 0.0, w=[S32])
        V("memset", Sb[:], 0.0, w=[Sb])
        order = list(range(NT)) if fwd else (list(range(NTC - 1, -1, -1)) + list(range(NT - 1, NTC - 1, -1)))
        for c in order:
            tok0, tok1 = c * 128, (c + 1) * 128
            for h in range(4):
                mm(KQ[:, h * 128:(h + 1) * 128], kT[:, h, tok0:tok1], qT[:, h, tok0:tok1], r=[kT, qT], w=[KQ])
            V("tensor_tensor", At[:], KQ[:, :].rearrange("p (h t) -> p h t", t=128), Dm[:, d, :, :], ALU.mult, r=[KQ, Dm], w=[At])
            G("tensor_tensor", qeb[:], qT[:, :, tok0:tok1], qd[:, d, :, :], ALU.mult, r=[qT, qd], w=[qeb])
            for h in range(4):
                self.tr(KTp[:, h * 64:(h + 1) * 64], kT[:, h, tok0:tok1], self.ident_b[0:64, 0:64], r=[kT, self.ident_b], w=[KTp])
            V("tensor_tensor", kw[:], KTp[:, 0:256].rearrange("p (h d) -> p h d", d=64), kd[:, d, :].unsqueeze(2).broadcast_to([128, 4, 64]),
              ALU.mult, r=[KTp, kd], w=[kw])
            for h in range(4):
                o_ = OP[:, h * 128:(h + 1) * 128]
                mm(o_, At[:, h, :], vp[:, c, h * 128:(h + 1) * 128], start=(h == 0), stop=False, r=[At, vp], w=[OP], skip=True)
                mm(o_, qeb[:, h, :], Sb[:, h, :], start=False, stop=True, r=[qeb, Sb], w=[OP], skip=True)
            hflat = hout[:].rearrange("p h d -> p (h d)")
            if not fwd:
                A("copy", hflat, OP[:, :], r=[OP], w=[hout])
                self.dma("sp", self.hB[tok0:tok1, :], hflat, r=[hout], w=["hB"])
            else:
                hb, og = hbs.next(), ogs.next()
                self.dma("sp", hb[:], self.hB[tok0:tok1, :], r=["hB"], w=[hb])
                self.dma("sp", og[:], self.ogB[tok0:tok1, :], r=["ogB"], w=[og])
                V("tensor_tensor", hflat, OP[:, :], hb[:], ALU.add, r=[OP, hb], w=[hout])
                G("tensor_tensor", sq[:], hout[:], hout[:], ALU.mult, r=[hout], w=[sq])
                V("tensor_reduce", st4[:], sq[:], axis=AX.X, op=ALU.add, r=[sq], w=[st4])
                V("tensor_scalar", st4[:], st4[:], 1.0 / 128, EPS, ALU.mult, ALU.add, r=[st4], w=[st4])
                A("activation", st4[:], st4[:], AF.Sqrt, r=[st4], w=[st4])
                V("reciprocal", st4[:], st4[:], r=[st4], w=[st4])
                V("tensor_tensor", hout[:], hout[:], st4[:].unsqueeze(2).broadcast_to([128, 4, 128]), ALU.mult, r=[hout, st4], w=[hout])
                cbt = catb.next()
                V("tensor_tensor", cbt[:], hflat, og[:], ALU.mult, r=[hout, og], w=[cbt])
                self.dma("sp", self.cat[tok0:tok1, 0:512], cbt[:], r=[cbt], w=["cat"])
            for h in range(4):
                mm(UP[0:64, h * 128:(h + 1) * 128], kw[:, h, :], vp[:, c, h * 128:(h + 1) * 128], start=(h == 0), stop=True,
                   r=[kw, vp], w=[UP], skip=True)
            for h in range(4):
                V("scalar_tensor_tensor", S32[:, h, :], S32[:, h, :], cd[0:64, d, h:h + 1], UP[0:64, h * 128:(h + 1) * 128], ALU.mult, ALU.add,
                  r=[S32, cd, UP], w=[S32])
            A("copy", Sb[:], S32[:], r=[S32], w=[Sb])


Builder.retention = _retention


def _gqa(self, l):
    T, NT, NTC = self.T, self.NT, self.NTC
    self.new_phase()
    psf = self.psf
    V, A, G, mm = self.V, self.A, self.G, self.mm
    self.tr(psf[0][0:16, 0:128], self.nmax[:, 0:16], self.ident_f[:], r=[self.nmax, self.ident_f], w=[psf[0]])
    mx = self.sb([16, 1], F32)
    V("tensor_reduce", mx[:], psf[0][0:16, 0:128], axis=AX.X, op=ALU.max, r=[psf[0]], w=[mx])
    dg = self.sb([16, 16], F32)
    V("tensor_scalar", dg[:], self.ident_f[0:16, 0:16], mx[:, 0:1], None, ALU.mult, r=[mx, self.ident_f], w=[dg])
    mm(psf[1][:, 0:16], self.ones_f[0:16, 0:128], dg[:], r=[self.ones_f, dg], w=[psf[1]])
    mxb = self.sb([128, 16], F32)
    V("tensor_copy", mxb[:], psf[1][:, 0:16], r=[psf[1]], w=[mxb])
    negm = self.sb([128, 8], F32)
    for kv in range(2):
        V("tensor_scalar", negm[:, kv * 4:(kv + 1) * 4], mxb[:, kv * 4:(kv + 1) * 4], mxb[:, 8 + kv:9 + kv], None, ALU.mult, r=[mxb], w=[negm])
    A("activation", negm[:], negm[:], AF.Sqrt, r=[negm], w=[negm])
    V("tensor_scalar", negm[:], negm[:], -0.125, None, ALU.mult, r=[negm], w=[negm])
    kT = self.sb([128, T], BF16)
    self.dma("sp", kT[:], self.kTA[0], r=["kTA"], w=[kT])
    vh = self.sb([128, NT, 2, 65], BF16)
    V("memset", vh[:, :, :, 64:65], 1.0, w=[vh])
    for n in range(NT):
        self.dma("sp", vh[:, n, :, 0:64], self.vA[n * 128:(n + 1) * 128, 0:128].rearrange("p (k d) -> p k d", d=64), r=["vA"], w=[vh])
    qTs = Rot([self.sb([128, T], BF16) for _ in range(2)])
    pTs = Rot([self.sb([128, 512], BF16) for _ in range(3)])
    st_rot = Rot([psf[4], psf[5]])
    srow = self.sb([65, 512], F32)
    rbs = Rot([self.sb([64, 512], F32) for _ in range(2)])
    outb = Rot([self.sb([64, 512], BF16) for _ in range(2)])
    bank = 0
    for j in range(8):
        kv = j // 4
        qT = qTs.next()
        self.dma("sp", qT[kv * 64:(kv + 1) * 64, :], self.qTA[j // 2][(j % 2) * 64:(j % 2 + 1) * 64, :], r=["qTA"], w=[qT])
        jobs = []
        for (q0, nq, ktiles) in self.qblocks():
            o_ps = psf[bank]
            b_ps = psf[2 + bank]
            bank = 1 - bank

            def post(j=j, q0=q0, nq=nq, o_ps=o_ps, b_ps=b_ps):
                A("copy", srow[64:65, 0:nq], o_ps[64:65, 0:nq], r=[o_ps], w=[srow])
                mm(b_ps[0:64, 0:nq], self.ones_f[64:65, 0:64], srow[64:65, 0:nq], r=[self.ones_f, srow], w=[b_ps])
                rb = rbs.next()
                V("reciprocal", rb[:, 0:nq], b_ps[0:64, 0:nq], r=[b_ps], w=[rb])
                ob = outb.next()
                V("tensor_tensor", ob[:, 0:nq], o_ps[0:64, 0:nq], rb[:, 0:nq], ALU.mult, r=[o_ps, rb], w=[ob])
                self.dma("sp", self.catT[4 + j // 2][(j % 2) * 64:(j % 2 + 1) * 64, q0:q0 + nq], ob[:, 0:nq], r=[ob], w=["catT"])

            jobs.append(dict(kT=kT, qT=qT, krow=(kv * 64, (kv + 1) * 64), v_fn=(lambda kt, kv=kv: vh[:, kt, kv, :]), vdep=vh, dv=65,
                             q0=q0, nq=nq, ktiles=ktiles, negm_col=negm[:, j:j + 1], ndep=negm, o_ps=o_ps, s_ps=None, sum_mode="col", post=post))
        self.run_attn_jobs(jobs, pTs, st_rot)


Builder.gqa = _gqa


def build_program(TC, TL, debug=()):
    B = Builder(TC, TL, debug=debug)
    B.declare_io()
    B.setup_persistent()
    B.phase_mods()
    for l in range(DEPTH):
        i = l // 2
        last = l == DEPTH - 1
        if l % 2 == 0:
            B.proj_even(l)
            B.attnA(l)
            B.mlstm(l)
            B.outproj(l, B.ev_w_out[i])
        else:
            B.proj_odd(l)
            B.retention(l)
            B.gqa(l)
            B.outproj(l, B.od_w_out[i])
        B.moe(l, last)
    B.new_phase()
    B.P.final_wait(list(B.outputs.keys()))
    B.P.emit()
    return B


def kernel(**inputs):
    inp = {k: np.asarray(v) for k, v in inputs.items()}
    BATCH, TL, _ = inp["x"].shape
    TC = inp["ctx"].shape[1]
    n_cores = 8
    B = build_program(TC, TL)
    sh = prep_shared(inp, TL)
    in_maps = []
    for core in range(n_cores):
        b = core % BATCH
        m = dict(sh)
        m.update(prep_core(inp, b))
        in_maps.append({k: v for k, v in m.items() if k in B.inputs})
    res = run_bass_kernel_spmd(B.nc, in_maps, core_ids=list(range(n_cores)))
    out = np.stack([np.asarray(res.results[b]["yout"], dtype=np.float32) for b in range(BATCH)], 0)
    return out
```

```python
import math
from contextlib import ExitStack
import numpy as np
import concourse.bass as bass
import concourse.mybir as mybir
from concourse.bass_utils import run_bass_kernel_spmd

F32 = mybir.dt.float32
BF16 = mybir.dt.bfloat16
I32 = mybir.dt.int32
AF = mybir.ActivationFunctionType
ALU = mybir.AluOpType
AX = mybir.AxisListType

D = 1024
KT = 8
DEPTH = 4
EPS = 1e-5
ALPHA = (2 * DEPTH) ** 0.25
EVEN_IN = 3600
ODD_IN = 2304
NEXP = 32
NEG_BIG = -1.0e30


def _k(r):
    if isinstance(r, (str, tuple, int)):
        return r
    return r.name


class Op:
    __slots__ = ("eng", "fn", "reads", "writes", "is_dma", "deps", "needs_inc", "tok", "seq", "dsem", "barrier")

    def __init__(self, eng, fn, reads, writes, is_dma):
        self.eng = eng
        self.fn = fn
        self.reads = reads
        self.writes = writes
        self.is_dma = is_dma
        self.deps = []
        self.needs_inc = False
        self.tok = None
        self.seq = -1
        self.dsem = -1
        self.barrier = False


class Prog:
    COMPUTE = ("pe", "act", "dve", "pool")
    ALL = ("pe", "act", "dve", "pool", "sp")

    def __init__(self, nc, n_dma_sems=20):
        self.nc = nc
        self.ops = []
        self.engs = {"pe": nc.tensor, "act": nc.scalar, "dve": nc.vector, "pool": nc.gpsimd, "sp": nc.sync}
        self.n_dma_sems = n_dma_sems
        self._n = 0

    def op(self, eng, fn, reads=(), writes=()):
        o = Op(eng, fn, tuple(_k(r) for r in reads), tuple(_k(w) for w in writes), False)
        self.ops.append(o)
        return o

    def dma(self, q, out, in_, reads=(), writes=(), **kw):
        o = Op(q, lambda e: e.dma_start(out=out, in_=in_, **kw), tuple(_k(r) for r in reads),
               tuple(_k(w) for w in writes), True)
        self.ops.append(o)
        return o

    def barrier(self):
        for e in self.ALL:
            o = Op(e, lambda en: en.nop(), (), (), False)
            o.barrier = True
            self.ops.append(o)

    def emit(self):
        nc = self.nc
        state = {}
        eng_seq = {e: 0 for e in self.engs}
        last_op = {e: None for e in self.COMPUTE}
        waited_c = {e: {p: -1 for p in self.COMPUTE} for e in self.engs}
        waited_d = {e: set() for e in self.engs}
        dma_q_count = {"sp": 0, "pool": 0, "act": 0}
        dma_last_on_sem = {}
        nbar = 0
        for o in self.ops:
            o.seq = eng_seq[o.eng]
            eng_seq[o.eng] += 1
            deps = []
            if o.barrier:
                for p in self.COMPUTE:
                    if last_op[p] is not None and p != o.eng:
                        deps.append(last_op[p])
                    elif last_op[p] is not None and p == o.eng and p != "pe":
                        deps.append(last_op[p])
                deps.extend(dma_last_on_sem.values())
                nbar += 1
                if nbar % len(self.ALL) == 0:
                    state = {}
            else:
                for r in o.reads:
                    st = state.get(r)
                    if st is not None and st[0] is not None:
                        deps.append(st[0])
                for w in o.writes:
                    st = state.get(w)
                    if st is not None:
                        if st[0] is not None:
                            deps.append(st[0])
                        deps.extend(st[1])
            if o.is_dma:
                k = dma_q_count[o.eng]
                dma_q_count[o.eng] += 1
                o.dsem = (o.eng, k % self.n_dma_sems)
                prev = dma_last_on_sem.get(o.dsem)
                if prev is not None:
                    deps.append(prev)
                dma_last_on_sem[o.dsem] = o
            final = []
            for d in deps:
                if d is o:
                    continue
                if d.is_dma:
                    if d in waited_d[o.eng]:
                        continue
                    waited_d[o.eng].add(d)
                    final.append(d)
                else:
                    if d.eng == "pe" and o.eng == "pe" and not o.is_dma:
                        continue
                    if waited_c[o.eng][d.eng] >= d.seq:
                        continue
                    waited_c[o.eng][d.eng] = d.seq
                    final.append(d)
            best = {}
            dm = []
            for d in final:
                if d.is_dma:
                    dm.append(d)
                elif d.eng not in best or best[d.eng].seq < d.seq:
                    best[d.eng] = d
            o.deps = dm + list(best.values())
            for d in o.deps:
                d.needs_inc = True
            if not o.barrier:
                for r in o.reads:
                    st = state.setdefault(r, [None, []])
                    st[1].append(o)
                for w in o.writes:
                    state[w] = [o, []]
            if not o.is_dma and o.eng in last_op and not o.barrier:
                last_op[o.eng] = o
        self._sem_ctx = []

        def mk(name):
            cm = nc.semaphore(name)
            s = cm.__enter__()
            self._sem_ctx.append(cm)
            return s

        sems = {e: mk(f"s_{e}") for e in self.COMPUTE}
        dsems = {}
        for q in ("sp", "pool", "act"):
            for i in range(min(self.n_dma_sems, dma_q_count[q])):
                dsems[(q, i)] = mk(f"d_{q}{i}")
        cnt = {e: 0 for e in self.COMPUTE}
        dcnt = {}
        n_wait = 0
        for o in self.ops:
            e = self.engs[o.eng]
            for d in o.deps:
                s, v = d.tok
                e.wait_ge(s, v)
                n_wait += 1
            inst = o.fn(e)
            if o.is_dma:
                s = dsems[o.dsem]
                dcnt[o.dsem] = dcnt.get(o.dsem, 0) + 16
                inst.then_inc(s, 16)
                o.tok = (s, dcnt[o.dsem])
            elif o.needs_inc:
                cnt[o.eng] += 1
                inst.then_inc(sems[o.eng], 1)
                o.tok = (sems[o.eng], cnt[o.eng])
        self.stats = dict(n_ops=len(self.ops), n_wait=n_wait, cnt=dict(cnt))
        return self.stats

    def final_wait(self, resources):
        self.op("sp", lambda e: e.nop(), reads=tuple(resources))


class Rot:
    def __init__(self, tiles):
        self.tiles = tiles
        self.i = 0

    def next(self):
        t = self.tiles[self.i % len(self.tiles)]
        self.i += 1
        return t


class Builder:
    def __init__(self, TC, TL, debug=()):
        self.TC, self.TL = TC, TL
        self.T = TC + TL
        self.NT = self.T // 128
        self.NTC = TC // 128
        self.debug = set(debug)
        nc = self.nc = bass.Bass("TRN2", target_bir_lowering=False)
        self.P = Prog(nc)
        arena_bytes = 196608
        ar = nc.alloc_sbuf_tensor("arena", [128, arena_bytes], mybir.dt.uint8)
        self.abase = nc.lookup_mloc(ar).addr
        self.aend = self.abase + arena_bytes
        self.ptop = self.abase
        self.top = self.abase
        self._n = 0
        self.inputs = {}
        self.outputs = {}
        self.psf = [nc.alloc_psum_tensor(f"psf{i}", [128, 512], F32) for i in range(6)]
        self.psb = [nc.alloc_psum_tensor(f"psb{i}", [128, 1024], BF16) for i in range(2)]

    def sb(self, shape, dt, persistent=False, name=None):
        self._n += 1
        esz = {F32: 4, BF16: 2, I32: 4}[dt]
        nbytes = int(np.prod(shape[1:])) * esz
        nbytes = (nbytes + 63) // 64 * 64
        if persistent:
            assert self.top == self.ptop, "persistent alloc only at phase boundary"
            off = self.ptop
            self.ptop += nbytes
            self.top = self.ptop
        else:
            off = self.top
            self.top += nbytes
        assert self.top <= self.aend, f"SBUF arena overflow {self.top - self.abase}"
        return self.nc.alloc_sbuf_tensor_at(name or f"t{self._n}", list(shape), dt, offset=off)

    def new_phase(self):
        self.P.barrier()
        self.top = self.ptop

    def din(self, name, shape, dt=F32):
        t = self.nc.dram_tensor(name, list(shape), dt, kind="ExternalInput")
        self.inputs[name] = t
        return t.ap()

    def dout(self, name, shape, dt=F32):
        t = self.nc.dram_tensor(name, list(shape), dt, kind="ExternalOutput")
        self.outputs[name] = t
        return t.ap()

    def dscr(self, name, shape, dt):
        kind = "ExternalOutput" if name in self.debug else "Internal"
        t = self.nc.dram_tensor(name, list(shape), dt, kind=kind)
        if name in self.debug:
            self.outputs[name] = t
        return t.ap()

    def V(self, fn, *a, r=(), w=(), **kw):
        return self.P.op("dve", lambda e: getattr(e, fn)(*a, **kw), r, w)

    def A(self, fn, *a, r=(), w=(), **kw):
        return self.P.op("act", lambda e: getattr(e, fn)(*a, **kw), r, w)

    def G(self, fn, *a, r=(), w=(), **kw):
        return self.P.op("pool", lambda e: getattr(e, fn)(*a, **kw), r, w)

    def mm(self, out, lhsT, rhs, start=True, stop=True, r=(), w=(), skip=False):
        return self.P.op("pe", lambda e: e.matmul(out, lhsT, rhs, start=start, stop=stop, skip_group_check=skip), r, w)

    def tr(self, out, in_, ident, r=(), w=()):
        return self.P.op("pe", lambda e: e.transpose(out, in_, ident), r, w)

    def dma(self, q, out, in_, r=(), w=(), **kw):
        return self.P.dma(q, out, in_, r, w, **kw)

    def consts(self):
        iot = self.iot = self.sb([128, 128], F32, True)
        self.G("iota", iot[:], [[1, 128]], base=0, channel_multiplier=-1, allow_small_or_imprecise_dtypes=True, w=[iot])
        def cmp(op, dt):
            t = self.sb([128, 128], dt, True)
            self.V("tensor_single_scalar", t[:], iot[:], 0.0, op, r=[iot], w=[t])
            return t
        self.ident_f = cmp(ALU.is_equal, F32)
        self.ident_b = cmp(ALU.is_equal, BF16)
        self.triU_f = cmp(ALU.is_ge, F32)
        self.triL_f = cmp(ALU.is_le, F32)
        self.sU_f = cmp(ALU.is_gt, F32)
        self.sU_b = cmp(ALU.is_gt, BF16)
        self.ones_f = self.sb([128, 128], F32, True)
        self.V("memset", self.ones_f[:], 1.0, w=[self.ones_f])
        self.ones_b = self.sb([128, 128], BF16, True)
        self.V("memset", self.ones_b[:], 1.0, w=[self.ones_b])
        self.pidx = self.sb([128, 1], F32, True)
        self.G("iota", self.pidx[:], [[0, 1]], base=0, channel_multiplier=1, allow_small_or_imprecise_dtypes=True, w=[self.pidx])

    def bcast_row(self, col_ap_fn, out_tile, r=()):
        for half in range(2):
            ps = self.psf[4 + half]
            for k4 in range(4):
                kt = half * 4 + k4
                tmp = self.sb([128, 128], F32)
                self.V("tensor_scalar", tmp[:], self.ones_f[:], col_ap_fn(kt), None, ALU.mult, r=[self.ones_f, *r], w=[tmp])
                self.mm(ps[:, k4 * 128:(k4 + 1) * 128], tmp[:], self.ident_f[:], r=[tmp, self.ident_f], w=[ps])
            self.A("copy", out_tile[:, half * 512:(half + 1) * 512], ps[:], r=[ps], w=[out_tile])

    def declare_io(self):
        T = self.T
        self.xin = self.din("xin", [T, D])
        self.cT = self.din("cT", [128, 16])
        self.w_mod = self.din("w_mod", [DEPTH, D, 6 * D])
        self.bmodT = self.din("bmodT", [DEPTH, 128, 48])
        self.ln_g = self.din("ln_g", [DEPTH * 2, D])
        self.ln_b = self.din("ln_b", [DEPTH * 2, D])
        self.ev_w_in = self.din("ev_w_in", [2, D, EVEN_IN])
        self.ev_w_out = self.din("ev_w_out", [2, D, D])
        self.ev_lambda = self.din("ev_lambda", [2, 256])
        self.ev_subln_g = self.din("ev_subln_g", [2, 128])
        self.convwT = self.din("convwT", [2, 128, 24])
        self.convbT = self.din("convbT", [2, 128, 8])
        self.ev_gate_b = self.din("ev_gate_b", [2, 16])
        self.ev_mnorm_g = self.din("ev_mnorm_g", [2, 512])
        self.od_w_in = self.din("od_w_in", [2, D, ODD_IN])
        self.od_w_out = self.din("od_w_out", [2, D, D])
        self.od_decay = self.din("od_decay", [2, 8])
        self.od_qk_g = self.din("od_qk_g", [2, 128])
        self.wr = self.din("wr", [DEPTH, D, 36])
        self.br = self.din("br", [DEPTH, 36])
        self.wexp = [self.din(f"wexp{j}", [DEPTH * NEXP * 128, 2048]) for j in range(6)]
        self.rope_cos = self.din("rope_cos", [self.TL, 32])
        self.rope_sin = self.din("rope_sin", [self.TL, 32])
        self.yout = self.dout("yout", [self.TL, D])
        self.X = self.dscr("X", [T, D], F32)
        self.cat = self.dscr("cat", [T, D], BF16)
        self.catT = self.dscr("catT", [8, 128, T], BF16)
        self.qTA = self.dscr("qTA", [4, 128, T], BF16)
        self.kTA = self.dscr("kTA", [4, 128, T], BF16)
        self.vA = self.dscr("vA", [T, 512], BF16)
        self.qkraw = self.dscr("qkraw", [8, 128, T], BF16)
        self.vB = self.dscr("vB", [T, 512], BF16)
        self.ogB = self.dscr("ogB", [T, 512], F32)
        self.gB = self.dscr("gB", [T, 16], F32)
        self.hB = self.dscr("hB", [T, 512], F32)
        self.NB = self.NT * 2 + NEXP
        self.H2 = self.dscr("H2", [T, D], BF16)
        self.XB = self.dscr("XB", [self.NB * 128, D], BF16)
        self.YB = self.dscr("YB", [self.NB * 128, D], F32)
        self.wbf = self.dscr("wbf", [NEXP * 128, 12288], BF16)

    def phase_mods(self):
        cT = self.sb([128, 16], F32)
        self.dma("sp", cT[:], self.cT[:, :], w=[cT])
        cact = self.sb([128, 16], F32)
        self.A("activation", cact[:], cT[:], AF.Silu, r=[cT], w=[cact])
        wts = Rot([self.sb([128, 8, 768], F32) for _ in range(2)])
        bm = self.sb([128, DEPTH, 48], F32)
        for l in range(DEPTH):
            self.dma("sp", bm[:, l, :], self.bmodT[l], w=[bm])
        for l in range(DEPTH):
            ps = self.psf[l % 2]
            for cc in range(8):
                wt = wts.next()
                src = self.w_mod[l].rearrange("(kt p) n -> p kt n", p=128)[:, :, cc * 768:(cc + 1) * 768]
                self.dma("sp", wt[:], src, w=[wt])
                for mi in range(6):
                    m = cc * 6 + mi
                    for kt in range(KT):
                        self.mm(ps[:, m * 2:m * 2 + 2], wt[:, kt, mi * 128:(mi + 1) * 128], cact[:, kt * 2:kt * 2 + 2],
                                start=(kt == 0), stop=(kt == KT - 1), r=[wt, cact], w=[ps])
            self.V("tensor_tensor", self.modT[l][:], ps[:, 0:96].rearrange("p (m r) -> p m r", r=2),
                   bm[:, l, :].unsqueeze(2).broadcast_to([128, 48, 2]), ALU.add, r=[ps, bm], w=[self.modT[l]])

    def mod_col(self, l, idx, r):
        return lambda kt: self.modT[l][:, idx * 8 + kt, r:r + 1]

    def load_weight_bf16(self, dst, src2d, ncols):
        step = 2048
        for kt in range(KT):
            c0 = 0
            while c0 < ncols:
                c1 = min(ncols, c0 + step)
                self.dma("pool", dst[:, kt, c0:c1], src2d[kt * 128:(kt + 1) * 128, c0:c1], w=[dst])
                c0 = c1

    def x_src(self, l):
        return self.xin if l == 0 else self.X

    def make_hT(self, l, ti, xt, hT, s1p, sh, pst):
        r = 1 if ti < self.NTC else 0
        for kt in range(KT):
            ps = pst[kt // 4]
            self.tr(ps[:, (kt % 4) * 128:(kt % 4 + 1) * 128], xt[:, kt * 128:(kt + 1) * 128], self.ident_f[:],
                    r=[xt, self.ident_f], w=[ps])
        for kt in range(KT):
            ps = pst[kt // 4]
            src = ps[:, (kt % 4) * 128:(kt % 4 + 1) * 128]
            if kt % 2 == 0:
                self.V("tensor_scalar", hT[:, kt, :], src, s1p[:, kt, r:r + 1], sh(kt, r), ALU.mult, ALU.add,
                       r=[ps, s1p, self.modT[l]], w=[hT])
            else:
                self.A("activation", hT[:, kt, :], src, AF.Identity, bias=sh(kt, r), scale=s1p[:, kt, r:r + 1],
                       r=[ps, s1p, self.modT[l]], w=[hT])

    def setup_persistent(self):
        self.consts()
        self.modT = [self.sb([128, 48, 2], F32, True) for _ in range(DEPTH)]
        self.nmax = self.sb([128, 16], F32, True)
        self.wts = self.sb([128, self.NT, 2], F32, True)
        self.Msel = self.sb([128, self.NT, 2, 32], F32, True)
        NTL = self.TL // 128
        self.cos = self.sb([128, NTL, 32], F32, True)
        self.sin = self.sb([128, NTL, 32], F32, True)
        self.dma("sp", self.cos[:], self.rope_cos.rearrange("(n p) f -> p n f", p=128), w=[self.cos])
        self.dma("sp", self.sin[:], self.rope_sin.rearrange("(n p) f -> p n f", p=128), w=[self.sin])
        zt = self.sb([128, D], BF16)
        self.V("memset", zt[:], 0.0, w=[zt])
        for b in range(self.NB):
            self.dma("sp", self.XB[b * 128:(b + 1) * 128, :], zt[:], r=[zt], w=["XB"])

    def rope(self, ps, o, lt, tmps):
        G = o.shape[1]
        if not hasattr(o, "ap"):
            pass
        pst = ps
        p3 = pst[:, 0:G * 64].rearrange("p (g d) -> p g d", d=64)
        x1, x2 = p3[:, :, 0:32], p3[:, :, 32:64]
        cb = self.cos[:, lt, :].unsqueeze(1).broadcast_to([128, G, 32])
        sbn = self.sin[:, lt, :].unsqueeze(1).broadcast_to([128, G, 32])
        ta, tb = tmps
        rs = [self.cos, self.sin]
        self.V("tensor_tensor", ta[:, 0:G, :], x1, cb, ALU.mult, r=[pst, *rs], w=[ta])
        self.V("tensor_tensor", tb[:, 0:G, :], x2, sbn, ALU.mult, r=[pst, *rs], w=[tb])
        self.V("tensor_tensor", o[:, :, 0:32], ta[:, 0:G, :], tb[:, 0:G, :], ALU.subtract, r=[ta, tb], w=[o])
        self.V("tensor_tensor", ta[:, 0:G, :], x1, sbn, ALU.mult, r=[pst, *rs], w=[ta])
        self.V("tensor_tensor", tb[:, 0:G, :], x2, cb, ALU.mult, r=[pst, *rs], w=[tb])
        self.V("tensor_tensor", o[:, :, 32:64], ta[:, 0:G, :], tb[:, 0:G, :], ALU.add, r=[ta, tb], w=[o])

    def proj_even(self, l):
        i = l // 2
        NT, NTC = self.NT, self.NTC
        self.new_phase()
        wb = self.sb([128, KT, EVEN_IN], BF16)
        self.load_weight_bf16(wb, self.ev_w_in[i], EVEN_IN)
        s1p = self.sb([128, 8, 2], F32)
        self.V("tensor_scalar", s1p[:], self.modT[l][:, 8:16, :], 1.0, None, ALU.add, r=[self.modT[l]], w=[s1p])
        sh = lambda kt, r: self.modT[l][:, kt, r:r + 1]
        gb = self.sb([128, 16], F32)
        self.dma("sp", gb[:], self.ev_gate_b[i:i + 1, :].partition_broadcast(128), w=[gb])
        self.V("memset", self.nmax[:], 0.0, w=[self.nmax])
        xts = Rot([self.sb([128, D], F32) for _ in range(2)])
        hTs = Rot([self.sb([128, KT, 128], BF16) for _ in range(2)])
        os_ = Rot([self.sb([128, 8, 64], F32) for _ in range(2)])
        ta, tb = self.sb([128, 8, 32], F32), self.sb([128, 8, 32], F32)
        sq = self.sb([128, 8, 64], F32)
        nrm = self.sb([128, 8], F32)
        obs = Rot([self.sb([128, 512], BF16) for _ in range(2)])
        stg = Rot([self.sb([128, 4, 128], BF16) for _ in range(2)])
        vbs = Rot([self.sb([128, 512], BF16) for _ in range(2)])
        og = self.sb([128, 512], F32)
        g = self.sb([128, 16], F32)
        e4 = self.sb([128, 4], F32)
        stg2 = self.sb([128, 8, 128], BF16)
        rawbs = Rot([self.sb([128, 512], BF16) for _ in range(2)])
        psf, psb = self.psf, self.psb
        xsrc = self.x_src(l)
        for ti in range(NT):
            t0 = ti * 128
            is_ctx = ti < NTC
            xt = xts.next()
            self.dma("sp", xt[:], xsrc[t0:t0 + 128, :], r=["X"], w=[xt])
            hT = hTs.next()
            self.make_hT(l, ti, xt, hT, s1p, sh, [psf[0], psf[1]])
            for qi, (c0, dst, dname) in enumerate(((0, self.qTA, "qTA"), (512, self.kTA, "kTA"))):
                ps = psf[2 + qi]
                for kt in range(KT):
                    self.mm(ps[:, :], hT[:, kt, :], wb[:, kt, c0:c0 + 512], start=(kt == 0), stop=(kt == KT - 1),
                            r=[hT, wb], w=[ps])
                o = os_.next()
                if is_ctx:
                    self.A("copy", o[:].rearrange("p g d -> p (g d)"), ps[:, :], r=[ps], w=[o])
                else:
                    self.rope(ps, o, ti - NTC, (ta, tb))
                self.V("tensor_tensor", sq[:], o[:], o[:], ALU.mult, r=[o], w=[sq])
                self.V("tensor_reduce", nrm[:], sq[:], axis=AX.X, op=ALU.add, r=[sq], w=[nrm])
                self.V("tensor_tensor", self.nmax[:, qi * 8:qi * 8 + 8], self.nmax[:, qi * 8:qi * 8 + 8], nrm[:], ALU.max,
                       r=[nrm, self.nmax], w=[self.nmax])
                ob = obs.next()
                self.A("activation", ob[:], o[:].rearrange("p g d -> p (g d)"), AF.Copy, scale=(0.125 if qi == 0 else 1.0),
                       r=[o], w=[ob])
                pb = psb[qi]
                for pr in range(4):
                    self.tr(pb[:, pr * 128:(pr + 1) * 128], ob[:, pr * 128:(pr + 1) * 128], self.ident_b[:],
                            r=[ob, self.ident_b], w=[pb])
                st = stg.next()
                self.V("tensor_copy", st[:].rearrange("p a b -> p (a b)"), pb[:, 0:512], r=[pb], w=[st])
                self.dma("sp", dst.rearrange("h p t -> p h t")[:, :, t0:t0 + 128], st[:], r=[st], w=[dname])
            for vi, (c0, dst, dname) in enumerate(((1024, self.vA, "vA"), (2560, self.vB, "vB"))):
                ps = psf[4 + vi]
                for kt in range(KT):
                    self.mm(ps[:, :], hT[:, kt, :], wb[:, kt, c0:c0 + 512], start=(kt == 0), stop=(kt == KT - 1),
                            r=[hT, wb], w=[ps])
                vb = vbs.next()
                self.A("copy", vb[:], ps[:, :], r=[ps], w=[vb])
                self.dma("sp", dst[t0:t0 + 128, :], vb[:], r=[vb], w=[dname])
            ps = psf[4]
            for kt in range(KT):
                self.mm(ps[:, :], hT[:, kt, :], wb[:, kt, 3072:3584], start=(kt == 0), stop=(kt == KT - 1), r=[hT, wb], w=[ps])
            self.A("activation", og[:], ps[:, :], AF.Sigmoid, r=[ps], w=[og])
            self.dma("sp", self.ogB[t0:t0 + 128, :], og[:], r=[og], w=["ogB"])
            ps = psf[5]
            for kt in range(KT):
                self.mm(ps[:, 0:16], hT[:, kt, :], wb[:, kt, 3584:3600], start=(kt == 0), stop=(kt == KT - 1), r=[hT, wb], w=[ps])
            self.V("tensor_tensor", g[:], ps[:, 0:16], gb[:], ALU.add, r=[ps, gb], w=[g])
            for off in (4, 12):
                self.A("activation", e4[:], g[:, off:off + 4], AF.Exp, scale=-1.0, r=[g], w=[e4])
                self.A("activation", e4[:], e4[:], AF.Ln, bias=1.0, r=[e4], w=[e4])
                self.V("tensor_scalar", g[:, off:off + 4], e4[:], -1.0, None, ALU.mult, r=[e4], w=[g])
            self.dma("sp", self.gB[t0:t0 + 128, :], g[:], r=[g], w=["gB"])
            for half in range(2):
                ps = psf[2 + half]
                c0 = 1536 + half * 512
                for kt in range(KT):
                    self.mm(ps[:, :], hT[:, kt, :], wb[:, kt, c0:c0 + 512], start=(kt == 0), stop=(kt == KT - 1), r=[hT, wb], w=[ps])
                rb_ = rawbs.next()
                if half == 0:
                    self.A("copy", rb_[:], ps[:, :], r=[ps], w=[rb_])
                else:
                    self.V("tensor_copy", rb_[:], ps[:, :], r=[ps], w=[rb_])
                for cc in range(4):
                    self.tr(psb[half][:, cc * 128:(cc + 1) * 128], rb_[:, cc * 128:(cc + 1) * 128], self.ident_b[:],
                            r=[rb_, self.ident_b], w=[psb[half]])
                if half == 0:
                    self.V("tensor_copy", stg2[:, 0:4, :].rearrange("p a b -> p (a b)"), psb[0][:, 0:512], r=[psb[0]], w=[stg2])
                else:
                    self.A("copy", stg2[:, 4:8, :].rearrange("p a b -> p (a b)"), psb[1][:, 0:512], r=[psb[1]], w=[stg2])
            self.dma("sp", self.qkraw.rearrange("c p t -> p c t")[:, :, t0:t0 + 128], stg2[:], r=[stg2], w=["qkraw"])

    def compute_negm(self, scale):
        psf = self.psf
        self.tr(psf[0][0:16, 0:128], self.nmax[:, 0:16], self.ident_f[:], r=[self.nmax, self.ident_f], w=[psf[0]])
        mx = self.sb([16, 1], F32)
        self.V("tensor_reduce", mx[:], psf[0][0:16, 0:128], axis=AX.X, op=ALU.max, r=[psf[0]], w=[mx])
        dg = self.sb([16, 16], F32)
        self.V("tensor_scalar", dg[:], self.ident_f[0:16, 0:16], mx[:, 0:1], None, ALU.mult, r=[mx, self.ident_f], w=[dg])
        self.mm(psf[1][:, 0:16], self.ones_f[0:16, 0:128], dg[:], r=[self.ones_f, dg], w=[psf[1]])
        mxb = self.sb([128, 16], F32)
        self.V("tensor_copy", mxb[:], psf[1][:, 0:16], r=[psf[1]], w=[mxb])
        negm = self.sb([128, 8], F32)
        self.V("tensor_tensor", negm[:], mxb[:, 0:8], mxb[:, 8:16], ALU.mult, r=[mxb], w=[negm])
        self.A("activation", negm[:], negm[:], AF.Sqrt, r=[negm], w=[negm])
        self.V("tensor_scalar", negm[:], negm[:], -float(scale), None, ALU.mult, r=[negm], w=[negm])
        return negm

    def qblocks(self):
        blocks = []
        q0 = 0
        while q0 < self.TC:
            nq = min(512, self.TC - q0)
            blocks.append((q0, nq, list(range(self.NTC))))
            q0 += nq
        while q0 < self.T:
            nq = min(512, self.T - q0)
            blocks.append((q0, nq, list(range(self.NT))))
            q0 += nq
        return blocks

    def run_attn_jobs(self, jobs, pTs, st_rot):
        steps = [(job, ki, kt) for job in jobs for ki, kt in enumerate(job["ktiles"])]
        recs = {}

        def emit_S(idx):
            job, ki, kt = steps[idx]
            nq, kr = job["nq"], job["krow"]
            st = st_rot.next()
            self.mm(st[:, 0:nq], job["kT"][kr[0]:kr[1], kt * 128:(kt + 1) * 128], job["qT"][kr[0]:kr[1], job["q0"]:job["q0"] + nq],
                    r=[job["kT"], job["qT"]], w=[st])
            pT = pTs.next()
            self.A("activation", pT[:, 0:nq], st[:, 0:nq], AF.Exp, bias=job["negm_col"], scale=1.0, r=[st, job["ndep"]], w=[pT])
            recs[idx] = pT

        def emit_PV(idx):
            job, ki, kt = steps[idx]
            pT = recs.pop(idx)
            nq, dv = job["nq"], job["dv"]
            nsub = nq // 128
            first, last = ki == 0, ki == len(job["ktiles"]) - 1
            o_ps, s_ps = job["o_ps"], job["s_ps"]
            self.mm(o_ps[0:dv, 0:nq], job["v_fn"](kt), pT[:, 0:nq], start=first, stop=last, r=[pT, job["vdep"]], w=[o_ps])
            if job["sum_mode"] == "bc":
                self.mm(s_ps[:, 0:nq], self.ones_b[:, :], pT[:, 0:nq], start=first, stop=last, r=[pT, self.ones_b], w=[s_ps])
            if last:
                job["post"]()

        if not steps:
            return
        emit_S(0)
        for idx in range(len(steps)):
            if idx + 1 < len(steps):
                emit_S(idx + 1)
            emit_PV(idx)

    def rowsum_to_col(self, s_ps, nq, srow):
        nsub = nq // 128
        self.V("tensor_copy", srow[0:1, 0:nq], s_ps[0:1, 0:nq], r=[s_ps], w=[srow])
        for sub in range(nsub):
            self.mm(s_ps[:, sub:sub + 1], srow[0:1, sub * 128:(sub + 1) * 128], self.ones_f[0:1, 0:1], start=(sub == 0), stop=True,
                    r=[srow, self.ones_f], w=[s_ps], skip=True)

    def convert_expert_weights(self, l):
        R = NEXP * 128
        step = 512
        for j in range(6):
            for r0 in range(0, R, step):
                self.dma("pool", self.wbf[r0:r0 + step, j * 2048:(j + 1) * 2048], self.wexp[j][l * R + r0:l * R + r0 + step, :],
                         w=["wbf"])

    def attnA(self, l):
        i = l // 2
        T, NT = self.T, self.NT
        lam_init = 0.8 - 0.6 * math.exp(-0.3 * l)
        self.new_phase()
        psf = self.psf
        negm = self.compute_negm(0.125)
        self.convert_expert_weights(l)
        lrow = self.sb([128, 256], F32)
        self.dma("sp", lrow[:], self.ev_lambda[i:i + 1, :].partition_broadcast(128), w=[lrow])
        lp = self.sb([128, 2, 64], F32)
        l4 = lrow[:].rearrange("p (a d) -> p a d", d=64)
        self.V("tensor_tensor", lp[:, 0, :], l4[:, 0, :], l4[:, 1, :], ALU.mult, r=[lrow], w=[lp])
        self.V("tensor_tensor", lp[:, 1, :], l4[:, 2, :], l4[:, 3, :], ALU.mult, r=[lrow, lp], w=[lp])
        ls = self.sb([128, 2], F32)
        self.V("tensor_reduce", ls[:], lp[:], axis=AX.X, op=ALU.add, r=[lp], w=[ls])
        self.A("activation", ls[:], ls[:], AF.Exp, r=[ls], w=[ls])
        neglam = self.sb([128, 1], F32)
        self.V("tensor_tensor", neglam[:], ls[:, 1:2], ls[:, 0:1], ALU.subtract, r=[ls], w=[neglam])
        self.V("tensor_scalar", neglam[:], neglam[:], -lam_init, None, ALU.add, r=[neglam], w=[neglam])
        subg = self.sb([128, 128], F32)
        self.dma("sp", subg[:], self.ev_subln_g[i:i + 1, :].partition_broadcast(128), w=[subg])
        self.V("tensor_scalar", subg[:], subg[:], 1.0 - lam_init, None, ALU.mult, r=[subg], w=[subg])
        kTs = Rot([self.sb([128, T], BF16) for _ in range(2)])
        qTs = Rot([self.sb([128, T], BF16) for _ in range(2)])
        vhs = Rot([self.sb([128, NT, 128], BF16) for _ in range(2)])
        pTs = Rot([self.sb([128, 512], BF16) for _ in range(3)])
        st_rot = Rot([psf[4], psf[5]])
        r1 = self.sb([128, 512], F32)
        r2 = self.sb([128, 512], F32)
        t1 = self.sb([128, 512], F32)
        t2 = self.sb([128, 512], F32)
        outb = Rot([self.sb([128, 512], BF16) for _ in range(2)])
        subgc = self.sb([128, 1], F32)
        self.dma("sp", subgc[:], self.ev_subln_g[i:i + 1, :].rearrange("o d -> d o"), w=[subgc])
        self.V("tensor_scalar", subgc[:], subgc[:], 1.0 - lam_init, None, ALU.mult, r=[subgc], w=[subgc])
        srows = Rot([self.sb([1, 512], F32) for _ in range(2)])
        jobs = []
        for h in range(4):
            kT, qT, vh = kTs.next(), qTs.next(), vhs.next()
            def load(h=h, kT=kT, qT=qT, vh=vh):
                self.dma("sp", kT[:], self.kTA[h], r=["kTA"], w=[kT])
                self.dma("sp", qT[:], self.qTA[h], r=["qTA"], w=[qT])
                self.dma("sp", vh[:], self.vA.rearrange("(n p) f -> p n f", p=128)[:, :, h * 128:(h + 1) * 128], r=["vA"], w=[vh])
            load()
            for (q0, nq, ktiles) in self.qblocks():
                nsub = nq // 128

                def post(h=h, q0=q0, nq=nq, nsub=nsub):
                    V = self.V
                    V("reciprocal", r1[:, 0:nq], psf[1][:, 0:nq], r=[psf[1]], w=[r1])
                    V("reciprocal", r2[:, 0:nq], psf[3][:, 0:nq], r=[psf[3]], w=[r2])
                    V("tensor_tensor", t1[:, 0:nq], psf[0][:, 0:nq], r1[:, 0:nq], ALU.mult, r=[psf[0], r1], w=[t1])
                    V("scalar_tensor_tensor", t2[:, 0:nq], psf[2][:, 0:nq], neglam[:, 0:1], r2[:, 0:nq], ALU.mult, ALU.mult,
                      r=[psf[2], neglam, r2], w=[t2])
                    V("tensor_tensor", t1[:, 0:nq], t1[:, 0:nq], t2[:, 0:nq], ALU.add, r=[t1, t2], w=[t1])
                    V("tensor_tensor", t2[:, 0:nq], t1[:, 0:nq], t1[:, 0:nq], ALU.mult, r=[t1, t2], w=[t2])
                    self.mm(psf[1][:, 0:nq], self.ones_f[:, :], t2[:, 0:nq], r=[self.ones_f, t2], w=[psf[1]])
                    V("tensor_scalar", r1[:, 0:nq], psf[1][:, 0:nq], 1.0 / 128, EPS, ALU.mult, ALU.add, r=[psf[1], r1], w=[r1])
                    self.A("activation", r1[:, 0:nq], r1[:, 0:nq], AF.Sqrt, r=[r1], w=[r1])
                    V("reciprocal", r1[:, 0:nq], r1[:, 0:nq], r=[r1], w=[r1])
                    V("tensor_tensor", t1[:, 0:nq], t1[:, 0:nq], r1[:, 0:nq], ALU.mult, r=[t1, r1], w=[t1])
                    ob = outb.next()
                    self.A("activation", ob[:, 0:nq], t1[:, 0:nq], AF.Copy, scale=subgc[:, 0:1], r=[t1, subgc], w=[ob])
                    self.dma("sp", self.catT[h][:, q0:q0 + nq], ob[:, 0:nq], r=[ob], w=["catT"])

                for c in range(2):
                    jobs.append(dict(kT=kT, qT=qT, krow=(c * 64, (c + 1) * 64), v_fn=(lambda kt, vh=vh: vh[:, kt, :]), vdep=vh, dv=128,
                                     q0=q0, nq=nq, ktiles=ktiles, negm_col=negm[:, h * 2 + c:h * 2 + c + 1], ndep=negm,
                                     o_ps=psf[c * 2], s_ps=psf[c * 2 + 1], sum_mode="bc", post=(post if c == 1 else (lambda: None))))
            self.run_attn_jobs(jobs, pTs, st_rot)
            jobs = []


def rope_tables(TL):
    n_rows = TL // 64
    row = np.repeat(np.arange(n_rows, dtype=np.float32), 64)
    col = np.tile(np.arange(64, dtype=np.float32), n_rows)
    quarter = 16
    inv_freq = (10000.0 ** (-np.arange(quarter, dtype=np.float32) / quarter)).astype(np.float32)
    ang = np.concatenate([row[:, None] * inv_freq, col[:, None] * inv_freq], -1).astype(np.float32)
    return np.cos(ang).astype(np.float32), np.sin(ang).astype(np.float32)


_SHARED_CACHE = {}


def prep_shared(inp, TL):
    f = np.float32
    sh = {}
    sh["w_mod"] = np.ascontiguousarray(inp["w_mod"], f)
    sh["bmodT"] = np.ascontiguousarray(inp["b_mod"].reshape(DEPTH, 48, 128).transpose(0, 2, 1), f)
    sh["ln_g"] = np.ascontiguousarray(inp["ln_g"].reshape(DEPTH * 2, D), f)
    sh["ln_b"] = np.ascontiguousarray(inp["ln_b"].reshape(DEPTH * 2, D), f)
    sh["ev_w_in"] = np.ascontiguousarray(inp["ev_w_in"], f)
    sh["ev_w_out"] = np.ascontiguousarray(inp["ev_w_out"], f)
    sh["ev_lambda"] = np.ascontiguousarray(inp["ev_lambda"].reshape(2, 256), f)
    sh["ev_subln_g"] = np.ascontiguousarray(inp["ev_subln_g"], f)
    cw = inp["ev_conv_w"]
    sh["convwT"] = np.ascontiguousarray(cw.reshape(2, 3, 8, 128).transpose(0, 3, 2, 1).reshape(2, 128, 24), f)
    sh["convbT"] = np.ascontiguousarray(inp["ev_conv_b"].reshape(2, 8, 128).transpose(0, 2, 1), f)
    sh["ev_gate_b"] = np.ascontiguousarray(inp["ev_gate_b"].reshape(2, 16), f)
    sh["ev_mnorm_g"] = np.ascontiguousarray(inp["ev_mnorm_g"], f)
    sh["od_w_in"] = np.ascontiguousarray(inp["od_w_in"], f)
    sh["od_w_out"] = np.ascontiguousarray(inp["od_w_out"], f)
    sh["od_decay"] = np.ascontiguousarray(inp["od_decay"].reshape(2, 8), f)
    sh["od_qk_g"] = np.ascontiguousarray(inp["od_qk_g"].reshape(2, 128), f)
    sh["wr"] = np.ascontiguousarray(np.concatenate([inp["moe_w_group"], inp["moe_w_router"]], -1), f)
    sh["br"] = np.ascontiguousarray(np.concatenate([inp["moe_b_group"], inp["moe_b_router"]], -1), f)
    wg = inp["moe_w_gate"].reshape(DEPTH, NEXP, 8, 128, 512)
    wu = inp["moe_w_up"].reshape(DEPTH, NEXP, 8, 128, 512)
    wd = inp["moe_w_down"].reshape(DEPTH, NEXP, 4, 128, 1024)
    def lay(w, k0, k1):
        return np.ascontiguousarray(w[:, :, k0:k1].transpose(0, 1, 3, 2, 4).reshape(DEPTH * NEXP * 128, 2048), f)
    sh["wexp0"] = lay(wg, 0, 4)
    sh["wexp1"] = lay(wg, 4, 8)
    sh["wexp2"] = lay(wu, 0, 4)
    sh["wexp3"] = lay(wu, 4, 8)
    sh["wexp4"] = lay(wd, 0, 2)
    sh["wexp5"] = lay(wd, 2, 4)
    c, s = rope_tables(TL)
    sh["rope_cos"], sh["rope_sin"] = c, s
    return sh


def prep_core(inp, b):
    f = np.float32
    d = {}
    d["xin"] = np.ascontiguousarray(np.concatenate([inp["ctx"][b], inp["x"][b]], 0), f)
    c2 = np.stack([inp["c"][b], inp["c_ctx"]], 0)
    d["cT"] = np.ascontiguousarray(c2.reshape(2, 8, 128).transpose(2, 1, 0).reshape(128, 16), f)
    return d


def _mlstm(self, l):
    i = l // 2
    T, NT, NTC = self.T, self.NT, self.NTC
    self.new_phase()
    psf, psb = self.psf, self.psb
    V, A, G, mm = self.V, self.A, self.G, self.mm
    lnscale = -0.5 * math.log(128.0)
    qT = self.sb([128, 4, T], BF16)
    kT = self.sb([128, 4, T], BF16)
    cw = self.sb([128, 24], F32)
    cb = self.sb([128, 8], F32)
    self.dma("sp", cw[:], self.convwT[i], w=[cw])
    self.dma("sp", cb[:], self.convbT[i], w=[cb])
    PIECE = 1024
    rbs = Rot([self.sb([128, PIECE + 2], BF16) for _ in range(2)])
    accs = Rot([self.sb([128, PIECE], F32) for _ in range(2)])
    for cc in range(8):
        dstT = qT if cc < 4 else kT
        for (s0, s1) in ((0, self.TC), (self.TC, T)):
            a = s0
            while a < s1:
                b = min(s1, a + PIECE)
                n = b - a
                rb = rbs.next()
                lo = a - 1 if a > s0 else a
                hi = b + 1 if b < s1 else b
                if a == s0:
                    V("memset", rb[:, 0:1], 0.0, w=[rb])
                if b == s1:
                    V("memset", rb[:, n + 1:n + 2], 0.0, w=[rb])
                self.dma("sp", rb[:, 1 - (a - lo):1 + n + (hi - b)], self.qkraw[cc][:, lo:hi], r=["qkraw"], w=[rb])
                acc = accs.next()
                V("tensor_scalar", acc[:, 0:n], rb[:, 0:n], cw[:, cc * 3:cc * 3 + 1], None, ALU.mult, r=[rb, cw], w=[acc])
                V("scalar_tensor_tensor", acc[:, 0:n], rb[:, 1:n + 1], cw[:, cc * 3 + 1:cc * 3 + 2], acc[:, 0:n], ALU.mult, ALU.add,
                  r=[rb, cw, acc], w=[acc])
                V("scalar_tensor_tensor", acc[:, 0:n], rb[:, 2:n + 2], cw[:, cc * 3 + 2:cc * 3 + 3], acc[:, 0:n], ALU.mult, ALU.add,
                  r=[rb, cw, acc], w=[acc])
                A("activation", dstT[:, cc % 4, a:b], acc[:, 0:n], AF.Silu, bias=cb[:, cc:cc + 1], r=[acc, cb], w=[dstT])
                a = b
    vp = self.sb([128, NT, 4, 129], BF16)
    V("memset", vp[:, :, :, 128:129], 1.0, w=[vp])
    for n in range(NT):
        self.dma("sp", vp[:, n, :, 0:128], self.vB[n * 128:(n + 1) * 128, :].rearrange("p (h d) -> p h d", d=128), r=["vB"], w=[vp])
    gt = self.sb([128, NT, 16], F32)
    self.dma("sp", gt[:], self.gB.rearrange("(n p) g -> p n g", p=128), r=["gB"], w=[gt])
    mg = self.sb([128, 512], F32)
    self.dma("sp", mg[:], self.ev_mnorm_g[i:i + 1, :].partition_broadcast(128), w=[mg])
    S32 = self.sb([128, 4, 129], F32)
    Sb = self.sb([128, 4, 129], BF16)
    lfbcs = Rot([self.sb([128, 128], F32) for _ in range(4)])
    colterm = self.sb([128, 4], F32)
    X = self.sb([128, 4, 128], F32)
    At = self.sb([128, 4, 128], BF16)
    eB = self.sb([128, 4, 128], F32)
    qeb = self.sb([128, 4, 128], BF16)
    wk = self.sb([128, 4], F32)
    kw = self.sb([128, 4, 128], BF16)
    dn = self.sb([128, 4], F32)
    hout = self.sb([128, 4, 128], F32)
    hbs = Rot([self.sb([128, 512], F32) for _ in range(2)])
    ogs = Rot([self.sb([128, 512], F32) for _ in range(2)])
    st4 = self.sb([128, 4], F32)
    sq = self.sb([128, 4, 128], F32)
    catb = Rot([self.sb([128, 512], BF16) for _ in range(2)])
    BR, KQ, ND0, ND1 = psf[0], psf[2], psf[3], psf[4]
    BR3 = BR[:, :].rearrange("p (h t) -> p h t", t=128)
    KTp = psb[0]
    UPs = [psf[5][:, 0:258], psf[1][:, 128:386]]
    UPt = [psf[5], psf[1]]
    NDs = [ND0, ND1]
    for direction in ("bwd", "fwd"):
        fwd = direction == "fwd"
        V("memset", S32[:], 0.0, w=[S32])
        V("memset", Sb[:], 0.0, w=[Sb])
        order = list(range(NT)) if fwd else (list(range(NTC - 1, -1, -1)) + list(range(NT - 1, NTC - 1, -1)))
        tri = self.triU_f if fwd else self.triL_f
        e_idx = 127 if fwd else 0
        lic, lfc = (0, 4) if fwd else (8, 12)
        for c in order:
            tok0, tok1 = c * 128, (c + 1) * 128
            for h in range(4):
                lfbc = lfbcs.next()
                V("tensor_scalar", lfbc[:], self.ones_f[:], gt[:, c, lfc + h:lfc + h + 1], None, ALU.mult, r=[self.ones_f, gt], w=[lfbc])
                mm(BR[:, h * 128:(h + 1) * 128], lfbc[:], tri[:], r=[lfbc, tri], w=[BR])
            mm(psf[1][:, 0:4], tri[:], gt[:, c, lfc:lfc + 4], r=[tri, gt], w=[psf[1]])
            V("scalar_tensor_tensor", colterm[:], gt[:, c, lic:lic + 4], lnscale, psf[1][:, 0:4], ALU.add, ALU.subtract,
              r=[gt, psf[1]], w=[colterm])
            V("tensor_tensor", X[:], BR3, colterm[:].unsqueeze(2).broadcast_to([128, 4, 128]), ALU.add, r=[BR, colterm], w=[X])
            A("activation", X[:], X[:], AF.Exp, r=[X], w=[X])
            G("tensor_tensor", X[:], X[:], tri[:].unsqueeze(1).broadcast_to([128, 4, 128]), ALU.mult, r=[X, tri], w=[X])
            for h in range(4):
                mm(KQ[:, h * 128:(h + 1) * 128], kT[:, h, tok0:tok1], qT[:, h, tok0:tok1], r=[kT, qT], w=[KQ])
            V("tensor_tensor", At[:], KQ[:, :].rearrange("p (h t) -> p h t", t=128), X[:], ALU.mult, r=[KQ, X], w=[At])
            A("activation", eB[:], BR3, AF.Exp, r=[BR], w=[eB])
            G("tensor_tensor", qeb[:], qT[:, :, tok0:tok1], eB[:], ALU.mult, r=[qT, eB], w=[qeb])
            V("tensor_tensor", wk[:], colterm[:], BR3[:, :, e_idx], ALU.add, r=[colterm, BR], w=[wk])
            A("activation", wk[:], wk[:], AF.Exp, r=[wk], w=[wk])
            for h in range(4):
                self.tr(KTp[:, h * 128:(h + 1) * 128], kT[:, h, tok0:tok1], self.ident_b[:], r=[kT, self.ident_b], w=[KTp])
            V("tensor_tensor", kw[:], KTp[:, 0:512].rearrange("p (h d) -> p h d", d=128), wk[:].unsqueeze(2).broadcast_to([128, 4, 128]),
              ALU.mult, r=[KTp, wk], w=[kw])
            for h in range(4):
                nd = NDs[h // 2][:, (h % 2) * 129:(h % 2 + 1) * 129]
                mm(nd, At[:, h, :], vp[:, c, h, :], start=(h % 2 == 0), stop=False, r=[At, vp], w=[NDs[h // 2]], skip=True)
                mm(nd, qeb[:, h, :], Sb[:, h, :], start=False, stop=True, r=[qeb, Sb], w=[NDs[h // 2]], skip=True)
            for b2 in range(2):
                nd3 = NDs[b2][:, 0:258].rearrange("p (j e) -> p j e", e=129)
                A("activation", dn[:, b2 * 2:b2 * 2 + 2], nd3[:, :, 128], AF.Abs, r=[NDs[b2]], w=[dn])
            V("tensor_scalar", dn[:], dn[:], 1.0, None, ALU.max, r=[dn], w=[dn])
            V("reciprocal", dn[:], dn[:], r=[dn], w=[dn])
            for b2 in range(2):
                nd3 = NDs[b2][:, 0:258].rearrange("p (j e) -> p j e", e=129)
                V("tensor_tensor", hout[:, b2 * 2:b2 * 2 + 2, :], nd3[:, :, 0:128],
                  dn[:, b2 * 2:b2 * 2 + 2].unsqueeze(2).broadcast_to([128, 2, 128]), ALU.mult, r=[NDs[b2], dn], w=[hout])
            hflat = hout[:].rearrange("p h d -> p (h d)")
            if not fwd:
                self.dma("sp", self.hB[tok0:tok1, :], hflat, r=[hout], w=["hB"])
            else:
                hb, og = hbs.next(), ogs.next()
                self.dma("sp", hb[:], self.hB[tok0:tok1, :], r=["hB"], w=[hb])
                self.dma("sp", og[:], self.ogB[tok0:tok1, :], r=["ogB"], w=[og])
                V("tensor_tensor", hflat, hflat, hb[:], ALU.add, r=[hout, hb], w=[hout])
                V("tensor_tensor", hflat, hflat, og[:], ALU.mult, r=[hout, og], w=[hout])
                V("tensor_reduce", st4[:], hout[:], axis=AX.X, op=ALU.add, r=[hout], w=[st4])
                V("tensor_scalar", st4[:], st4[:], 1.0 / 128, None, ALU.mult, r=[st4], w=[st4])
                V("tensor_tensor", hout[:], hout[:], st4[:].unsqueeze(2).broadcast_to([128, 4, 128]), ALU.subtract, r=[hout, st4], w=[hout])
                G("tensor_tensor", sq[:], hout[:], hout[:], ALU.mult, r=[hout], w=[sq])
                V("tensor_reduce", st4[:], sq[:], axis=AX.X, op=ALU.add, r=[sq], w=[st4])
                V("tensor_scalar", st4[:], st4[:], 1.0 / 128, EPS, ALU.mult, ALU.add, r=[st4], w=[st4])
                A("activation", st4[:], st4[:], AF.Sqrt, r=[st4], w=[st4])
                V("reciprocal", st4[:], st4[:], r=[st4], w=[st4])
                V("tensor_tensor", hout[:], hout[:], st4[:].unsqueeze(2).broadcast_to([128, 4, 128]), ALU.mult, r=[hout, st4], w=[hout])
                cbt = catb.next()
                V("tensor_tensor", cbt[:], hflat, mg[:], ALU.mult, r=[hout, mg], w=[cbt])
                self.dma("sp", self.cat[tok0:tok1, 512:1024], cbt[:], r=[cbt], w=["cat"])
            for h in range(4):
                up = UPs[h // 2][:, (h % 2) * 129:(h % 2 + 1) * 129]
                mm(up, kw[:, h, :], vp[:, c, h, :], start=(h % 2 == 0), stop=True, r=[kw, vp], w=[UPt[h // 2]], skip=True)
            for h in range(4):
                up = UPs[h // 2][:, (h % 2) * 129:(h % 2 + 1) * 129]
                V("scalar_tensor_tensor", S32[:, h, :], S32[:, h, :], eB[:, h, e_idx:e_idx + 1], up, ALU.mult, ALU.add,
                  r=[S32, eB, UPt[h // 2]], w=[S32])
            A("copy", Sb[:], S32[:], r=[S32], w=[Sb])


Builder.mlstm = _mlstm


def _ln_tile(self, z, st, junk, lng, lnb, out):
    V, A = self.V, self.A
    V("tensor_reduce", st[:, 0:1], z[:], axis=AX.X, op=ALU.add, r=[z], w=[st])
    V("tensor_scalar", st[:, 0:1], st[:, 0:1], 1.0 / D, None, ALU.mult, r=[st], w=[st])
    V("tensor_scalar", z[:], z[:], st[:, 0:1], None, ALU.subtract, r=[z, st], w=[z])
    V("memset", st[:, 1:2], 0.0, r=[st], w=[st])
    A("activation", junk[:], z[:], AF.Square, accum_out=st[:, 1:2], r=[z, st], w=[junk, st])
    V("tensor_scalar", st[:, 1:2], st[:, 1:2], 1.0 / D, EPS, ALU.mult, ALU.add, r=[st], w=[st])
    A("activation", st[:, 1:2], st[:, 1:2], AF.Sqrt, r=[st], w=[st])
    V("reciprocal", st[:, 1:2], st[:, 1:2], r=[st], w=[st])
    V("scalar_tensor_tensor", out[:], z[:], st[:, 1:2], lng[:], ALU.mult, ALU.mult, r=[z, st, lng], w=[out])
    V("tensor_tensor", out[:], out[:], lnb[:], ALU.add, r=[out, lnb], w=[out])


Builder.ln_tile = _ln_tile


def _outproj(self, l, w_out_ap):
    T, NT, NTC = self.T, self.NT, self.NTC
    self.new_phase()
    psf, psb = self.psf, self.psb
    V, A, G, mm = self.V, self.A, self.G, self.mm
    mT = self.modT[l]
    wo = self.sb([128, KT, D], BF16)
    self.load_weight_bf16(wo, w_out_ap, D)
    s4p = self.sb([128, 8, 2], F32)
    V("tensor_scalar", s4p[:], mT[:, 32:40, :], 1.0, None, ALU.add, r=[mT], w=[s4p])
    g2, s4b, sh4b = [], [], []
    for r in range(2):
        t = self.sb([128, D], F32); self.bcast_row(self.mod_col(l, 2, r), t, r=[mT]); g2.append(t)
        t = self.sb([128, D], F32); self.bcast_row(lambda kt, r=r: s4p[:, kt, r:r + 1], t, r=[s4p]); s4b.append(t)
        t = self.sb([128, D], F32); self.bcast_row(self.mod_col(l, 3, r), t, r=[mT]); sh4b.append(t)
    lng = self.sb([128, D], F32)
    lnb = self.sb([128, D], F32)
    self.dma("sp", lng[:], self.ln_g[l * 2:l * 2 + 1, :].partition_broadcast(128), w=[lng])
    self.dma("sp", lnb[:], self.ln_b[l * 2:l * 2 + 1, :].partition_broadcast(128), w=[lnb])
    wrt = self.sb([128, KT, 36], F32)
    self.dma("sp", wrt[:], self.wr[l].rearrange("(kt p) n -> p kt n", p=128), w=[wrt])
    brb = self.sb([128, 36], F32)
    self.dma("sp", brb[:], self.br[l:l + 1, :].partition_broadcast(128), w=[brb])
    catts = Rot([self.sb([128, D], BF16) for _ in range(2)])
    catTs = Rot([self.sb([128, KT, 128], BF16) for _ in range(2)])
    xts = Rot([self.sb([128, D], F32) for _ in range(2)])
    zs = Rot([self.sb([128, D], F32) for _ in range(2)])
    xms = Rot([self.sb([128, D], F32) for _ in range(2)])
    h2s = Rot([self.sb([128, D], F32) for _ in range(2)])
    h2bs = Rot([self.sb([128, D], BF16) for _ in range(2)])
    h2T = self.sb([128, KT, 128], F32)
    junk = self.sb([128, D], F32)
    st = self.sb([128, 2], F32)
    lg = self.sb([128, 36], F32)
    sm = self.sb([128, 16], F32)
    g1h = self.sb([128, 4], F32)
    ge = self.sb([128, 4], F32)
    tmp48 = self.sb([128, 4, 8], F32)
    esel = self.sb([128, 8], F32)
    oh1 = self.sb([128, 8], F32)
    oh2 = self.sb([128, 8], F32)
    msk = self.sb([128, 8], F32)
    xsrc = self.x_src(l)
    for ti in range(NT):
        t0 = ti * 128
        r = 1 if ti < NTC else 0
        fm0 = 0 if l % 2 == 0 else 4
        tm0 = 4 - fm0
        catt = catts.next()
        self.dma("sp", catt[:, 0:512], self.cat[t0:t0 + 128, tm0 * 128:tm0 * 128 + 512], r=["cat"], w=[catt])
        catT = catTs.next()
        self.dma("sp", catT[:, fm0:fm0 + 4, :], self.catT[fm0:fm0 + 4].rearrange("c p t -> p c t")[:, :, t0:t0 + 128], r=["catT"], w=[catT])
        for k4 in range(4):
            self.tr(psb[0][:, k4 * 128:(k4 + 1) * 128], catt[:, k4 * 128:(k4 + 1) * 128], self.ident_b[:], r=[catt, self.ident_b], w=[psb[0]])
        A("copy", catT[:, tm0:tm0 + 4, :].rearrange("p a b -> p (a b)"), psb[0][:, 0:512], r=[psb[0]], w=[catT])
        for half in range(2):
            for kt in range(KT):
                mm(psf[half][:, :], catT[:, kt, :], wo[:, kt, half * 512:(half + 1) * 512], start=(kt == 0), stop=(kt == KT - 1),
                   r=[catT, wo], w=[psf[half]])
        xt = xts.next()
        self.dma("sp", xt[:], xsrc[t0:t0 + 128, :], r=["X"], w=[xt])
        z = zs.next()
        for half in range(2):
            V("tensor_tensor", z[:, half * 512:(half + 1) * 512], psf[half][:, :], g2[r][:, half * 512:(half + 1) * 512], ALU.mult,
              r=[psf[half], g2[r]], w=[z])
        V("scalar_tensor_tensor", z[:], xt[:], ALPHA, z[:], ALU.mult, ALU.add, r=[xt, z], w=[z])
        xm = xms.next()
        self.ln_tile(z, st, junk, lng, lnb, xm)
        self.dma("sp", self.X[t0:t0 + 128, :], xm[:], r=[xm], w=["X"])
        h2 = h2s.next()
        V("tensor_tensor", h2[:], xm[:], s4b[r][:], ALU.mult, r=[xm, s4b[r]], w=[h2])
        G("tensor_tensor", h2[:], h2[:], sh4b[r][:], ALU.add, r=[h2, sh4b[r]], w=[h2])
        h2b = h2bs.next()
        A("copy", h2b[:], h2[:], r=[h2], w=[h2b])
        self.dma("sp", self.H2[t0:t0 + 128, :], h2b[:], r=[h2b], w=["H2"])
        for kt in range(KT):
            self.tr(psf[2 + kt // 4][:, (kt % 4) * 128:(kt % 4 + 1) * 128], h2[:, kt * 128:(kt + 1) * 128], self.ident_f[:],
                    r=[h2, self.ident_f], w=[psf[2 + kt // 4]])
        A("copy", h2T[:, 0:4, :].rearrange("p a b -> p (a b)"), psf[2][:, :], r=[psf[2]], w=[h2T])
        V("tensor_copy", h2T[:, 4:8, :].rearrange("p a b -> p (a b)"), psf[3][:, :], r=[psf[3]], w=[h2T])
        for kt in range(KT):
            mm(psf[4][:, 0:36], h2T[:, kt, :], wrt[:, kt, :], start=(kt == 0), stop=(kt == KT - 1), r=[h2T, wrt], w=[psf[4]])
        V("tensor_tensor", lg[:], psf[4][:, 0:36], brb[:], ALU.add, r=[psf[4], brb], w=[lg])
        V("tensor_reduce", sm[:, 0:1], lg[:, 0:4], axis=AX.X, op=ALU.max, r=[lg], w=[sm])
        V("tensor_scalar", g1h[:], lg[:, 0:4], sm[:, 0:1], None, ALU.is_equal, r=[lg, sm], w=[g1h])
        V("tensor_scalar", sm[:, 1:2], sm[:, 0:1], -1.0, None, ALU.mult, r=[sm], w=[sm])
        V("memset", sm[:, 2:3], 0.0, r=[sm], w=[sm])
        A("activation", ge[:], lg[:, 0:4], AF.Exp, bias=sm[:, 1:2], accum_out=sm[:, 2:3], r=[lg, sm], w=[ge, sm])
        V("reciprocal", sm[:, 3:4], sm[:, 2:3], r=[sm], w=[sm])
        V("tensor_tensor", tmp48[:], lg[:, 4:36].rearrange("p (g e) -> p g e", e=8), g1h[:].unsqueeze(2).broadcast_to([128, 4, 8]),
          ALU.mult, r=[lg, g1h], w=[tmp48])
        V("tensor_reduce", esel[:], tmp48[:].rearrange("p g e -> p e g"), axis=AX.X, op=ALU.add, r=[tmp48], w=[esel])
        V("tensor_reduce", sm[:, 4:5], esel[:], axis=AX.X, op=ALU.max, r=[esel], w=[sm])
        V("tensor_scalar", oh1[:], esel[:], sm[:, 4:5], None, ALU.is_equal, r=[esel, sm], w=[oh1])
        V("scalar_tensor_tensor", msk[:], oh1[:], NEG_BIG, esel[:], ALU.mult, ALU.add, r=[oh1, esel], w=[msk])
        V("tensor_reduce", sm[:, 5:6], msk[:], axis=AX.X, op=ALU.max, r=[msk], w=[sm])
        V("tensor_scalar", oh2[:], msk[:], sm[:, 5:6], None, ALU.is_equal, r=[msk, sm], w=[oh2])
        V("tensor_tensor", sm[:, 6:7], sm[:, 5:6], sm[:, 4:5], ALU.subtract, r=[sm], w=[sm])
        A("activation", sm[:, 6:7], sm[:, 6:7], AF.Exp, r=[sm], w=[sm])
        V("tensor_scalar", sm[:, 7:8], sm[:, 6:7], 1.0, None, ALU.add, r=[sm], w=[sm])
        V("reciprocal", sm[:, 7:8], sm[:, 7:8], r=[sm], w=[sm])
        V("tensor_tensor", sm[:, 8:9], sm[:, 6:7], sm[:, 7:8], ALU.mult, r=[sm], w=[sm])
        V("tensor_scalar", self.wts[:, ti, 0:1], sm[:, 7:8], sm[:, 3:4], None, ALU.mult, r=[sm], w=[self.wts])
        V("tensor_scalar", self.wts[:, ti, 1:2], sm[:, 8:9], sm[:, 3:4], None, ALU.mult, r=[sm, self.wts], w=[self.wts])
        for k, oh in ((0, oh1), (1, oh2)):
            V("tensor_tensor", self.Msel[:, ti, k, :].rearrange("p (g e) -> p g e", e=8), g1h[:].unsqueeze(2).broadcast_to([128, 4, 8]),
              oh[:].unsqueeze(1).broadcast_to([128, 4, 8]), ALU.mult, r=[g1h, oh, self.Msel], w=[self.Msel])


Builder.outproj = _outproj


def _moe(self, l, last):
    T, NT, NTC = self.T, self.NT, self.NTC
    NB = self.NB
    self.new_phase()
    psf, psb = self.psf, self.psb
    V, A, G, mm = self.V, self.A, self.G, self.mm
    mT = self.modT[l]
    Msel, wts = self.Msel, self.wts
    Mb = self.sb([128, NT, 32], BF16)
    V("tensor_tensor", Mb[:], Msel[:, :, 0, :], Msel[:, :, 1, :], ALU.add, r=[Msel], w=[Mb])
    for ti in range(NT):
        mm(psf[0][0:32, 0:1], Mb[:, ti, :], self.ones_b[:, 0:1], start=(ti == 0), stop=(ti == NT - 1), r=[Mb, self.ones_b], w=[psf[0]])
    cc = self.sb([32, 4], F32)
    V("tensor_scalar", cc[:, 0:1], psf[0][0:32, 0:1], 1.0 / 128, 0.49609375, ALU.mult, ALU.add, r=[psf[0]], w=[cc])
    nbi = self.sb([32, 1], I32)
    V("tensor_copy", nbi[:], cc[:, 0:1], r=[cc], w=[nbi])
    V("tensor_copy", cc[:, 1:2], nbi[:], r=[nbi, cc], w=[cc])
    V("tensor_scalar", cc[:, 2:3], cc[:, 1:2], 128.0, None, ALU.mult, r=[cc], w=[cc])
    pbc = self.sb([32, 128], F32)
    V("tensor_scalar", pbc[:], self.ones_f[0:32, :], cc[:, 2:3], None, ALU.mult, r=[self.ones_f, cc], w=[pbc])
    mm(psf[1][:, 0:32], pbc[:], self.sU_f[0:32, 0:32], r=[pbc, self.sU_f], w=[psf[1]])
    mm(psf[1][:, 32:64], pbc[:], self.ident_f[0:32, 0:32], start=False, r=[pbc, self.ident_f], w=[psf[1]], skip=True)
    stb = self.sb([128, 64], F32)
    V("tensor_copy", stb[:], psf[1][:, 0:64], r=[psf[1]], w=[stb])
    endb = self.sb([128, 32], F32)
    V("tensor_tensor", endb[:], stb[:, 0:32], stb[:, 32:64], ALU.add, r=[stb], w=[endb])
    tot = self.sb([128, 32], F32)
    V("memset", tot[:], 0.0, w=[tot])
    sr = self.sb([128, 32], F32)
    pr = self.sb([128, 32], F32)
    destf = self.sb([128, NT, 2], F32)
    for ti in range(NT):
        mm(psf[2][:, 0:32], self.sU_b[:], Mb[:, ti, :], r=[self.sU_b, Mb], w=[psf[2]])
        mm(psf[3][:, 0:32], self.ones_b[:], Mb[:, ti, :], r=[self.ones_b, Mb], w=[psf[3]])
        V("tensor_tensor", sr[:], psf[2][:, 0:32], tot[:], ALU.add, r=[psf[2], tot], w=[sr])
        V("tensor_tensor", sr[:], sr[:], stb[:, 0:32], ALU.add, r=[sr, stb], w=[sr])
        V("tensor_tensor", tot[:], tot[:], psf[3][:, 0:32], ALU.add, r=[tot, psf[3]], w=[tot])
        for k in range(2):
            V("tensor_tensor", pr[:], Msel[:, ti, k, :], sr[:], ALU.mult, r=[Msel, sr], w=[pr])
            V("tensor_reduce", destf[:, ti, k:k + 1], pr[:], axis=AX.X, op=ALU.add, r=[pr, destf], w=[destf])
    desti = self.sb([128, NT, 2], I32)
    V("tensor_copy", desti[:], destf[:], r=[destf], w=[desti])
    bst = self.sb([128, NB], F32)
    G("iota", bst[:], [[128, NB]], base=0, channel_multiplier=0, allow_small_or_imprecise_dtypes=True, w=[bst])
    cmp = self.sb([128, NB, 32], F32)
    V("tensor_tensor", cmp[:], endb[:].unsqueeze(1).broadcast_to([128, NB, 32]), bst[:].unsqueeze(2).broadcast_to([128, NB, 32]),
      ALU.is_le, r=[endb, bst], w=[cmp])
    bex = self.sb([128, NB], F32)
    V("tensor_reduce", bex[:], cmp[:], axis=AX.X, op=ALU.add, r=[cmp], w=[bex])
    V("tensor_scalar", bex[:], bex[:], float(NEXP - 1), None, ALU.min, r=[bex], w=[bex])
    V("tensor_scalar", bex[:], bex[:], 128.0, self.pidx[:, 0:1], ALU.mult, ALU.add, r=[bex, self.pidx], w=[bex])
    widx = self.sb([128, NB], I32)
    V("tensor_copy", widx[:], bex[:], r=[bex], w=[widx])
    h2ts = Rot([self.sb([128, D], BF16) for _ in range(2)])
    for ti in range(NT):
        h2t = h2ts.next()
        self.dma("sp", h2t[:], self.H2[ti * 128:(ti + 1) * 128, :], r=["H2"], w=[h2t])
        for k in range(2):
            idx = desti[:, ti, k:k + 1]
            o = self.P.op("pool", lambda g, idx=idx, h2t=h2t: g.indirect_dma_start(
                out=self.XB[:, :], out_offset=bass.IndirectOffsetOnAxis(ap=idx, axis=0), in_=h2t[:], in_offset=None),
                reads=[desti, h2t, "XB"], writes=["XB"])
            o.is_dma = True
    wrot = Rot([self.sb([128, 12288], BF16) for _ in range(3)])
    xbs = Rot([self.sb([128, D], BF16) for _ in range(2)])
    xTs = Rot([self.sb([128, KT, 128], BF16) for _ in range(2)])
    sg = self.sb([128, 512], F32)
    hms = Rot([self.sb([128, 512], BF16) for _ in range(2)])
    hTs = Rot([self.sb([128, 4, 128], BF16) for _ in range(2)])
    ybs = Rot([self.sb([128, D], F32) for _ in range(2)])
    for b in range(NB):
        wt = wrot.next()
        idx = widx[:, b:b + 1]
        o = self.P.op("pool", lambda g, idx=idx, wt=wt: g.indirect_dma_start(
            out=wt[:], out_offset=None, in_=self.wbf[:, :], in_offset=bass.IndirectOffsetOnAxis(ap=idx, axis=0)),
            reads=[widx, "wbf"], writes=[wt])
        o.is_dma = True
        w6 = [wt[:, j * 2048:(j + 1) * 2048] for j in range(6)]
        xb = xbs.next()
        self.dma("sp", xb[:], self.XB[b * 128:(b + 1) * 128, :], r=["XB"], w=[xb])
        for kt in range(KT):
            self.tr(psb[0][:, kt * 128:(kt + 1) * 128], xb[:, kt * 128:(kt + 1) * 128], self.ident_b[:], r=[xb, self.ident_b], w=[psb[0]])
        xT = xTs.next()
        A("copy", xT[:, 0:4, :].rearrange("p a b -> p (a b)"), psb[0][:, 0:512], r=[psb[0]], w=[xT])
        V("tensor_copy", xT[:, 4:8, :].rearrange("p a b -> p (a b)"), psb[0][:, 512:1024], r=[psb[0]], w=[xT])
        for gi in range(2):
            for kt in range(KT):
                wsrc = w6[gi * 2 + kt // 4]
                c0 = (kt % 4) * 512
                mm(psf[gi][:, :], xT[:, kt, :], wsrc[:, c0:c0 + 512], start=(kt == 0), stop=(kt == KT - 1), r=[xT, wt], w=[psf[gi]])
        A("activation", sg[:], psf[0][:, :], AF.Silu, r=[psf[0]], w=[sg])
        hm = hms.next()
        V("tensor_tensor", hm[:], sg[:], psf[1][:, :], ALU.mult, r=[sg, psf[1]], w=[hm])
        for m in range(4):
            self.tr(psb[1][:, m * 128:(m + 1) * 128], hm[:, m * 128:(m + 1) * 128], self.ident_b[:], r=[hm, self.ident_b], w=[psb[1]])
        hT = hTs.next()
        A("copy", hT[:].rearrange("p a b -> p (a b)"), psb[1][:, 0:512], r=[psb[1]], w=[hT])
        for half in range(2):
            for m in range(4):
                wsrc = w6[4 + m // 2]
                c0 = (m % 2) * 1024 + half * 512
                mm(psf[2 + half][:, :], hT[:, m, :], wsrc[:, c0:c0 + 512], start=(m == 0), stop=(m == 3), r=[hT, wt], w=[psf[2 + half]])
        yb = ybs.next()
        A("copy", yb[:, 0:512], psf[2][:, :], r=[psf[2]], w=[yb])
        V("tensor_copy", yb[:, 512:1024], psf[3][:, :], r=[psf[3]], w=[yb])
        self.dma("sp", self.YB[b * 128:(b + 1) * 128, :], yb[:], r=[yb], w=["YB"])
    g5 = []
    for r in range(2):
        t = self.sb([128, D], F32); self.bcast_row(self.mod_col(l, 5, r), t, r=[mT]); g5.append(t)
    lng = self.sb([128, D], F32)
    lnb = self.sb([128, D], F32)
    self.dma("sp", lng[:], self.ln_g[l * 2 + 1:l * 2 + 2, :].partition_broadcast(128), w=[lng])
    self.dma("sp", lnb[:], self.ln_b[l * 2 + 1:l * 2 + 2, :].partition_broadcast(128), w=[lnb])
    y0s = Rot([self.sb([128, D], F32) for _ in range(2)])
    y1s = Rot([self.sb([128, D], F32) for _ in range(1)])
    xts = Rot([self.sb([128, D], F32) for _ in range(2)])
    xns = Rot([self.sb([128, D], F32) for _ in range(1)])
    junk = self.sb([128, D], F32)
    st = self.sb([128, 2], F32)
    for ti in range(NT):
        if last and ti < NTC:
            continue
        r = 1 if ti < NTC else 0
        ys = []
        for k, rot in ((0, y0s), (1, y1s)):
            yk = rot.next()
            idx = desti[:, ti, k:k + 1]
            o = self.P.op("pool", lambda g, idx=idx, yk=yk: g.indirect_dma_start(
                out=yk[:], out_offset=None, in_=self.YB[:, :], in_offset=bass.IndirectOffsetOnAxis(ap=idx, axis=0)),
                reads=[desti, "YB"], writes=[yk])
            o.is_dma = True
            ys.append(yk)
        xt = xts.next()
        self.dma("sp", xt[:], self.X[ti * 128:(ti + 1) * 128, :], r=["X"], w=[xt])
        y0, y1 = ys
        V("tensor_scalar", y0[:], y0[:], wts[:, ti, 0:1], None, ALU.mult, r=[y0, wts], w=[y0])
        V("scalar_tensor_tensor", y0[:], y1[:], wts[:, ti, 1:2], y0[:], ALU.mult, ALU.add, r=[y1, wts, y0], w=[y0])
        G("tensor_tensor", y0[:], y0[:], g5[r][:], ALU.mult, r=[y0, g5[r]], w=[y0])
        V("scalar_tensor_tensor", y0[:], xt[:], ALPHA, y0[:], ALU.mult, ALU.add, r=[xt, y0], w=[y0])
        xn = xns.next()
        self.ln_tile(y0, st, junk, lng, lnb, xn)
        if last:
            lt = ti - NTC
            self.dma("sp", self.yout[lt * 128:(lt + 1) * 128, :], xn[:], r=[xn], w=["yout"])
        else:
            self.dma("sp", self.X[ti * 128:(ti + 1) * 128, :], xn[:], r=[xn, "X"], w=["X"])


Builder.moe = _moe


def _proj_odd(self, l):
    i = l // 2
    NT, NTC = self.NT, self.NTC
    self.new_phase()
    psf, psb = self.psf, self.psb
    V, A, G, mm = self.V, self.A, self.G, self.mm
    wb = self.sb([128, KT, ODD_IN], BF16)
    self.load_weight_bf16(wb, self.od_w_in[i], ODD_IN)
    s1p = self.sb([128, 8, 2], F32)
    V("tensor_scalar", s1p[:], self.modT[l][:, 8:16, :], 1.0, None, ALU.add, r=[self.modT[l]], w=[s1p])
    sh = lambda kt, r: self.modT[l][:, kt, r:r + 1]
    qkg = self.sb([128, 128], F32)
    self.dma("sp", qkg[:], self.od_qk_g[i:i + 1, :].partition_broadcast(128), w=[qkg])
    V("memset", self.nmax[:], 0.0, w=[self.nmax])
    xts = Rot([self.sb([128, D], F32) for _ in range(2)])
    hTs = Rot([self.sb([128, KT, 128], BF16) for _ in range(2)])
    sqt = self.sb([128, 8, 64], F32)
    xn = self.sb([128, 8 * 64], F32)
    o = self.sb([128, 8, 64], F32)
    ta, tb = self.sb([128, 8, 32], F32), self.sb([128, 8, 32], F32)
    ss = self.sb([128, 8], F32)
    nrm = self.sb([128, 8], F32)
    obs = Rot([self.sb([128, 512], BF16) for _ in range(2)])
    stg = Rot([self.sb([128, 4, 128], BF16) for _ in range(2)])
    vbs = Rot([self.sb([128, 512], BF16) for _ in range(2)])
    og = self.sb([128, 512], F32)
    stg2 = self.sb([128, 4, 128], BF16)
    xsrc = self.x_src(l)

    def norm_rope(ps, c0, Gn, gcol, is_ctx, lt, nm_off, scale):
        p3 = ps[:, c0:c0 + Gn * 64].rearrange("p (g d) -> p g d", d=64)
        A("activation", sqt[:, 0:Gn, :], p3, AF.Square, r=[ps], w=[sqt])
        V("tensor_reduce", ss[:, 0:Gn], sqt[:, 0:Gn, :], axis=AX.X, op=ALU.add, r=[sqt], w=[ss])
        V("tensor_scalar", ss[:, 0:Gn], ss[:, 0:Gn], 1.0 / 64, EPS, ALU.mult, ALU.add, r=[ss], w=[ss])
        A("activation", ss[:, 0:Gn], ss[:, 0:Gn], AF.Sqrt, r=[ss], w=[ss])
        V("reciprocal", ss[:, 0:Gn], ss[:, 0:Gn], r=[ss], w=[ss])
        x3 = xn[:, 0:Gn * 64].rearrange("p (g d) -> p g d", d=64)
        V("tensor_tensor", x3, p3, ss[:, 0:Gn].unsqueeze(2).broadcast_to([128, Gn, 64]), ALU.mult, r=[ps, ss], w=[xn])
        G("tensor_tensor", x3, x3, qkg[:, gcol:gcol + 64].unsqueeze(1).broadcast_to([128, Gn, 64]), ALU.mult, r=[xn, qkg], w=[xn])
        if is_ctx:
            V("tensor_copy", o[:, 0:Gn, :], x3, r=[xn], w=[o])
        else:
            self.rope(xn, o[:, 0:Gn, :], lt, (ta, tb))
        V("tensor_tensor", sqt[:, 0:Gn, :], o[:, 0:Gn, :], o[:, 0:Gn, :], ALU.mult, r=[o], w=[sqt])
        V("tensor_reduce", nrm[:, 0:Gn], sqt[:, 0:Gn, :], axis=AX.X, op=ALU.add, r=[sqt], w=[nrm])
        V("tensor_tensor", self.nmax[:, nm_off:nm_off + Gn], self.nmax[:, nm_off:nm_off + Gn], nrm[:, 0:Gn], ALU.max,
          r=[nrm, self.nmax], w=[self.nmax])
        ob = obs.next()
        A("activation", ob[:, 0:Gn * 64], o[:, 0:Gn, :].rearrange("p g d -> p (g d)"), AF.Copy, scale=scale, r=[o], w=[ob])
        return ob

    for ti in range(NT):
        t0 = ti * 128
        is_ctx = ti < NTC
        xt = xts.next()
        self.dma("sp", xt[:], xsrc[t0:t0 + 128, :], r=["X"], w=[xt])
        hT = hTs.next()
        self.make_hT(l, ti, xt, hT, s1p, sh, [psf[0], psf[1]])

        def tokmajor(ps, c0, n):
            for kt in range(KT):
                mm(ps[:, 0:n], hT[:, kt, :], wb[:, kt, c0:c0 + n], start=(kt == 0), stop=(kt == KT - 1), r=[hT, wb], w=[ps])
        tokmajor(psf[2], 512, 512)
        vb = vbs.next()
        A("copy", vb[:], psf[2][:, :], r=[psf[2]], w=[vb])
        self.dma("sp", self.vB[t0:t0 + 128, :], vb[:], r=[vb], w=["vB"])
        tokmajor(psf[3], 1024, 512)
        A("activation", og[:], psf[3][:, :], AF.Silu, r=[psf[3]], w=[og])
        self.dma("sp", self.ogB[t0:t0 + 128, :], og[:], r=[og], w=["ogB"])
        tokmajor(psf[4], 1536, 512)
        ob = norm_rope(psf[4], 0, 8, 0, is_ctx, ti - NTC, 0, 0.125)
        for pr in range(4):
            self.tr(psb[0][:, pr * 128:(pr + 1) * 128], ob[:, pr * 128:(pr + 1) * 128], self.ident_b[:], r=[ob, self.ident_b], w=[psb[0]])
        st = stg.next()
        V("tensor_copy", st[:].rearrange("p a b -> p (a b)"), psb[0][:, 0:512], r=[psb[0]], w=[st])
        self.dma("sp", self.qTA.rearrange("h p t -> p h t")[:, :, t0:t0 + 128], st[:], r=[st], w=["qTA"])
        tokmajor(psf[5], 2048, 256)
        ob = norm_rope(psf[5], 0, 2, 64, is_ctx, ti - NTC, 8, 1.0)
        self.tr(psb[1][:, 0:128], ob[:, 0:128], self.ident_b[:], r=[ob, self.ident_b], w=[psb[1]])
        st = stg.next()
        V("tensor_copy", st[:, 0, :], psb[1][:, 0:128], r=[psb[1]], w=[st])
        self.dma("sp", self.kTA[0][:, t0:t0 + 128], st[:, 0, :], r=[st], w=["kTA"])
        vb = vbs.next()
        A("copy", vb[:, 0:128], psf[5][:, 128:256], r=[psf[5]], w=[vb])
        self.dma("sp", self.vA[t0:t0 + 128, 0:128], vb[:, 0:128], r=[vb], w=["vA"])
        tokmajor(psf[2], 0, 512)
        rb_ = obs.next()
        A("copy", rb_[:], psf[2][:, :], r=[psf[2]], w=[rb_])
        for cc in range(4):
            self.tr(psb[0][:, cc * 128:(cc + 1) * 128], rb_[:, cc * 128:(cc + 1) * 128], self.ident_b[:], r=[rb_, self.ident_b], w=[psb[0]])
        V("tensor_copy", stg2[:].rearrange("p a b -> p (a b)"), psb[0][:, 0:512], r=[psb[0]], w=[stg2])
        self.dma("sp", self.qkraw.rearrange("c p t -> p c t")[:, 0:4, t0:t0 + 128], stg2[:], r=[stg2], w=["qkraw"])


Builder.proj_odd = _proj_odd


def _retention(self, l):
    i = l // 2
    T, NT, NTC = self.T, self.NT, self.NTC
    self.new_phase()
    psf, psb = self.psf, self.psb
    V, A, G, mm = self.V, self.A, self.G, self.mm
    qT = self.sb([64, 4, T], BF16)
    kT = self.sb([64, 4, T], BF16)
    for h in range(4):
        self.dma("sp", qT[:, h, :], self.qkraw[h // 2][(h % 2) * 64:(h % 2 + 1) * 64, :], r=["qkraw"], w=[qT])
        self.dma("sp", kT[:, h, :], self.qkraw[2 + h // 2][(h % 2) * 64:(h % 2 + 1) * 64, :], r=["qkraw"], w=[kT])
    vp = self.sb([128, NT, 512], BF16)
    self.dma("sp", vp[:], self.vB.rearrange("(n p) f -> p n f", p=128), r=["vB"], w=[vp])
    ld = self.sb([128, 8], F32)
    self.dma("sp", ld[:], self.od_decay[i:i + 1, :].partition_broadcast(128), w=[ld])
    A("activation", ld[:], ld[:], AF.Exp, scale=-1.0, r=[ld], w=[ld])
    A("activation", ld[:], ld[:], AF.Ln, bias=1.0, r=[ld], w=[ld])
    V("tensor_scalar", ld[:], ld[:], -1.0, None, ALU.mult, r=[ld], w=[ld])
    lagp = self.sb([128, 128], F32)
    lagn = self.sb([128, 128], F32)
    V("tensor_scalar", lagp[:], self.iot[:], 0.0, None, ALU.max, r=[self.iot], w=[lagp])
    V("tensor_scalar", lagn[:], self.iot[:], -1.0, 0.0, ALU.mult, ALU.max, r=[self.iot], w=[lagn])
    rowf = self.sb([128, 128], F32)
    rowb = self.sb([128, 128], F32)
    G("iota", rowf[:], [[1, 128]], base=1, channel_multiplier=0, allow_small_or_imprecise_dtypes=True, w=[rowf])
    G("iota", rowb[:], [[-1, 128]], base=128, channel_multiplier=0, allow_small_or_imprecise_dtypes=True, w=[rowb])
    colf = self.sb([128, 1], F32)
    V("tensor_scalar", colf[:], self.pidx[:], -1.0, 127.0, ALU.mult, ALU.add, r=[self.pidx], w=[colf])
    Dm = self.sb([128, 2, 4, 128], F32)
    qd = self.sb([64, 2, 4, 128], F32)
    kd = self.sb([128, 2, 4], F32)
    cd = self.sb([128, 2, 4], F32)
    for d in range(2):
        for h in range(4):
            c = ld[:, d * 4 + h:d * 4 + h + 1]
            A("activation", Dm[:, d, h, :], (lagp if d == 0 else lagn)[:], AF.Exp, scale=c, r=[lagp, lagn, ld], w=[Dm])
            V("scalar_tensor_tensor", Dm[:, d, h, :], Dm[:, d, h, :], 0.125, (self.triU_f if d == 0 else self.triL_f)[:], ALU.mult, ALU.mult,
              r=[Dm, self.triU_f, self.triL_f], w=[Dm])
            A("activation", qd[:, d, h, :], (rowf if d == 0 else rowb)[0:64, :], AF.Exp, scale=ld[0:64, d * 4 + h:d * 4 + h + 1],
              r=[rowf, rowb, ld], w=[qd])
            A("activation", kd[:, d, h:h + 1], (colf if d == 0 else self.pidx)[:], AF.Exp, scale=c, r=[colf, self.pidx, ld], w=[kd])
    V("tensor_scalar", kd[:], kd[:], 0.125, None, ALU.mult, r=[kd], w=[kd])
    A("activation", cd[:].rearrange("p a b -> p (a b)"), ld[:], AF.Exp, scale=128.0, r=[ld], w=[cd])
    S32 = self.sb([64, 4, 128], F32)
    Sb = self.sb([64, 4, 128], BF16)
    At = self.sb([128, 4, 128], BF16)
    qeb = self.sb([64, 4, 128], BF16)
    kw = self.sb([128, 4, 64], BF16)
    hout = self.sb([128, 4, 128], F32)
    hbs = Rot([self.sb([128, 512], F32) for _ in range(2)])
    ogs = Rot([self.sb([128, 512], F32) for _ in range(2)])
    st4 = self.sb([128, 4], F32)
    sq = self.sb([128, 4, 128], F32)
    catb = Rot([self.sb([128, 512], BF16) for _ in range(2)])
    KQ, OP, UP, KTp = psf[0], psf[1], psf[2], psb[0]
    for direction in ("bwd", "fwd"):
        fwd = direction == "fwd"
        d = 0 if fwd else 1
        V("memset", S32[:], 0.0, w=[S32])
        V("memset", Sb[:], 0.0, w=[Sb])
        order = list(range(NT)) if fwd else (list(range(NTC - 1, -1, -1)) + list(range(NT - 1, NTC - 1, -1)))
        for c in order:
            tok0, tok1 = c * 128, (c + 1) * 128
            for h in range(4):
                mm(KQ[:, h * 128:(h + 1) * 128], kT[:, h, tok0:tok1], qT[:, h, tok0:tok1], r=[kT, qT], w=[KQ])
            V("tensor_tensor", At[:], KQ[:, :].rearrange("p (h t) -> p h t", t=128), Dm[:, d, :, :], ALU.mult, r=[KQ, Dm], w=[At])
            G("tensor_tensor", qeb[:], qT[:, :, tok0:tok1], qd[:, d, :, :], ALU.mult, r=[qT, qd], w=[qeb])
            for h in range(4):
                self.tr(KTp[:, h * 64:(h + 1) * 64], kT[:, h, tok0:tok1], self.ident_b[0:64, 0:64], r=[kT, self.ident_b], w=[KTp])
            V("tensor_tensor", kw[:], KTp[:, 0:256].rearrange("p (h d) -> p h d", d=64), kd[:, d, :].unsqueeze(2).broadcast_to([128, 4, 64]),
              ALU.mult, r=[KTp, kd], w=[kw])
            for h in range(4):
                o_ = OP[:, h * 128:(h + 1) * 128]
                mm(o_, At[:, h, :], vp[:, c, h * 128:(h + 1) * 128], start=(h == 0), stop=False, r=[At, vp], w=[OP], skip=True)
                mm(o_, qeb[:, h, :], Sb[:, h, :], start=False, stop=True, r=[qeb, Sb], w=[OP], skip=True)
            hflat = hout[:].rearrange("p h d -> p (h d)")
            if not fwd:
                A("copy", hflat, OP[:, :], r=[OP], w=[hout])
                self.dma("sp", self.hB[tok0:tok1, :], hflat, r=[hout], w=["hB"])
            else:
                hb, og = hbs.next(), ogs.next()
                self.dma("sp", hb[:], self.hB[tok0:tok1, :], r=["hB"], w=[hb])
                self.dma("sp", og[:], self.ogB[tok0:tok1, :], r=["ogB"], w=[og])
                V("tensor_tensor", hflat, OP[:, :], hb[:], ALU.add, r=[OP, hb], w=[hout])
                G("tensor_tensor", sq[:], hout[:], hout[:], ALU.mult, r=[hout], w=[sq])
                V("tensor_reduce", st4[:], sq[:], axis=AX.X, op=ALU.add, r=[sq], w=[st4])
                V("tensor_scalar", st4[:], st4[:], 1.0 / 128, EPS, ALU.mult, ALU.add, r=[st4], w=[st4])
                A("activation", st4[:], st4[:], AF.Sqrt, r=[st4], w=[st4])
                V("reciprocal", st4[:], st4[:], r=[st4], w=[st4])
                V("tensor_tensor", hout[:], hout[:], st4[:].unsqueeze(2).broadcast_to([128, 4, 128]), ALU.mult, r=[hout, st4], w=[hout])
                cbt = catb.next()
                V("tensor_tensor", cbt[:], hflat, og[:], ALU.mult, r=[hout, og], w=[cbt])
                self.dma("sp", self.cat[tok0:tok1, 0:512], cbt[:], r=[cbt], w=["cat"])
            for h in range(4):
                mm(UP[0:64, h * 128:(h + 1) * 128], kw[:, h, :], vp[:, c, h * 128:(h + 1) * 128], start=(h == 0), stop=True,
                   r=[kw, vp], w=[UP], skip=True)
            for h in range(4):
                V("scalar_tensor_tensor", S32[:, h, :], S32[:, h, :], cd[0:64, d, h:h + 1], UP[0:64, h * 128:(h + 1) * 128], ALU.mult, ALU.add,
                  r=[S32, cd, UP], w=[S32])
            A("copy", Sb[:], S32[:], r=[S32], w=[Sb])


Builder.retention = _retention


def _gqa(self, l):
    T, NT, NTC = self.T, self.NT, self.NTC
    self.new_phase()
    psf = self.psf
    V, A, G, mm = self.V, self.A, self.G, self.mm
    self.tr(psf[0][0:16, 0:128], self.nmax[:, 0:16], self.ident_f[:], r=[self.nmax, self.ident_f], w=[psf[0]])
    mx = self.sb([16, 1], F32)
    V("tensor_reduce", mx[:], psf[0][0:16, 0:128], axis=AX.X, op=ALU.max, r=[psf[0]], w=[mx])
    dg = self.sb([16, 16], F32)
    V("tensor_scalar", dg[:], self.ident_f[0:16, 0:16], mx[:, 0:1], None, ALU.mult, r=[mx, self.ident_f], w=[dg])
    mm(psf[1][:, 0:16], self.ones_f[0:16, 0:128], dg[:], r=[self.ones_f, dg], w=[psf[1]])
    mxb = self.sb([128, 16], F32)
    V("tensor_copy", mxb[:], psf[1][:, 0:16], r=[psf[1]], w=[mxb])
    negm = self.sb([128, 8], F32)
    for kv in range(2):
        V("tensor_scalar", negm[:, kv * 4:(kv + 1) * 4], mxb[:, kv * 4:(kv + 1) * 4], mxb[:, 8 + kv:9 + kv], None, ALU.mult, r=[mxb], w=[negm])
    A("activation", negm[:], negm[:], AF.Sqrt, r=[negm], w=[negm])
    V("tensor_scalar", negm[:], negm[:], -0.125, None, ALU.mult, r=[negm], w=[negm])
    kT = self.sb([128, T], BF16)
    self.dma("sp", kT[:], self.kTA[0], r=["kTA"], w=[kT])
    self.convert_expert_weights(l)
    vh = self.sb([128, NT, 2, 65], BF16)
    V("memset", vh[:, :, :, 64:65], 1.0, w=[vh])
    for n in range(NT):
        self.dma("sp", vh[:, n, :, 0:64], self.vA[n * 128:(n + 1) * 128, 0:128].rearrange("p (k d) -> p k d", d=64), r=["vA"], w=[vh])
    qTs = Rot([self.sb([128, T], BF16) for _ in range(2)])
    pTs = Rot([self.sb([128, 512], BF16) for _ in range(3)])
    st_rot = Rot([psf[4], psf[5]])
    srow = self.sb([65, 512], F32)
    rbs = Rot([self.sb([64, 512], F32) for _ in range(2)])
    outb = Rot([self.sb([64, 512], BF16) for _ in range(2)])
    bank = 0
    for j in range(8):
        kv = j // 4
        qT = qTs.next()
        self.dma("sp", qT[kv * 64:(kv + 1) * 64, :], self.qTA[j // 2][(j % 2) * 64:(j % 2 + 1) * 64, :], r=["qTA"], w=[qT])
        jobs = []
        for (q0, nq, ktiles) in self.qblocks():
            o_ps = psf[bank]
            b_ps = psf[2 + bank]
            bank = 1 - bank

            def post(j=j, q0=q0, nq=nq, o_ps=o_ps, b_ps=b_ps):
                A("copy", srow[64:65, 0:nq], o_ps[64:65, 0:nq], r=[o_ps], w=[srow])
                mm(b_ps[0:64, 0:nq], self.ones_f[64:65, 0:64], srow[64:65, 0:nq], r=[self.ones_f, srow], w=[b_ps])
                rb = rbs.next()
                V("reciprocal", rb[:, 0:nq], b_ps[0:64, 0:nq], r=[b_ps], w=[rb])
                ob = outb.next()
                V("tensor_tensor", ob[:, 0:nq], o_ps[0:64, 0:nq], rb[:, 0:nq], ALU.mult, r=[o_ps, rb], w=[ob])
                self.dma("sp", self.catT[4 + j // 2][(j % 2) * 64:(j % 2 + 1) * 64, q0:q0 + nq], ob[:, 0:nq], r=[ob], w=["catT"])

            jobs.append(dict(kT=kT, qT=qT, krow=(kv * 64, (kv + 1) * 64), v_fn=(lambda kt, kv=kv: vh[:, kt, kv, :]), vdep=vh, dv=65,
                             q0=q0, nq=nq, ktiles=ktiles, negm_col=negm[:, j:j + 1], ndep=negm, o_ps=o_ps, s_ps=None, sum_mode="col", post=post))
        self.run_attn_jobs(jobs, pTs, st_rot)


Builder.gqa = _gqa


def build_program(TC, TL, debug=()):
    B = Builder(TC, TL, debug=debug)
    B.declare_io()
    B.setup_persistent()
    B.phase_mods()
    for l in range(DEPTH):
        i = l // 2
        last = l == DEPTH - 1
        if l % 2 == 0:
            B.proj_even(l)
            B.attnA(l)
            B.mlstm(l)
            B.outproj(l, B.ev_w_out[i])
        else:
            B.proj_odd(l)
            B.retention(l)
            B.gqa(l)
            B.outproj(l, B.od_w_out[i])
        B.moe(l, last)
    B.new_phase()
    B.P.final_wait(list(B.outputs.keys()))
    B.P.emit()
    return B


def kernel(**inputs):
    inp = {k: np.asarray(v) for k, v in inputs.items()}
    BATCH, TL, _ = inp["x"].shape
    TC = inp["ctx"].shape[1]
    n_cores = 8
    B = build_program(TC, TL)
    sh = prep_shared(inp, TL)
    in_maps = []
    for core in range(n_cores):
        b = core % BATCH
        m = dict(sh)
        m.update(prep_core(inp, b))
        in_maps.append({k: v for k, v in m.items() if k in B.inputs})
    res = run_bass_kernel_spmd(B.nc, in_maps, core_ids=list(range(n_cores)))
    out = np.stack([np.asarray(res.results[b]["yout"], dtype=np.float32) for b in range(BATCH)], 0)
    return out
```

```python
import math
from contextlib import ExitStack
import numpy as np
import concourse.bass as bass
import concourse.mybir as mybir
from concourse.bass_utils import run_bass_kernel_spmd

F32 = mybir.dt.float32
BF16 = mybir.dt.bfloat16
I32 = mybir.dt.int32
AF = mybir.ActivationFunctionType
ALU = mybir.AluOpType
AX = mybir.AxisListType

D = 1024
KT = 8
DEPTH = 4
EPS = 1e-5
ALPHA = (2 * DEPTH) ** 0.25
EVEN_IN = 3600
ODD_IN = 2304
NEXP = 32
NEG_BIG = -1.0e30


def _k(r):
    if isinstance(r, (str, tuple, int)):
        return r
    return r.name


class Op:
    __slots__ = ("eng", "fn", "reads", "writes", "is_dma", "deps", "needs_inc", "tok", "seq", "dsem", "barrier")

    def __init__(self, eng, fn, reads, writes, is_dma):
        self.eng = eng
        self.fn = fn
        self.reads = reads
        self.writes = writes
        self.is_dma = is_dma
        self.deps = []
        self.needs_inc = False
        self.tok = None
        self.seq = -1
        self.dsem = -1
        self.barrier = False


class Prog:
    COMPUTE = ("pe", "act", "dve", "pool")
    ALL = ("pe", "act", "dve", "pool", "sp")

    def __init__(self, nc, n_dma_sems=20):
        self.nc = nc
        self.ops = []
        self.engs = {"pe": nc.tensor, "act": nc.scalar, "dve": nc.vector, "pool": nc.gpsimd, "sp": nc.sync}
        self.n_dma_sems = n_dma_sems
        self._n = 0

    def op(self, eng, fn, reads=(), writes=()):
        o = Op(eng, fn, tuple(_k(r) for r in reads), tuple(_k(w) for w in writes), False)
        self.ops.append(o)
        return o

    def dma(self, q, out, in_, reads=(), writes=(), **kw):
        o = Op(q, lambda e: e.dma_start(out=out, in_=in_, **kw), tuple(_k(r) for r in reads),
               tuple(_k(w) for w in writes), True)
        self.ops.append(o)
        return o

    def barrier(self):
        for e in self.ALL:
            o = Op(e, lambda en: en.nop(), (), (), False)
            o.barrier = True
            self.ops.append(o)

    def emit(self):
        nc = self.nc
        state = {}
        eng_seq = {e: 0 for e in self.engs}
        last_op = {e: None for e in self.COMPUTE}
        waited_c = {e: {p: -1 for p in self.COMPUTE} for e in self.engs}
        waited_d = {e: set() for e in self.engs}
        dma_q_count = {"sp": 0, "pool": 0, "act": 0}
        dma_last_on_sem = {}
        nbar = 0
        for o in self.ops:
            o.seq = eng_seq[o.eng]
            eng_seq[o.eng] += 1
            deps = []
            if o.barrier:
                for p in self.COMPUTE:
                    if last_op[p] is not None and p != o.eng:
                        deps.append(last_op[p])
                    elif last_op[p] is not None and p == o.eng and p != "pe":
                        deps.append(last_op[p])
                deps.extend(dma_last_on_sem.values())
                nbar += 1
                if nbar % len(self.ALL) == 0:
                    state = {}
            else:
                for r in o.reads:
                    st = state.get(r)
                    if st is not None and st[0] is not None:
                        deps.append(st[0])
                for w in o.writes:
                    st = state.get(w)
                    if st is not None:
                        if st[0] is not None:
                            deps.append(st[0])
                        deps.extend(st[1])
            if o.is_dma:
                k = dma_q_count[o.eng]
                dma_q_count[o.eng] += 1
                o.dsem = (o.eng, k % self.n_dma_sems)
                prev = dma_last_on_sem.get(o.dsem)
                if prev is not None:
                    deps.append(prev)
                dma_last_on_sem[o.dsem] = o
            final = []
            for d in deps:
                if d is o:
                    continue
                if d.is_dma:
                    if d in waited_d[o.eng]:
                        continue
                    waited_d[o.eng].add(d)
                    final.append(d)
                else:
                    if d.eng == "pe" and o.eng == "pe" and not o.is_dma:
                        continue
                    if waited_c[o.eng][d.eng] >= d.seq:
                        continue
                    waited_c[o.eng][d.eng] = d.seq
                    final.append(d)
            best = {}
            dm = []
            for d in final:
                if d.is_dma:
                    dm.append(d)
                elif d.eng not in best or best[d.eng].seq < d.seq:
                    best[d.eng] = d
            o.deps = dm + list(best.values())
            for d in o.deps:
                d.needs_inc = True
            if not o.barrier:
                for r in o.reads:
                    st = state.setdefault(r, [None, []])
                    st[1].append(o)
                for w in o.writes:
                    state[w] = [o, []]
            if not o.is_dma and o.eng in last_op and not o.barrier:
                last_op[o.eng] = o
        self._sem_ctx = []

        def mk(name):
            cm = nc.semaphore(name)
            s = cm.__enter__()
            self._sem_ctx.append(cm)
            return s

        sems = {e: mk(f"s_{e}") for e in self.COMPUTE}
        dsems = {}
        for q in ("sp", "pool", "act"):
            for i in range(min(self.n_dma_sems, dma_q_count[q])):
                dsems[(q, i)] = mk(f"d_{q}{i}")
        cnt = {e: 0 for e in self.COMPUTE}
        dcnt = {}
        n_wait = 0
        for o in self.ops:
            e = self.engs[o.eng]
            for d in o.deps:
                s, v = d.tok
                e.wait_ge(s, v)
                n_wait += 1
            inst = o.fn(e)
            if o.is_dma:
                s = dsems[o.dsem]
                dcnt[o.dsem] = dcnt.get(o.dsem, 0) + 16
                inst.then_inc(s, 16)
                o.tok = (s, dcnt[o.dsem])
            elif o.needs_inc:
                cnt[o.eng] += 1
                inst.then_inc(sems[o.eng], 1)
                o.tok = (sems[o.eng], cnt[o.eng])
        self.stats = dict(n_ops=len(self.ops), n_wait=n_wait, cnt=dict(cnt))
        return self.stats

    def final_wait(self, resources):
        self.op("sp", lambda e: e.nop(), reads=tuple(resources))


class Rot:
    def __init__(self, tiles):
        self.tiles = tiles
        self.i = 0

    def next(self):
        t = self.tiles[self.i % len(self.tiles)]
        self.i += 1
        return t


class Builder:
    def __init__(self, TC, TL, debug=()):
        self.TC, self.TL = TC, TL
        self.T = TC + TL
        self.NT = self.T // 128
        self.NTC = TC // 128
        self.debug = set(debug)
        nc = self.nc = bass.Bass("TRN2", target_bir_lowering=False)
        self.P = Prog(nc)
        arena_bytes = 196608
        ar = nc.alloc_sbuf_tensor("arena", [128, arena_bytes], mybir.dt.uint8)
        self.abase = nc.lookup_mloc(ar).addr
        self.aend = self.abase + arena_bytes
        self.ptop = self.abase
        self.top = self.abase
        self._n = 0
        self.inputs = {}
        self.outputs = {}
        self.psf = [nc.alloc_psum_tensor(f"psf{i}", [128, 512], F32) for i in range(6)]
        self.psb = [nc.alloc_psum_tensor(f"psb{i}", [128, 1024], BF16) for i in range(2)]

    def sb(self, shape, dt, persistent=False, name=None):
        self._n += 1
        esz = {F32: 4, BF16: 2, I32: 4}[dt]
        nbytes = int(np.prod(shape[1:])) * esz
        nbytes = (nbytes + 63) // 64 * 64
        if persistent:
            assert self.top == self.ptop, "persistent alloc only at phase boundary"
            off = self.ptop
            self.ptop += nbytes
            self.top = self.ptop
        else:
            off = self.top
            self.top += nbytes
        assert self.top <= self.aend, f"SBUF arena overflow {self.top - self.abase}"
        return self.nc.alloc_sbuf_tensor_at(name or f"t{self._n}", list(shape), dt, offset=off)

    def new_phase(self):
        self.P.barrier()
        self.top = self.ptop

    def din(self, name, shape, dt=F32):
        t = self.nc.dram_tensor(name, list(shape), dt, kind="ExternalInput")
        self.inputs[name] = t
        return t.ap()

    def dout(self, name, shape, dt=F32):
        t = self.nc.dram_tensor(name, list(shape), dt, kind="ExternalOutput")
        self.outputs[name] = t
        return t.ap()

    def dscr(self, name, shape, dt):
        kind = "ExternalOutput" if name in self.debug else "Internal"
        t = self.nc.dram_tensor(name, list(shape), dt, kind=kind)
        if name in self.debug:
            self.outputs[name] = t
        return t.ap()

    def V(self, fn, *a, r=(), w=(), **kw):
        return self.P.op("dve", lambda e: getattr(e, fn)(*a, **kw), r, w)

    def A(self, fn, *a, r=(), w=(), **kw):
        return self.P.op("act", lambda e: getattr(e, fn)(*a, **kw), r, w)

    def G(self, fn, *a, r=(), w=(), **kw):
        return self.P.op("pool", lambda e: getattr(e, fn)(*a, **kw), r, w)

    def mm(self, out, lhsT, rhs, start=True, stop=True, r=(), w=(), skip=False):
        return self.P.op("pe", lambda e: e.matmul(out, lhsT, rhs, start=start, stop=stop, skip_group_check=skip), r, w)

    def tr(self, out, in_, ident, r=(), w=()):
        return self.P.op("pe", lambda e: e.transpose(out, in_, ident), r, w)

    def dma(self, q, out, in_, r=(), w=(), **kw):
        return self.P.dma(q, out, in_, r, w, **kw)

    def consts(self):
        iot = self.iot = self.sb([128, 128], F32, True)
        self.G("iota", iot[:], [[1, 128]], base=0, channel_multiplier=-1, allow_small_or_imprecise_dtypes=True, w=[iot])
        def cmp(op, dt):
            t = self.sb([128, 128], dt, True)
            self.V("tensor_single_scalar", t[:], iot[:], 0.0, op, r=[iot], w=[t])
            return t
        self.ident_f = cmp(ALU.is_equal, F32)
        self.ident_b = cmp(ALU.is_equal, BF16)
        self.triU_f = cmp(ALU.is_ge, F32)
        self.triL_f = cmp(ALU.is_le, F32)
        self.sU_f = cmp(ALU.is_gt, F32)
        self.sU_b = cmp(ALU.is_gt, BF16)
        self.ones_f = self.sb([128, 128], F32, True)
        self.V("memset", self.ones_f[:], 1.0, w=[self.ones_f])
        self.ones_b = self.sb([128, 128], BF16, True)
        self.V("memset", self.ones_b[:], 1.0, w=[self.ones_b])
        self.pidx = self.sb([128, 1], F32, True)
        self.G("iota", self.pidx[:], [[0, 1]], base=0, channel_multiplier=1, allow_small_or_imprecise_dtypes=True, w=[self.pidx])

    def bcast_row(self, col_ap_fn, out_tile, r=()):
        for half in range(2):
            ps = self.psf[4 + half]
            for k4 in range(4):
                kt = half * 4 + k4
                tmp = self.sb([128, 128], F32)
                self.V("tensor_scalar", tmp[:], self.ones_f[:], col_ap_fn(kt), None, ALU.mult, r=[self.ones_f, *r], w=[tmp])
                self.mm(ps[:, k4 * 128:(k4 + 1) * 128], tmp[:], self.ident_f[:], r=[tmp, self.ident_f], w=[ps])
            self.A("copy", out_tile[:, half * 512:(half + 1) * 512], ps[:], r=[ps], w=[out_tile])

    def declare_io(self):
        T = self.T
        self.xin = self.din("xin", [T, D])
        self.cT = self.din("cT", [128, 16])
        self.w_mod = self.din("w_mod", [DEPTH, D, 6 * D])
        self.bmodT = self.din("bmodT", [DEPTH, 128, 48])
        self.ln_g = self.din("ln_g", [DEPTH * 2, D])
        self.ln_b = self.din("ln_b", [DEPTH * 2, D])
        self.ev_w_in = self.din("ev_w_in", [2, D, EVEN_IN])
        self.ev_w_out = self.din("ev_w_out", [2, D, D])
        self.ev_lambda = self.din("ev_lambda", [2, 256])
        self.ev_subln_g = self.din("ev_subln_g", [2, 128])
        self.convwT = self.din("convwT", [2, 128, 24])
        self.convbT = self.din("convbT", [2, 128, 8])
        self.ev_gate_b = self.din("ev_gate_b", [2, 16])
        self.ev_mnorm_g = self.din("ev_mnorm_g", [2, 512])
        self.od_w_in = self.din("od_w_in", [2, D, ODD_IN])
        self.od_w_out = self.din("od_w_out", [2, D, D])
        self.od_decay = self.din("od_decay", [2, 8])
        self.od_qk_g = self.din("od_qk_g", [2, 128])
        self.wr = self.din("wr", [DEPTH, D, 36])
        self.br = self.din("br", [DEPTH, 36])
        self.wexp = [self.din(f"wexp{j}", [DEPTH * NEXP * 128, 2048]) for j in range(6)]
        self.rope_cos = self.din("rope_cos", [self.TL, 32])
        self.rope_sin = self.din("rope_sin", [self.TL, 32])
        self.yout = self.dout("yout", [self.TL, D])
        self.X = self.dscr("X", [T, D], F32)
        self.cat = self.dscr("cat", [T, D], BF16)
        self.catT = self.dscr("catT", [8, 128, T], BF16)
        self.qTA = self.dscr("qTA", [4, 128, T], BF16)
        self.kTA = self.dscr("kTA", [4, 128, T], BF16)
        self.vA = self.dscr("vA", [T, 512], BF16)
        self.qkraw = self.dscr("qkraw", [8, 128, T], BF16)
        self.vB = self.dscr("vB", [T, 512], BF16)
        self.ogB = self.dscr("ogB", [T, 512], F32)
        self.gB = self.dscr("gB", [T, 16], F32)
        self.hB = self.dscr("hB", [T, 512], F32)
        self.NB = self.NT * 2 + NEXP
        self.H2 = self.dscr("H2", [T, D], BF16)
        self.XB = self.dscr("XB", [self.NB * 128, D], BF16)
        self.YB = self.dscr("YB", [self.NB * 128, D], F32)
        self.wbf = self.dscr("wbf", [NEXP * 128, 12288], BF16)

    def phase_mods(self):
        cT = self.sb([128, 16], F32)
        self.dma("sp", cT[:], self.cT[:, :], w=[cT])
        cact = self.sb([128, 16], F32)
        self.A("activation", cact[:], cT[:], AF.Silu, r=[cT], w=[cact])
        wts = Rot([self.sb([128, 8, 768], F32) for _ in range(2)])
        bm = self.sb([128, DEPTH, 48], F32)
        for l in range(DEPTH):
            self.dma("sp", bm[:, l, :], self.bmodT[l], w=[bm])
        for l in range(DEPTH):
            ps = self.psf[l % 2]
            for cc in range(8):
                wt = wts.next()
                src = self.w_mod[l].rearrange("(kt p) n -> p kt n", p=128)[:, :, cc * 768:(cc + 1) * 768]
                self.dma("sp", wt[:], src, w=[wt])
                for mi in range(6):
                    m = cc * 6 + mi
                    for kt in range(KT):
                        self.mm(ps[:, m * 2:m * 2 + 2], wt[:, kt, mi * 128:(mi + 1) * 128], cact[:, kt * 2:kt * 2 + 2],
                                start=(kt == 0), stop=(kt == KT - 1), r=[wt, cact], w=[ps])
            self.V("tensor_tensor", self.modT[l][:], ps[:, 0:96].rearrange("p (m r) -> p m r", r=2),
                   bm[:, l, :].unsqueeze(2).broadcast_to([128, 48, 2]), ALU.add, r=[ps, bm], w=[self.modT[l]])

    def mod_col(self, l, idx, r):
        return lambda kt: self.modT[l][:, idx * 8 + kt, r:r + 1]

    def load_weight_bf16(self, dst, src2d, ncols):
        step = 2048
        for kt in range(KT):
            c0 = 0
            while c0 < ncols:
                c1 = min(ncols, c0 + step)
                self.dma("pool", dst[:, kt, c0:c1], src2d[kt * 128:(kt + 1) * 128, c0:c1], w=[dst])
                c0 = c1

    def x_src(self, l):
        return self.xin if l == 0 else self.X

    def make_hT(self, l, ti, xt, hT, s1p, sh, pst):
        r = 1 if ti < self.NTC else 0
        for kt in range(KT):
            ps = pst[kt // 4]
            self.tr(ps[:, (kt % 4) * 128:(kt % 4 + 1) * 128], xt[:, kt * 128:(kt + 1) * 128], self.ident_f[:],
                    r=[xt, self.ident_f], w=[ps])
        for kt in range(KT):
            ps = pst[kt // 4]
            src = ps[:, (kt % 4) * 128:(kt % 4 + 1) * 128]
            if kt % 2 == 0:
                self.V("tensor_scalar", hT[:, kt, :], src, s1p[:, kt, r:r + 1], sh(kt, r), ALU.mult, ALU.add,
                       r=[ps, s1p, self.modT[l]], w=[hT])
            else:
                self.A("activation", hT[:, kt, :], src, AF.Identity, bias=sh(kt, r), scale=s1p[:, kt, r:r + 1],
                       r=[ps, s1p, self.modT[l]], w=[hT])

    def setup_persistent(self):
        self.consts()
        self.modT = [self.sb([128, 48, 2], F32, True) for _ in range(DEPTH)]
        self.nmax = self.sb([128, 16], F32, True)
        self.wts = self.sb([128, self.NT, 2], F32, True)
        self.Msel = self.sb([128, self.NT, 2, 32], F32, True)
        NTL = self.TL // 128
        self.cos = self.sb([128, NTL, 32], F32, True)
        self.sin = self.sb([128, NTL, 32], F32, True)
        self.dma("sp", self.cos[:], self.rope_cos.rearrange("(n p) f -> p n f", p=128), w=[self.cos])
        self.dma("sp", self.sin[:], self.rope_sin.rearrange("(n p) f -> p n f", p=128), w=[self.sin])
        zt = self.sb([128, D], BF16)
        self.V("memset", zt[:], 0.0, w=[zt])
        for b in range(self.NB):
            self.dma("sp", self.XB[b * 128:(b + 1) * 128, :], zt[:], r=[zt], w=["XB"])

    def rope(self, ps, o, lt, tmps):
        G = o.shape[1]
        if not hasattr(o, "ap"):
            pass
        pst = ps
        p3 = pst[:, 0:G * 64].rearrange("p (g d) -> p g d", d=64)
        x1, x2 = p3[:, :, 0:32], p3[:, :, 32:64]
        cb = self.cos[:, lt, :].unsqueeze(1).broadcast_to([128, G, 32])
        sbn = self.sin[:, lt, :].unsqueeze(1).broadcast_to([128, G, 32])
        ta, tb = tmps
        rs = [self.cos, self.sin]
        self.V("tensor_tensor", ta[:, 0:G, :], x1, cb, ALU.mult, r=[pst, *rs], w=[ta])
        self.V("tensor_tensor", tb[:, 0:G, :], x2, sbn, ALU.mult, r=[pst, *rs], w=[tb])
        self.V("tensor_tensor", o[:, :, 0:32], ta[:, 0:G, :], tb[:, 0:G, :], ALU.subtract, r=[ta, tb], w=[o])
        self.V("tensor_tensor", ta[:, 0:G, :], x1, sbn, ALU.mult, r=[pst, *rs], w=[ta])
        self.V("tensor_tensor", tb[:, 0:G, :], x2, cb, ALU.mult, r=[pst, *rs], w=[tb])
        self.V("tensor_tensor", o[:, :, 32:64], ta[:, 0:G, :], tb[:, 0:G, :], ALU.add, r=[ta, tb], w=[o])

    def proj_even(self, l):
        i = l // 2
        NT, NTC = self.NT, self.NTC
        self.new_phase()
        wb = self.sb([128, KT, EVEN_IN], BF16)
        self.load_weight_bf16(wb, self.ev_w_in[i], EVEN_IN)
        s1p = self.sb([128, 8, 2], F32)
        self.V("tensor_scalar", s1p[:], self.modT[l][:, 8:16, :], 1.0, None, ALU.add, r=[self.modT[l]], w=[s1p])
        sh = lambda kt, r: self.modT[l][:, kt, r:r + 1]
        gb = self.sb([128, 16], F32)
        self.dma("sp", gb[:], self.ev_gate_b[i:i + 1, :].partition_broadcast(128), w=[gb])
        self.V("memset", self.nmax[:], 0.0, w=[self.nmax])
        xts = Rot([self.sb([128, D], F32) for _ in range(2)])
        hTs = Rot([self.sb([128, KT, 128], BF16) for _ in range(2)])
        os_ = Rot([self.sb([128, 8, 64], F32) for _ in range(2)])
        ta, tb = self.sb([128, 8, 32], F32), self.sb([128, 8, 32], F32)
        sq = self.sb([128, 8, 64], F32)
        nrm = self.sb([128, 8], F32)
        obs = Rot([self.sb([128, 512], BF16) for _ in range(2)])
        stg = Rot([self.sb([128, 4, 128], BF16) for _ in range(2)])
        vbs = Rot([self.sb([128, 512], BF16) for _ in range(2)])
        og = self.sb([128, 512], F32)
        g = self.sb([128, 16], F32)
        e4 = self.sb([128, 4], F32)
        stg2 = self.sb([128, 8, 128], BF16)
        rawbs = Rot([self.sb([128, 512], BF16) for _ in range(2)])
        psf, psb = self.psf, self.psb
        xsrc = self.x_src(l)
        for ti in range(NT):
            t0 = ti * 128
            is_ctx = ti < NTC
            xt = xts.next()
            self.dma("sp", xt[:], xsrc[t0:t0 + 128, :], r=["X"], w=[xt])
            hT = hTs.next()
            self.make_hT(l, ti, xt, hT, s1p, sh, [psf[0], psf[1]])
            for qi, (c0, dst, dname) in enumerate(((0, self.qTA, "qTA"), (512, self.kTA, "kTA"))):
                ps = psf[2 + qi]
                for kt in range(KT):
                    self.mm(ps[:, :], hT[:, kt, :], wb[:, kt, c0:c0 + 512], start=(kt == 0), stop=(kt == KT - 1),
                            r=[hT, wb], w=[ps])
                o = os_.next()
                if is_ctx:
                    self.A("copy", o[:].rearrange("p g d -> p (g d)"), ps[:, :], r=[ps], w=[o])
                else:
                    self.rope(ps, o, ti - NTC, (ta, tb))
                self.V("tensor_tensor", sq[:], o[:], o[:], ALU.mult, r=[o], w=[sq])
                self.V("tensor_reduce", nrm[:], sq[:], axis=AX.X, op=ALU.add, r=[sq], w=[nrm])
                self.V("tensor_tensor", self.nmax[:, qi * 8:qi * 8 + 8], self.nmax[:, qi * 8:qi * 8 + 8], nrm[:], ALU.max,
                       r=[nrm, self.nmax], w=[self.nmax])
                ob = obs.next()
                self.A("activation", ob[:], o[:].rearrange("p g d -> p (g d)"), AF.Copy, scale=(0.125 if qi == 0 else 1.0),
                       r=[o], w=[ob])
                pb = psb[qi]
                for pr in range(4):
                    self.tr(pb[:, pr * 128:(pr + 1) * 128], ob[:, pr * 128:(pr + 1) * 128], self.ident_b[:],
                            r=[ob, self.ident_b], w=[pb])
                st = stg.next()
                self.V("tensor_copy", st[:].rearrange("p a b -> p (a b)"), pb[:, 0:512], r=[pb], w=[st])
                self.dma("sp", dst.rearrange("h p t -> p h t")[:, :, t0:t0 + 128], st[:], r=[st], w=[dname])
            for vi, (c0, dst, dname) in enumerate(((1024, self.vA, "vA"), (2560, self.vB, "vB"))):
                ps = psf[4 + vi]
                for kt in range(KT):
                    self.mm(ps[:, :], hT[:, kt, :], wb[:, kt, c0:c0 + 512], start=(kt == 0), stop=(kt == KT - 1),
                            r=[hT, wb], w=[ps])
                vb = vbs.next()
                self.A("copy", vb[:], ps[:, :], r=[ps], w=[vb])
                self.dma("sp", dst[t0:t0 + 128, :], vb[:], r=[vb], w=[dname])
            ps = psf[4]
            for kt in range(KT):
                self.mm(ps[:, :], hT[:, kt, :], wb[:, kt, 3072:3584], start=(kt == 0), stop=(kt == KT - 1), r=[hT, wb], w=[ps])
            self.A("activation", og[:], ps[:, :], AF.Sigmoid, r=[ps], w=[og])
            self.dma("sp", self.ogB[t0:t0 + 128, :], og[:], r=[og], w=["ogB"])
            ps = psf[5]
            for kt in range(KT):
                self.mm(ps[:, 0:16], hT[:, kt, :], wb[:, kt, 3584:3600], start=(kt == 0), stop=(kt == KT - 1), r=[hT, wb], w=[ps])
            self.V("tensor_tensor", g[:], ps[:, 0:16], gb[:], ALU.add, r=[ps, gb], w=[g])
            for off in (4, 12):
                self.A("activation", e4[:], g[:, off:off + 4], AF.Exp, scale=-1.0, r=[g], w=[e4])
                self.A("activation", e4[:], e4[:], AF.Ln, bias=1.0, r=[e4], w=[e4])
                self.V("tensor_scalar", g[:, off:off + 4], e4[:], -1.0, None, ALU.mult, r=[e4], w=[g])
            self.dma("sp", self.gB[t0:t0 + 128, :], g[:], r=[g], w=["gB"])
            for half in range(2):
                ps = psf[2 + half]
                c0 = 1536 + half * 512
                for kt in range(KT):
                    self.mm(ps[:, :], hT[:, kt, :], wb[:, kt, c0:c0 + 512], start=(kt == 0), stop=(kt == KT - 1), r=[hT, wb], w=[ps])
                rb_ = rawbs.next()
                if half == 0:
                    self.A("copy", rb_[:], ps[:, :], r=[ps], w=[rb_])
                else:
                    self.V("tensor_copy", rb_[:], ps[:, :], r=[ps], w=[rb_])
                for cc in range(4):
                    self.tr(psb[half][:, cc * 128:(cc + 1) * 128], rb_[:, cc * 128:(cc + 1) * 128], self.ident_b[:],
                            r=[rb_, self.ident_b], w=[psb[half]])
                if half == 0:
                    self.V("tensor_copy", stg2[:, 0:4, :].rearrange("p a b -> p (a b)"), psb[0][:, 0:512], r=[psb[0]], w=[stg2])
                else:
                    self.A("copy", stg2[:, 4:8, :].rearrange("p a b -> p (a b)"), psb[1][:, 0:512], r=[psb[1]], w=[stg2])
            self.dma("sp", self.qkraw.rearrange("c p t -> p c t")[:, :, t0:t0 + 128], stg2[:], r=[stg2], w=["qkraw"])

    def compute_negm(self, scale):
        psf = self.psf
        self.tr(psf[0][0:16, 0:128], self.nmax[:, 0:16], self.ident_f[:], r=[self.nmax, self.ident_f], w=[psf[0]])
        mx = self.sb([16, 1], F32)
        self.V("tensor_reduce", mx[:], psf[0][0:16, 0:128], axis=AX.X, op=ALU.max, r=[psf[0]], w=[mx])
        dg = self.sb([16, 16], F32)
        self.V("tensor_scalar", dg[:], self.ident_f[0:16, 0:16], mx[:, 0:1], None, ALU.mult, r=[mx, self.ident_f], w=[dg])
        self.mm(psf[1][:, 0:16], self.ones_f[0:16, 0:128], dg[:], r=[self.ones_f, dg], w=[psf[1]])
        mxb = self.sb([128, 16], F32)
        self.V("tensor_copy", mxb[:], psf[1][:, 0:16], r=[psf[1]], w=[mxb])
        negm = self.sb([128, 8], F32)
        self.V("tensor_tensor", negm[:], mxb[:, 0:8], mxb[:, 8:16], ALU.mult, r=[mxb], w=[negm])
        self.A("activation", negm[:], negm[:], AF.Sqrt, r=[negm], w=[negm])
        self.V("tensor_scalar", negm[:], negm[:], -float(scale), None, ALU.mult, r=[negm], w=[negm])
        return negm

    def qblocks(self):
        blocks = []
        q0 = 0
        while q0 < self.TC:
            nq = min(512, self.TC - q0)
            blocks.append((q0, nq, list(range(self.NTC))))
            q0 += nq
        while q0 < self.T:
            nq = min(512, self.T - q0)
            blocks.append((q0, nq, list(range(self.NT))))
            q0 += nq
        return blocks

    def run_attn_jobs(self, jobs, pTs, st_rot):
        steps = [(job, ki, kt) for job in jobs for ki, kt in enumerate(job["ktiles"])]
        recs = {}

        def emit_S(idx):
            job, ki, kt = steps[idx]
            nq, kr = job["nq"], job["krow"]
            st = st_rot.next()
            self.mm(st[:, 0:nq], job["kT"][kr[0]:kr[1], kt * 128:(kt + 1) * 128], job["qT"][kr[0]:kr[1], job["q0"]:job["q0"] + nq],
                    r=[job["kT"], job["qT"]], w=[st])
            pT = pTs.next()
            self.A("activation", pT[:, 0:nq], st[:, 0:nq], AF.Exp, bias=job["negm_col"], scale=1.0, r=[st, job["ndep"]], w=[pT])
            recs[idx] = pT

        def emit_PV(idx):
            job, ki, kt = steps[idx]
            pT = recs.pop(idx)
            nq, dv = job["nq"], job["dv"]
            nsub = nq // 128
            first, last = ki == 0, ki == len(job["ktiles"]) - 1
            o_ps, s_ps = job["o_ps"], job["s_ps"]
            self.mm(o_ps[0:dv, 0:nq], job["v_fn"](kt), pT[:, 0:nq], start=first, stop=last, r=[pT, job["vdep"]], w=[o_ps])
            if job["sum_mode"] == "bc":
                self.mm(s_ps[:, 0:nq], self.ones_b[:, :], pT[:, 0:nq], start=first, stop=last, r=[pT, self.ones_b], w=[s_ps])
            if last:
                job["post"]()

        if not steps:
            return
        LA = 2
        for k in range(min(LA, len(steps))):
            emit_S(k)
        for idx in range(len(steps)):
            if idx + LA < len(steps):
                emit_S(idx + LA)
            emit_PV(idx)

    def rowsum_to_col(self, s_ps, nq, srow):
        nsub = nq // 128
        self.V("tensor_copy", srow[0:1, 0:nq], s_ps[0:1, 0:nq], r=[s_ps], w=[srow])
        for sub in range(nsub):
            self.mm(s_ps[:, sub:sub + 1], srow[0:1, sub * 128:(sub + 1) * 128], self.ones_f[0:1, 0:1], start=(sub == 0), stop=True,
                    r=[srow, self.ones_f], w=[s_ps], skip=True)

    def convert_expert_weights(self, l):
        R = NEXP * 128
        step = 512
        for j in range(6):
            for r0 in range(0, R, step):
                self.dma("pool", self.wbf[r0:r0 + step, j * 2048:(j + 1) * 2048], self.wexp[j][l * R + r0:l * R + r0 + step, :],
                         w=["wbf"])

    def attnA(self, l):
        i = l // 2
        T, NT = self.T, self.NT
        lam_init = 0.8 - 0.6 * math.exp(-0.3 * l)
        self.new_phase()
        psf = self.psf
        negm = self.compute_negm(0.125)
        self.convert_expert_weights(l)
        lrow = self.sb([128, 256], F32)
        self.dma("sp", lrow[:], self.ev_lambda[i:i + 1, :].partition_broadcast(128), w=[lrow])
        lp = self.sb([128, 2, 64], F32)
        l4 = lrow[:].rearrange("p (a d) -> p a d", d=64)
        self.V("tensor_tensor", lp[:, 0, :], l4[:, 0, :], l4[:, 1, :], ALU.mult, r=[lrow], w=[lp])
        self.V("tensor_tensor", lp[:, 1, :], l4[:, 2, :], l4[:, 3, :], ALU.mult, r=[lrow, lp], w=[lp])
        ls = self.sb([128, 2], F32)
        self.V("tensor_reduce", ls[:], lp[:], axis=AX.X, op=ALU.add, r=[lp], w=[ls])
        self.A("activation", ls[:], ls[:], AF.Exp, r=[ls], w=[ls])
        neglam = self.sb([128, 1], F32)
        self.V("tensor_tensor", neglam[:], ls[:, 1:2], ls[:, 0:1], ALU.subtract, r=[ls], w=[neglam])
        self.V("tensor_scalar", neglam[:], neglam[:], -lam_init, None, ALU.add, r=[neglam], w=[neglam])
        subg = self.sb([128, 128], F32)
        self.dma("sp", subg[:], self.ev_subln_g[i:i + 1, :].partition_broadcast(128), w=[subg])
        self.V("tensor_scalar", subg[:], subg[:], 1.0 - lam_init, None, ALU.mult, r=[subg], w=[subg])
        kTs = Rot([self.sb([128, T], BF16) for _ in range(2)])
        qTs = Rot([self.sb([128, T], BF16) for _ in range(2)])
        vhs = Rot([self.sb([128, NT, 128], BF16) for _ in range(2)])
        pTs = Rot([self.sb([128, 512], BF16) for _ in range(5)])
        st_rot = Rot([psf[4], psf[5], self.psb[0].bitcast(F32), self.psb[1].bitcast(F32)])
        r1 = self.sb([128, 512], F32)
        r2 = self.sb([128, 512], F32)
        t1 = self.sb([128, 512], F32)
        t2 = self.sb([128, 512], F32)
        outb = Rot([self.sb([128, 512], BF16) for _ in range(2)])
        subgc = self.sb([128, 1], F32)
        self.dma("sp", subgc[:], self.ev_subln_g[i:i + 1, :].rearrange("o d -> d o"), w=[subgc])
        self.V("tensor_scalar", subgc[:], subgc[:], 1.0 - lam_init, None, ALU.mult, r=[subgc], w=[subgc])
        srows = Rot([self.sb([1, 512], F32) for _ in range(2)])
        jobs = []
        for h in range(4):
            kT, qT, vh = kTs.next(), qTs.next(), vhs.next()
            def load(h=h, kT=kT, qT=qT, vh=vh):
                self.dma("sp", kT[:], self.kTA[h], r=["kTA"], w=[kT])
                self.dma("sp", qT[:], self.qTA[h], r=["qTA"], w=[qT])
                self.dma("sp", vh[:], self.vA.rearrange("(n p) f -> p n f", p=128)[:, :, h * 128:(h + 1) * 128], r=["vA"], w=[vh])
            load()
            for (q0, nq, ktiles) in self.qblocks():
                nsub = nq // 128

                def post(h=h, q0=q0, nq=nq, nsub=nsub):
                    V = self.V
                    V("reciprocal", r1[:, 0:nq], psf[1][:, 0:nq], r=[psf[1]], w=[r1])
                    V("reciprocal", r2[:, 0:nq], psf[3][:, 0:nq], r=[psf[3]], w=[r2])
                    V("tensor_tensor", t1[:, 0:nq], psf[0][:, 0:nq], r1[:, 0:nq], ALU.mult, r=[psf[0], r1], w=[t1])
                    V("scalar_tensor_tensor", t2[:, 0:nq], psf[2][:, 0:nq], neglam[:, 0:1], r2[:, 0:nq], ALU.mult, ALU.mult,
                      r=[psf[2], neglam, r2], w=[t2])
                    V("tensor_tensor", t1[:, 0:nq], t1[:, 0:nq], t2[:, 0:nq], ALU.add, r=[t1, t2], w=[t1])
                    V("tensor_tensor", t2[:, 0:nq], t1[:, 0:nq], t1[:, 0:nq], ALU.mult, r=[t1, t2], w=[t2])
                    self.mm(psf[1][:, 0:nq], self.ones_f[:, :], t2[:, 0:nq], r=[self.ones_f, t2], w=[psf[1]])
                    V("tensor_scalar", r1[:, 0:nq], psf[1][:, 0:nq], 1.0 / 128, EPS, ALU.mult, ALU.add, r=[psf[1], r1], w=[r1])
                    self.A("activation", r1[:, 0:nq], r1[:, 0:nq], AF.Sqrt, r=[r1], w=[r1])
                    V("reciprocal", r1[:, 0:nq], r1[:, 0:nq], r=[r1], w=[r1])
                    V("tensor_tensor", t1[:, 0:nq], t1[:, 0:nq], r1[:, 0:nq], ALU.mult, r=[t1, r1], w=[t1])
                    ob = outb.next()
                    self.A("activation", ob[:, 0:nq], t1[:, 0:nq], AF.Copy, scale=subgc[:, 0:1], r=[t1, subgc], w=[ob])
                    self.dma("sp", self.catT[h][:, q0:q0 + nq], ob[:, 0:nq], r=[ob], w=["catT"])

                for c in range(2):
                    jobs.append(dict(kT=kT, qT=qT, krow=(c * 64, (c + 1) * 64), v_fn=(lambda kt, vh=vh: vh[:, kt, :]), vdep=vh, dv=128,
                                     q0=q0, nq=nq, ktiles=ktiles, negm_col=negm[:, h * 2 + c:h * 2 + c + 1], ndep=negm,
                                     o_ps=psf[c * 2], s_ps=psf[c * 2 + 1], sum_mode="bc", post=(post if c == 1 else (lambda: None))))
            self.run_attn_jobs(jobs, pTs, st_rot)
            jobs = []


def rope_tables(TL):
    n_rows = TL // 64
    row = np.repeat(np.arange(n_rows, dtype=np.float32), 64)
    col = np.tile(np.arange(64, dtype=np.float32), n_rows)
    quarter = 16
    inv_freq = (10000.0 ** (-np.arange(quarter, dtype=np.float32) / quarter)).astype(np.float32)
    ang = np.concatenate([row[:, None] * inv_freq, col[:, None] * inv_freq], -1).astype(np.float32)
    return np.cos(ang).astype(np.float32), np.sin(ang).astype(np.float32)


_SHARED_CACHE = {}


def prep_shared(inp, TL):
    f = np.float32
    sh = {}
    sh["w_mod"] = np.ascontiguousarray(inp["w_mod"], f)
    sh["bmodT"] = np.ascontiguousarray(inp["b_mod"].reshape(DEPTH, 48, 128).transpose(0, 2, 1), f)
    sh["ln_g"] = np.ascontiguousarray(inp["ln_g"].reshape(DEPTH * 2, D), f)
    sh["ln_b"] = np.ascontiguousarray(inp["ln_b"].reshape(DEPTH * 2, D), f)
    sh["ev_w_in"] = np.ascontiguousarray(inp["ev_w_in"], f)
    sh["ev_w_out"] = np.ascontiguousarray(inp["ev_w_out"], f)
    sh["ev_lambda"] = np.ascontiguousarray(inp["ev_lambda"].reshape(2, 256), f)
    sh["ev_subln_g"] = np.ascontiguousarray(inp["ev_subln_g"], f)
    cw = inp["ev_conv_w"]
    sh["convwT"] = np.ascontiguousarray(cw.reshape(2, 3, 8, 128).transpose(0, 3, 2, 1).reshape(2, 128, 24), f)
    sh["convbT"] = np.ascontiguousarray(inp["ev_conv_b"].reshape(2, 8, 128).transpose(0, 2, 1), f)
    sh["ev_gate_b"] = np.ascontiguousarray(inp["ev_gate_b"].reshape(2, 16), f)
    sh["ev_mnorm_g"] = np.ascontiguousarray(inp["ev_mnorm_g"], f)
    sh["od_w_in"] = np.ascontiguousarray(inp["od_w_in"], f)
    sh["od_w_out"] = np.ascontiguousarray(inp["od_w_out"], f)
    sh["od_decay"] = np.ascontiguousarray(inp["od_decay"].reshape(2, 8), f)
    sh["od_qk_g"] = np.ascontiguousarray(inp["od_qk_g"].reshape(2, 128), f)
    sh["wr"] = np.ascontiguousarray(np.concatenate([inp["moe_w_group"], inp["moe_w_router"]], -1), f)
    sh["br"] = np.ascontiguousarray(np.concatenate([inp["moe_b_group"], inp["moe_b_router"]], -1), f)
    wg = inp["moe_w_gate"].reshape(DEPTH, NEXP, 8, 128, 512)
    wu = inp["moe_w_up"].reshape(DEPTH, NEXP, 8, 128, 512)
    wd = inp["moe_w_down"].reshape(DEPTH, NEXP, 4, 128, 1024)
    def lay(w, k0, k1):
        return np.ascontiguousarray(w[:, :, k0:k1].transpose(0, 1, 3, 2, 4).reshape(DEPTH * NEXP * 128, 2048), f)
    sh["wexp0"] = lay(wg, 0, 4)
    sh["wexp1"] = lay(wg, 4, 8)
    sh["wexp2"] = lay(wu, 0, 4)
    sh["wexp3"] = lay(wu, 4, 8)
    sh["wexp4"] = lay(wd, 0, 2)
    sh["wexp5"] = lay(wd, 2, 4)
    c, s = rope_tables(TL)
    sh["rope_cos"], sh["rope_sin"] = c, s
    return sh


def prep_core(inp, b):
    f = np.float32
    d = {}
    d["xin"] = np.ascontiguousarray(np.concatenate([inp["ctx"][b], inp["x"][b]], 0), f)
    c2 = np.stack([inp["c"][b], inp["c_ctx"]], 0)
    d["cT"] = np.ascontiguousarray(c2.reshape(2, 8, 128).transpose(2, 1, 0).reshape(128, 16), f)
    return d


def _mlstm(self, l):
    i = l // 2
    T, NT, NTC = self.T, self.NT, self.NTC
    self.new_phase()
    psf, psb = self.psf, self.psb
    V, A, G, mm = self.V, self.A, self.G, self.mm
    lnscale = -0.5 * math.log(128.0)
    qT = self.sb([128, 4, T], BF16)
    kT = self.sb([128, 4, T], BF16)
    cw = self.sb([128, 24], F32)
    cb = self.sb([128, 8], F32)
    self.dma("sp", cw[:], self.convwT[i], w=[cw])
    self.dma("sp", cb[:], self.convbT[i], w=[cb])
    PIECE = 1024
    rbs = Rot([self.sb([128, PIECE + 2], BF16) for _ in range(2)])
    accs = Rot([self.sb([128, PIECE], F32) for _ in range(2)])
    for cc in range(8):
        dstT = qT if cc < 4 else kT
        for (s0, s1) in ((0, self.TC), (self.TC, T)):
            a = s0
            while a < s1:
                b = min(s1, a + PIECE)
                n = b - a
                rb = rbs.next()
                lo = a - 1 if a > s0 else a
                hi = b + 1 if b < s1 else b
                if a == s0:
                    V("memset", rb[:, 0:1], 0.0, w=[rb])
                if b == s1:
                    V("memset", rb[:, n + 1:n + 2], 0.0, w=[rb])
                self.dma("sp", rb[:, 1 - (a - lo):1 + n + (hi - b)], self.qkraw[cc][:, lo:hi], r=["qkraw"], w=[rb])
                acc = accs.next()
                V("tensor_scalar", acc[:, 0:n], rb[:, 0:n], cw[:, cc * 3:cc * 3 + 1], None, ALU.mult, r=[rb, cw], w=[acc])
                V("scalar_tensor_tensor", acc[:, 0:n], rb[:, 1:n + 1], cw[:, cc * 3 + 1:cc * 3 + 2], acc[:, 0:n], ALU.mult, ALU.add,
                  r=[rb, cw, acc], w=[acc])
                V("scalar_tensor_tensor", acc[:, 0:n], rb[:, 2:n + 2], cw[:, cc * 3 + 2:cc * 3 + 3], acc[:, 0:n], ALU.mult, ALU.add,
                  r=[rb, cw, acc], w=[acc])
                A("activation", dstT[:, cc % 4, a:b], acc[:, 0:n], AF.Silu, bias=cb[:, cc:cc + 1], r=[acc, cb], w=[dstT])
                a = b
    vp = self.sb([128, NT, 4, 129], BF16)
    V("memset", vp[:, :, :, 128:129], 1.0, w=[vp])
    for n in range(NT):
        self.dma("sp", vp[:, n, :, 0:128], self.vB[n * 128:(n + 1) * 128, :].rearrange("p (h d) -> p h d", d=128), r=["vB"], w=[vp])
    gt = self.sb([128, NT, 16], F32)
    self.dma("sp", gt[:], self.gB.rearrange("(n p) g -> p n g", p=128), r=["gB"], w=[gt])
    mg = self.sb([128, 512], F32)
    self.dma("sp", mg[:], self.ev_mnorm_g[i:i + 1, :].partition_broadcast(128), w=[mg])
    S32 = self.sb([128, 4, 129], F32)
    Sb = self.sb([128, 4, 129], BF16)
    lfbcs = Rot([self.sb([128, 128], F32) for _ in range(4)])
    colterm = self.sb([128, 4], F32)
    X = self.sb([128, 4, 128], F32)
    At = self.sb([128, 4, 128], BF16)
    eB = self.sb([128, 4, 128], F32)
    qeb = self.sb([128, 4, 128], BF16)
    wk = self.sb([128, 4], F32)
    kw = self.sb([128, 4, 128], BF16)
    dn = self.sb([128, 4], F32)
    hout = self.sb([128, 4, 128], F32)
    hbs = Rot([self.sb([128, 512], F32) for _ in range(2)])
    ogs = Rot([self.sb([128, 512], F32) for _ in range(2)])
    st4 = self.sb([128, 4], F32)
    sq = self.sb([128, 4, 128], F32)
    catb = Rot([self.sb([128, 512], BF16) for _ in range(2)])
    BR, KQ, ND0, ND1 = psf[0], psf[2], psf[3], psf[4]
    BR3 = BR[:, :].rearrange("p (h t) -> p h t", t=128)
    KTp = psb[0]
    UPs = [psf[5][:, 0:258], psf[1][:, 128:386]]
    UPt = [psf[5], psf[1]]
    NDs = [ND0, ND1]
    for direction in ("bwd", "fwd"):
        fwd = direction == "fwd"
        V("memset", S32[:], 0.0, w=[S32])
        V("memset", Sb[:], 0.0, w=[Sb])
        order = list(range(NT)) if fwd else (list(range(NTC - 1, -1, -1)) + list(range(NT - 1, NTC - 1, -1)))
        tri = self.triU_f if fwd else self.triL_f
        e_idx = 127 if fwd else 0
        lic, lfc = (0, 4) if fwd else (8, 12)
        for c in order:
            tok0, tok1 = c * 128, (c + 1) * 128
            for h in range(4):
                lfbc = lfbcs.next()
                V("tensor_scalar", lfbc[:], self.ones_f[:], gt[:, c, lfc + h:lfc + h + 1], None, ALU.mult, r=[self.ones_f, gt], w=[lfbc])
                mm(BR[:, h * 128:(h + 1) * 128], lfbc[:], tri[:], r=[lfbc, tri], w=[BR])
            mm(psf[1][:, 0:4], tri[:], gt[:, c, lfc:lfc + 4], r=[tri, gt], w=[psf[1]])
            V("scalar_tensor_tensor", colterm[:], gt[:, c, lic:lic + 4], lnscale, psf[1][:, 0:4], ALU.add, ALU.subtract,
              r=[gt, psf[1]], w=[colterm])
            V("tensor_tensor", X[:], BR3, colterm[:].unsqueeze(2).broadcast_to([128, 4, 128]), ALU.add, r=[BR, colterm], w=[X])
            A("activation", X[:], X[:], AF.Exp, r=[X], w=[X])
            G("tensor_tensor", X[:], X[:], tri[:].unsqueeze(1).broadcast_to([128, 4, 128]), ALU.mult, r=[X, tri], w=[X])
            for h in range(4):
                mm(KQ[:, h * 128:(h + 1) * 128], kT[:, h, tok0:tok1], qT[:, h, tok0:tok1], r=[kT, qT], w=[KQ])
            V("tensor_tensor", At[:], KQ[:, :].rearrange("p (h t) -> p h t", t=128), X[:], ALU.mult, r=[KQ, X], w=[At])
            A("activation", eB[:], BR3, AF.Exp, r=[BR], w=[eB])
            G("tensor_tensor", qeb[:], qT[:, :, tok0:tok1], eB[:], ALU.mult, r=[qT, eB], w=[qeb])
            V("tensor_tensor", wk[:], colterm[:], BR3[:, :, e_idx], ALU.add, r=[colterm, BR], w=[wk])
            A("activation", wk[:], wk[:], AF.Exp, r=[wk], w=[wk])
            for h in range(4):
                self.tr(KTp[:, h * 128:(h + 1) * 128], kT[:, h, tok0:tok1], self.ident_b[:], r=[kT, self.ident_b], w=[KTp])
            V("tensor_tensor", kw[:], KTp[:, 0:512].rearrange("p (h d) -> p h d", d=128), wk[:].unsqueeze(2).broadcast_to([128, 4, 128]),
              ALU.mult, r=[KTp, wk], w=[kw])
            for h in range(4):
                nd = NDs[h // 2][:, (h % 2) * 129:(h % 2 + 1) * 129]
                mm(nd, At[:, h, :], vp[:, c, h, :], start=(h % 2 == 0), stop=False, r=[At, vp], w=[NDs[h // 2]], skip=True)
                mm(nd, qeb[:, h, :], Sb[:, h, :], start=False, stop=True, r=[qeb, Sb], w=[NDs[h // 2]], skip=True)
            for b2 in range(2):
                nd3 = NDs[b2][:, 0:258].rearrange("p (j e) -> p j e", e=129)
                A("activation", dn[:, b2 * 2:b2 * 2 + 2], nd3[:, :, 128], AF.Abs, r=[NDs[b2]], w=[dn])
            V("tensor_scalar", dn[:], dn[:], 1.0, None, ALU.max, r=[dn], w=[dn])
            V("reciprocal", dn[:], dn[:], r=[dn], w=[dn])
            for b2 in range(2):
                nd3 = NDs[b2][:, 0:258].rearrange("p (j e) -> p j e", e=129)
                V("tensor_tensor", hout[:, b2 * 2:b2 * 2 + 2, :], nd3[:, :, 0:128],
                  dn[:, b2 * 2:b2 * 2 + 2].unsqueeze(2).broadcast_to([128, 2, 128]), ALU.mult, r=[NDs[b2], dn], w=[hout])
            hflat = hout[:].rearrange("p h d -> p (h d)")
            if not fwd:
                self.dma("sp", self.hB[tok0:tok1, :], hflat, r=[hout], w=["hB"])
            else:
                hb, og = hbs.next(), ogs.next()
                self.dma("sp", hb[:], self.hB[tok0:tok1, :], r=["hB"], w=[hb])
                self.dma("sp", og[:], self.ogB[tok0:tok1, :], r=["ogB"], w=[og])
                V("tensor_tensor", hflat, hflat, hb[:], ALU.add, r=[hout, hb], w=[hout])
                V("tensor_tensor", hflat, hflat, og[:], ALU.mult, r=[hout, og], w=[hout])
                V("tensor_reduce", st4[:], hout[:], axis=AX.X, op=ALU.add, r=[hout], w=[st4])
                V("tensor_scalar", st4[:], st4[:], 1.0 / 128, None, ALU.mult, r=[st4], w=[st4])
                V("tensor_tensor", hout[:], hout[:], st4[:].unsqueeze(2).broadcast_to([128, 4, 128]), ALU.subtract, r=[hout, st4], w=[hout])
                G("tensor_tensor", sq[:], hout[:], hout[:], ALU.mult, r=[hout], w=[sq])
                V("tensor_reduce", st4[:], sq[:], axis=AX.X, op=ALU.add, r=[sq], w=[st4])
                V("tensor_scalar", st4[:], st4[:], 1.0 / 128, EPS, ALU.mult, ALU.add, r=[st4], w=[st4])
                A("activation", st4[:], st4[:], AF.Sqrt, r=[st4], w=[st4])
                V("reciprocal", st4[:], st4[:], r=[st4], w=[st4])
                V("tensor_tensor", hout[:], hout[:], st4[:].unsqueeze(2).broadcast_to([128, 4, 128]), ALU.mult, r=[hout, st4], w=[hout])
                cbt = catb.next()
                V("tensor_tensor", cbt[:], hflat, mg[:], ALU.mult, r=[hout, mg], w=[cbt])
                self.dma("sp", self.cat[tok0:tok1, 512:1024], cbt[:], r=[cbt], w=["cat"])
            for h in range(4):
                up = UPs[h // 2][:, (h % 2) * 129:(h % 2 + 1) * 129]
                mm(up, kw[:, h, :], vp[:, c, h, :], start=(h % 2 == 0), stop=True, r=[kw, vp], w=[UPt[h // 2]], skip=True)
            for h in range(4):
                up = UPs[h // 2][:, (h % 2) * 129:(h % 2 + 1) * 129]
                V("scalar_tensor_tensor", S32[:, h, :], S32[:, h, :], eB[:, h, e_idx:e_idx + 1], up, ALU.mult, ALU.add,
                  r=[S32, eB, UPt[h // 2]], w=[S32])
            A("copy", Sb[:], S32[:], r=[S32], w=[Sb])


Builder.mlstm = _mlstm


def _ln_tile(self, z, st, junk, lng, lnb, out):
    V, A = self.V, self.A
    V("tensor_reduce", st[:, 0:1], z[:], axis=AX.X, op=ALU.add, r=[z], w=[st])
    V("tensor_scalar", st[:, 0:1], st[:, 0:1], 1.0 / D, None, ALU.mult, r=[st], w=[st])
    V("tensor_scalar", z[:], z[:], st[:, 0:1], None, ALU.subtract, r=[z, st], w=[z])
    V("memset", st[:, 1:2], 0.0, r=[st], w=[st])
    A("activation", junk[:], z[:], AF.Square, accum_out=st[:, 1:2], r=[z, st], w=[junk, st])
    V("tensor_scalar", st[:, 1:2], st[:, 1:2], 1.0 / D, EPS, ALU.mult, ALU.add, r=[st], w=[st])
    A("activation", st[:, 1:2], st[:, 1:2], AF.Sqrt, r=[st], w=[st])
    V("reciprocal", st[:, 1:2], st[:, 1:2], r=[st], w=[st])
    V("scalar_tensor_tensor", out[:], z[:], st[:, 1:2], lng[:], ALU.mult, ALU.mult, r=[z, st, lng], w=[out])
    V("tensor_tensor", out[:], out[:], lnb[:], ALU.add, r=[out, lnb], w=[out])


Builder.ln_tile = _ln_tile


def _outproj(self, l, w_out_ap):
    T, NT, NTC = self.T, self.NT, self.NTC
    self.new_phase()
    psf, psb = self.psf, self.psb
    V, A, G, mm = self.V, self.A, self.G, self.mm
    mT = self.modT[l]
    wo = self.sb([128, KT, D], BF16)
    self.load_weight_bf16(wo, w_out_ap, D)
    s4p = self.sb([128, 8, 2], F32)
    V("tensor_scalar", s4p[:], mT[:, 32:40, :], 1.0, None, ALU.add, r=[mT], w=[s4p])
    g2, s4b, sh4b = [], [], []
    for r in range(2):
        t = self.sb([128, D], F32); self.bcast_row(self.mod_col(l, 2, r), t, r=[mT]); g2.append(t)
        t = self.sb([128, D], F32); self.bcast_row(lambda kt, r=r: s4p[:, kt, r:r + 1], t, r=[s4p]); s4b.append(t)
        t = self.sb([128, D], F32); self.bcast_row(self.mod_col(l, 3, r), t, r=[mT]); sh4b.append(t)
    lng = self.sb([128, D], F32)
    lnb = self.sb([128, D], F32)
    self.dma("sp", lng[:], self.ln_g[l * 2:l * 2 + 1, :].partition_broadcast(128), w=[lng])
    self.dma("sp", lnb[:], self.ln_b[l * 2:l * 2 + 1, :].partition_broadcast(128), w=[lnb])
    wrt = self.sb([128, KT, 36], F32)
    self.dma("sp", wrt[:], self.wr[l].rearrange("(kt p) n -> p kt n", p=128), w=[wrt])
    brb = self.sb([128, 36], F32)
    self.dma("sp", brb[:], self.br[l:l + 1, :].partition_broadcast(128), w=[brb])
    catts = Rot([self.sb([128, D], BF16) for _ in range(2)])
    catTs = Rot([self.sb([128, KT, 128], BF16) for _ in range(2)])
    xts = Rot([self.sb([128, D], F32) for _ in range(2)])
    zs = Rot([self.sb([128, D], F32) for _ in range(2)])
    xms = Rot([self.sb([128, D], F32) for _ in range(2)])
    h2s = Rot([self.sb([128, D], F32) for _ in range(2)])
    h2bs = Rot([self.sb([128, D], BF16) for _ in range(2)])
    h2T = self.sb([128, KT, 128], F32)
    junk = self.sb([128, D], F32)
    st = self.sb([128, 2], F32)
    lg = self.sb([128, 36], F32)
    sm = self.sb([128, 16], F32)
    g1h = self.sb([128, 4], F32)
    ge = self.sb([128, 4], F32)
    tmp48 = self.sb([128, 4, 8], F32)
    esel = self.sb([128, 8], F32)
    oh1 = self.sb([128, 8], F32)
    oh2 = self.sb([128, 8], F32)
    msk = self.sb([128, 8], F32)
    xsrc = self.x_src(l)
    for ti in range(NT):
        t0 = ti * 128
        r = 1 if ti < NTC else 0
        fm0 = 0 if l % 2 == 0 else 4
        tm0 = 4 - fm0
        catt = catts.next()
        self.dma("sp", catt[:, 0:512], self.cat[t0:t0 + 128, tm0 * 128:tm0 * 128 + 512], r=["cat"], w=[catt])
        catT = catTs.next()
        self.dma("sp", catT[:, fm0:fm0 + 4, :], self.catT[fm0:fm0 + 4].rearrange("c p t -> p c t")[:, :, t0:t0 + 128], r=["catT"], w=[catT])
        for k4 in range(4):
            self.tr(psb[0][:, k4 * 128:(k4 + 1) * 128], catt[:, k4 * 128:(k4 + 1) * 128], self.ident_b[:], r=[catt, self.ident_b], w=[psb[0]])
        A("copy", catT[:, tm0:tm0 + 4, :].rearrange("p a b -> p (a b)"), psb[0][:, 0:512], r=[psb[0]], w=[catT])
        for half in range(2):
            for kt in range(KT):
                mm(psf[half][:, :], catT[:, kt, :], wo[:, kt, half * 512:(half + 1) * 512], start=(kt == 0), stop=(kt == KT - 1),
                   r=[catT, wo], w=[psf[half]])
        xt = xts.next()
        self.dma("sp", xt[:], xsrc[t0:t0 + 128, :], r=["X"], w=[xt])
        z = zs.next()
        for half in range(2):
            V("tensor_tensor", z[:, half * 512:(half + 1) * 512], psf[half][:, :], g2[r][:, half * 512:(half + 1) * 512], ALU.mult,
              r=[psf[half], g2[r]], w=[z])
        V("scalar_tensor_tensor", z[:], xt[:], ALPHA, z[:], ALU.mult, ALU.add, r=[xt, z], w=[z])
        xm = xms.next()
        self.ln_tile(z, st, junk, lng, lnb, xm)
        self.dma("sp", self.X[t0:t0 + 128, :], xm[:], r=[xm], w=["X"])
        h2 = h2s.next()
        V("tensor_tensor", h2[:], xm[:], s4b[r][:], ALU.mult, r=[xm, s4b[r]], w=[h2])
        G("tensor_tensor", h2[:], h2[:], sh4b[r][:], ALU.add, r=[h2, sh4b[r]], w=[h2])
        h2b = h2bs.next()
        A("copy", h2b[:], h2[:], r=[h2], w=[h2b])
        self.dma("sp", self.H2[t0:t0 + 128, :], h2b[:], r=[h2b], w=["H2"])
        for kt in range(KT):
            self.tr(psf[2 + kt // 4][:, (kt % 4) * 128:(kt % 4 + 1) * 128], h2[:, kt * 128:(kt + 1) * 128], self.ident_f[:],
                    r=[h2, self.ident_f], w=[psf[2 + kt // 4]])
        A("copy", h2T[:, 0:4, :].rearrange("p a b -> p (a b)"), psf[2][:, :], r=[psf[2]], w=[h2T])
        V("tensor_copy", h2T[:, 4:8, :].rearrange("p a b -> p (a b)"), psf[3][:, :], r=[psf[3]], w=[h2T])
        for kt in range(KT):
            mm(psf[4][:, 0:36], h2T[:, kt, :], wrt[:, kt, :], start=(kt == 0), stop=(kt == KT - 1), r=[h2T, wrt], w=[psf[4]])
        V("tensor_tensor", lg[:], psf[4][:, 0:36], brb[:], ALU.add, r=[psf[4], brb], w=[lg])
        V("tensor_reduce", sm[:, 0:1], lg[:, 0:4], axis=AX.X, op=ALU.max, r=[lg], w=[sm])
        V("tensor_scalar", g1h[:], lg[:, 0:4], sm[:, 0:1], None, ALU.is_equal, r=[lg, sm], w=[g1h])
        V("tensor_scalar", sm[:, 1:2], sm[:, 0:1], -1.0, None, ALU.mult, r=[sm], w=[sm])
        V("memset", sm[:, 2:3], 0.0, r=[sm], w=[sm])
        A("activation", ge[:], lg[:, 0:4], AF.Exp, bias=sm[:, 1:2], accum_out=sm[:, 2:3], r=[lg, sm], w=[ge, sm])
        V("reciprocal", sm[:, 3:4], sm[:, 2:3], r=[sm], w=[sm])
        V("tensor_tensor", tmp48[:], lg[:, 4:36].rearrange("p (g e) -> p g e", e=8), g1h[:].unsqueeze(2).broadcast_to([128, 4, 8]),
          ALU.mult, r=[lg, g1h], w=[tmp48])
        V("tensor_reduce", esel[:], tmp48[:].rearrange("p g e -> p e g"), axis=AX.X, op=ALU.add, r=[tmp48], w=[esel])
        V("tensor_reduce", sm[:, 4:5], esel[:], axis=AX.X, op=ALU.max, r=[esel], w=[sm])
        V("tensor_scalar", oh1[:], esel[:], sm[:, 4:5], None, ALU.is_equal, r=[esel, sm], w=[oh1])
        V("scalar_tensor_tensor", msk[:], oh1[:], NEG_BIG, esel[:], ALU.mult, ALU.add, r=[oh1, esel], w=[msk])
        V("tensor_reduce", sm[:, 5:6], msk[:], axis=AX.X, op=ALU.max, r=[msk], w=[sm])
        V("tensor_scalar", oh2[:], msk[:], sm[:, 5:6], None, ALU.is_equal, r=[msk, sm], w=[oh2])
        V("tensor_tensor", sm[:, 6:7], sm[:, 5:6], sm[:, 4:5], ALU.subtract, r=[sm], w=[sm])
        A("activation", sm[:, 6:7], sm[:, 6:7], AF.Exp, r=[sm], w=[sm])
        V("tensor_scalar", sm[:, 7:8], sm[:, 6:7], 1.0, None, ALU.add, r=[sm], w=[sm])
        V("reciprocal", sm[:, 7:8], sm[:, 7:8], r=[sm], w=[sm])
        V("tensor_tensor", sm[:, 8:9], sm[:, 6:7], sm[:, 7:8], ALU.mult, r=[sm], w=[sm])
        V("tensor_scalar", self.wts[:, ti, 0:1], sm[:, 7:8], sm[:, 3:4], None, ALU.mult, r=[sm], w=[self.wts])
        V("tensor_scalar", self.wts[:, ti, 1:2], sm[:, 8:9], sm[:, 3:4], None, ALU.mult, r=[sm, self.wts], w=[self.wts])
        for k, oh in ((0, oh1), (1, oh2)):
            V("tensor_tensor", self.Msel[:, ti, k, :].rearrange("p (g e) -> p g e", e=8), g1h[:].unsqueeze(2).broadcast_to([128, 4, 8]),
              oh[:].unsqueeze(1).broadcast_to([128, 4, 8]), ALU.mult, r=[g1h, oh, self.Msel], w=[self.Msel])


Builder.outproj = _outproj


def _moe(self, l, last):
    T, NT, NTC = self.T, self.NT, self.NTC
    NB = self.NB
    self.new_phase()
    psf, psb = self.psf, self.psb
    V, A, G, mm = self.V, self.A, self.G, self.mm
    mT = self.modT[l]
    Msel, wts = self.Msel, self.wts
    Mb = self.sb([128, NT, 32], BF16)
    V("tensor_tensor", Mb[:], Msel[:, :, 0, :], Msel[:, :, 1, :], ALU.add, r=[Msel], w=[Mb])
    for ti in range(NT):
        mm(psf[0][0:32, 0:1], Mb[:, ti, :], self.ones_b[:, 0:1], start=(ti == 0), stop=(ti == NT - 1), r=[Mb, self.ones_b], w=[psf[0]])
    cc = self.sb([32, 4], F32)
    V("tensor_scalar", cc[:, 0:1], psf[0][0:32, 0:1], 1.0 / 128, 0.49609375, ALU.mult, ALU.add, r=[psf[0]], w=[cc])
    nbi = self.sb([32, 1], I32)
    V("tensor_copy", nbi[:], cc[:, 0:1], r=[cc], w=[nbi])
    V("tensor_copy", cc[:, 1:2], nbi[:], r=[nbi, cc], w=[cc])
    V("tensor_scalar", cc[:, 2:3], cc[:, 1:2], 128.0, None, ALU.mult, r=[cc], w=[cc])
    pbc = self.sb([32, 128], F32)
    V("tensor_scalar", pbc[:], self.ones_f[0:32, :], cc[:, 2:3], None, ALU.mult, r=[self.ones_f, cc], w=[pbc])
    mm(psf[1][:, 0:32], pbc[:], self.sU_f[0:32, 0:32], r=[pbc, self.sU_f], w=[psf[1]])
    mm(psf[1][:, 32:64], pbc[:], self.ident_f[0:32, 0:32], start=False, r=[pbc, self.ident_f], w=[psf[1]], skip=True)
    stb = self.sb([128, 64], F32)
    V("tensor_copy", stb[:], psf[1][:, 0:64], r=[psf[1]], w=[stb])
    endb = self.sb([128, 32], F32)
    V("tensor_tensor", endb[:], stb[:, 0:32], stb[:, 32:64], ALU.add, r=[stb], w=[endb])
    tot = self.sb([128, 32], F32)
    V("memset", tot[:], 0.0, w=[tot])
    sr = self.sb([128, 32], F32)
    pr = self.sb([128, 32], F32)
    destf = self.sb([128, NT, 2], F32)
    for ti in range(NT):
        mm(psf[2][:, 0:32], self.sU_b[:], Mb[:, ti, :], r=[self.sU_b, Mb], w=[psf[2]])
        mm(psf[3][:, 0:32], self.ones_b[:], Mb[:, ti, :], r=[self.ones_b, Mb], w=[psf[3]])
        V("tensor_tensor", sr[:], psf[2][:, 0:32], tot[:], ALU.add, r=[psf[2], tot], w=[sr])
        V("tensor_tensor", sr[:], sr[:], stb[:, 0:32], ALU.add, r=[sr, stb], w=[sr])
        V("tensor_tensor", tot[:], tot[:], psf[3][:, 0:32], ALU.add, r=[tot, psf[3]], w=[tot])
        for k in range(2):
            V("tensor_tensor", pr[:], Msel[:, ti, k, :], sr[:], ALU.mult, r=[Msel, sr], w=[pr])
            V("tensor_reduce", destf[:, ti, k:k + 1], pr[:], axis=AX.X, op=ALU.add, r=[pr, destf], w=[destf])
    desti = self.sb([128, NT, 2], I32)
    V("tensor_copy", desti[:], destf[:], r=[destf], w=[desti])
    bst = self.sb([128, NB], F32)
    G("iota", bst[:], [[128, NB]], base=0, channel_multiplier=0, allow_small_or_imprecise_dtypes=True, w=[bst])
    cmp = self.sb([128, NB, 32], F32)
    V("tensor_tensor", cmp[:], endb[:].unsqueeze(1).broadcast_to([128, NB, 32]), bst[:].unsqueeze(2).broadcast_to([128, NB, 32]),
      ALU.is_le, r=[endb, bst], w=[cmp])
    bex = self.sb([128, NB], F32)
    V("tensor_reduce", bex[:], cmp[:], axis=AX.X, op=ALU.add, r=[cmp], w=[bex])
    V("tensor_scalar", bex[:], bex[:], float(NEXP - 1), None, ALU.min, r=[bex], w=[bex])
    V("tensor_scalar", bex[:], bex[:], 128.0, self.pidx[:, 0:1], ALU.mult, ALU.add, r=[bex, self.pidx], w=[bex])
    widx = self.sb([128, NB], I32)
    V("tensor_copy", widx[:], bex[:], r=[bex], w=[widx])
    h2ts = Rot([self.sb([128, D], BF16) for _ in range(2)])
    for ti in range(NT):
        h2t = h2ts.next()
        self.dma("sp", h2t[:], self.H2[ti * 128:(ti + 1) * 128, :], r=["H2"], w=[h2t])
        for k in range(2):
            idx = desti[:, ti, k:k + 1]
            o = self.P.op("pool", lambda g, idx=idx, h2t=h2t: g.indirect_dma_start(
                out=self.XB[:, :], out_offset=bass.IndirectOffsetOnAxis(ap=idx, axis=0), in_=h2t[:], in_offset=None),
                reads=[desti, h2t, "XB"], writes=["XB"])
            o.is_dma = True
    wrot = Rot([self.sb([128, 12288], BF16) for _ in range(3)])
    xbs = Rot([self.sb([128, D], BF16) for _ in range(2)])
    xTs = Rot([self.sb([128, KT, 128], BF16) for _ in range(2)])
    sg = self.sb([128, 512], F32)
    hms = Rot([self.sb([128, 512], BF16) for _ in range(2)])
    hTs = Rot([self.sb([128, 4, 128], BF16) for _ in range(2)])
    ybs = Rot([self.sb([128, D], F32) for _ in range(2)])
    for b in range(NB):
        wt = wrot.next()
        idx = widx[:, b:b + 1]
        o = self.P.op("pool", lambda g, idx=idx, wt=wt: g.indirect_dma_start(
            out=wt[:], out_offset=None, in_=self.wbf[:, :], in_offset=bass.IndirectOffsetOnAxis(ap=idx, axis=0)),
            reads=[widx, "wbf"], writes=[wt])
        o.is_dma = True
        w6 = [wt[:, j * 2048:(j + 1) * 2048] for j in range(6)]
        xb = xbs.next()
        self.dma("sp", xb[:], self.XB[b * 128:(b + 1) * 128, :], r=["XB"], w=[xb])
        for kt in range(KT):
            self.tr(psb[0][:, kt * 128:(kt + 1) * 128], xb[:, kt * 128:(kt + 1) * 128], self.ident_b[:], r=[xb, self.ident_b], w=[psb[0]])
        xT = xTs.next()
        A("copy", xT[:, 0:4, :].rearrange("p a b -> p (a b)"), psb[0][:, 0:512], r=[psb[0]], w=[xT])
        V("tensor_copy", xT[:, 4:8, :].rearrange("p a b -> p (a b)"), psb[0][:, 512:1024], r=[psb[0]], w=[xT])
        for gi in range(2):
            for kt in range(KT):
                wsrc = w6[gi * 2 + kt // 4]
                c0 = (kt % 4) * 512
                mm(psf[gi][:, :], xT[:, kt, :], wsrc[:, c0:c0 + 512], start=(kt == 0), stop=(kt == KT - 1), r=[xT, wt], w=[psf[gi]])
        A("activation", sg[:], psf[0][:, :], AF.Silu, r=[psf[0]], w=[sg])
        hm = hms.next()
        V("tensor_tensor", hm[:], sg[:], psf[1][:, :], ALU.mult, r=[sg, psf[1]], w=[hm])
        for m in range(4):
            self.tr(psb[1][:, m * 128:(m + 1) * 128], hm[:, m * 128:(m + 1) * 128], self.ident_b[:], r=[hm, self.ident_b], w=[psb[1]])
        hT = hTs.next()
        A("copy", hT[:].rearrange("p a b -> p (a b)"), psb[1][:, 0:512], r=[psb[1]], w=[hT])
        for half in range(2):
            for m in range(4):
                wsrc = w6[4 + m // 2]
                c0 = (m % 2) * 1024 + half * 512
                mm(psf[2 + half][:, :], hT[:, m, :], wsrc[:, c0:c0 + 512], start=(m == 0), stop=(m == 3), r=[hT, wt], w=[psf[2 + half]])
        yb = ybs.next()
        A("copy", yb[:, 0:512], psf[2][:, :], r=[psf[2]], w=[yb])
        V("tensor_copy", yb[:, 512:1024], psf[3][:, :], r=[psf[3]], w=[yb])
        self.dma("sp", self.YB[b * 128:(b + 1) * 128, :], yb[:], r=[yb], w=["YB"])
    g5 = []
    for r in range(2):
        t = self.sb([128, D], F32); self.bcast_row(self.mod_col(l, 5, r), t, r=[mT]); g5.append(t)
    lng = self.sb([128, D], F32)
    lnb = self.sb([128, D], F32)
    self.dma("sp", lng[:], self.ln_g[l * 2 + 1:l * 2 + 2, :].partition_broadcast(128), w=[lng])
    self.dma("sp", lnb[:], self.ln_b[l * 2 + 1:l * 2 + 2, :].partition_broadcast(128), w=[lnb])
    y0s = Rot([self.sb([128, D], F32) for _ in range(2)])
    y1s = Rot([self.sb([128, D], F32) for _ in range(1)])
    xts = Rot([self.sb([128, D], F32) for _ in range(2)])
    xns = Rot([self.sb([128, D], F32) for _ in range(1)])
    junk = self.sb([128, D], F32)
    st = self.sb([128, 2], F32)
    for ti in range(NT):
        if last and ti < NTC:
            continue
        r = 1 if ti < NTC else 0
        ys = []
        for k, rot in ((0, y0s), (1, y1s)):
            yk = rot.next()
            idx = desti[:, ti, k:k + 1]
            o = self.P.op("pool", lambda g, idx=idx, yk=yk: g.indirect_dma_start(
                out=yk[:], out_offset=None, in_=self.YB[:, :], in_offset=bass.IndirectOffsetOnAxis(ap=idx, axis=0)),
                reads=[desti, "YB"], writes=[yk])
            o.is_dma = True
            ys.append(yk)
        xt = xts.next()
        self.dma("sp", xt[:], self.X[ti * 128:(ti + 1) * 128, :], r=["X"], w=[xt])
        y0, y1 = ys
        V("tensor_scalar", y0[:], y0[:], wts[:, ti, 0:1], None, ALU.mult, r=[y0, wts], w=[y0])
        V("scalar_tensor_tensor", y0[:], y1[:], wts[:, ti, 1:2], y0[:], ALU.mult, ALU.add, r=[y1, wts, y0], w=[y0])
        G("tensor_tensor", y0[:], y0[:], g5[r][:], ALU.mult, r=[y0, g5[r]], w=[y0])
        V("scalar_tensor_tensor", y0[:], xt[:], ALPHA, y0[:], ALU.mult, ALU.add, r=[xt, y0], w=[y0])
        xn = xns.next()
        self.ln_tile(y0, st, junk, lng, lnb, xn)
        if last:
            lt = ti - NTC
            self.dma("sp", self.yout[lt * 128:(lt + 1) * 128, :], xn[:], r=[xn], w=["yout"])
        else:
            self.dma("sp", self.X[ti * 128:(ti + 1) * 128, :], xn[:], r=[xn, "X"], w=["X"])


Builder.moe = _moe


def _proj_odd(self, l):
    i = l // 2
    NT, NTC = self.NT, self.NTC
    self.new_phase()
    psf, psb = self.psf, self.psb
    V, A, G, mm = self.V, self.A, self.G, self.mm
    wb = self.sb([128, KT, ODD_IN], BF16)
    self.load_weight_bf16(wb, self.od_w_in[i], ODD_IN)
    s1p = self.sb([128, 8, 2], F32)
    V("tensor_scalar", s1p[:], self.modT[l][:, 8:16, :], 1.0, None, ALU.add, r=[self.modT[l]], w=[s1p])
    sh = lambda kt, r: self.modT[l][:, kt, r:r + 1]
    qkg = self.sb([128, 128], F32)
    self.dma("sp", qkg[:], self.od_qk_g[i:i + 1, :].partition_broadcast(128), w=[qkg])
    V("memset", self.nmax[:], 0.0, w=[self.nmax])
    xts = Rot([self.sb([128, D], F32) for _ in range(2)])
    hTs = Rot([self.sb([128, KT, 128], BF16) for _ in range(2)])
    sqt = self.sb([128, 8, 64], F32)
    xn = self.sb([128, 8 * 64], F32)
    o = self.sb([128, 8, 64], F32)
    ta, tb = self.sb([128, 8, 32], F32), self.sb([128, 8, 32], F32)
    ss = self.sb([128, 8], F32)
    nrm = self.sb([128, 8], F32)
    obs = Rot([self.sb([128, 512], BF16) for _ in range(2)])
    stg = Rot([self.sb([128, 4, 128], BF16) for _ in range(2)])
    vbs = Rot([self.sb([128, 512], BF16) for _ in range(2)])
    og = self.sb([128, 512], F32)
    stg2 = self.sb([128, 4, 128], BF16)
    xsrc = self.x_src(l)

    def norm_rope(ps, c0, Gn, gcol, is_ctx, lt, nm_off, scale):
        p3 = ps[:, c0:c0 + Gn * 64].rearrange("p (g d) -> p g d", d=64)
        A("activation", sqt[:, 0:Gn, :], p3, AF.Square, r=[ps], w=[sqt])
        V("tensor_reduce", ss[:, 0:Gn], sqt[:, 0:Gn, :], axis=AX.X, op=ALU.add, r=[sqt], w=[ss])
        V("tensor_scalar", ss[:, 0:Gn], ss[:, 0:Gn], 1.0 / 64, EPS, ALU.mult, ALU.add, r=[ss], w=[ss])
        A("activation", ss[:, 0:Gn], ss[:, 0:Gn], AF.Sqrt, r=[ss], w=[ss])
        V("reciprocal", ss[:, 0:Gn], ss[:, 0:Gn], r=[ss], w=[ss])
        x3 = xn[:, 0:Gn * 64].rearrange("p (g d) -> p g d", d=64)
        V("tensor_tensor", x3, p3, ss[:, 0:Gn].unsqueeze(2).broadcast_to([128, Gn, 64]), ALU.mult, r=[ps, ss], w=[xn])
        G("tensor_tensor", x3, x3, qkg[:, gcol:gcol + 64].unsqueeze(1).broadcast_to([128, Gn, 64]), ALU.mult, r=[xn, qkg], w=[xn])
        if is_ctx:
            V("tensor_copy", o[:, 0:Gn, :], x3, r=[xn], w=[o])
        else:
            self.rope(xn, o[:, 0:Gn, :], lt, (ta, tb))
        V("tensor_tensor", sqt[:, 0:Gn, :], o[:, 0:Gn, :], o[:, 0:Gn, :], ALU.mult, r=[o], w=[sqt])
        V("tensor_reduce", nrm[:, 0:Gn], sqt[:, 0:Gn, :], axis=AX.X, op=ALU.add, r=[sqt], w=[nrm])
        V("tensor_tensor", self.nmax[:, nm_off:nm_off + Gn], self.nmax[:, nm_off:nm_off + Gn], nrm[:, 0:Gn], ALU.max,
          r=[nrm, self.nmax], w=[self.nmax])
        ob = obs.next()
        A("activation", ob[:, 0:Gn * 64], o[:, 0:Gn, :].rearrange("p g d -> p (g d)"), AF.Copy, scale=scale, r=[o], w=[ob])
        return ob

    for ti in range(NT):
        t0 = ti * 128
        is_ctx = ti < NTC
        xt = xts.next()
        self.dma("sp", xt[:], xsrc[t0:t0 + 128, :], r=["X"], w=[xt])
        hT = hTs.next()
        self.make_hT(l, ti, xt, hT, s1p, sh, [psf[0], psf[1]])

        def tokmajor(ps, c0, n):
            for kt in range(KT):
                mm(ps[:, 0:n], hT[:, kt, :], wb[:, kt, c0:c0 + n], start=(kt == 0), stop=(kt == KT - 1), r=[hT, wb], w=[ps])
        tokmajor(psf[2], 512, 512)
        vb = vbs.next()
        A("copy", vb[:], psf[2][:, :], r=[psf[2]], w=[vb])
        self.dma("sp", self.vB[t0:t0 + 128, :], vb[:], r=[vb], w=["vB"])
        tokmajor(psf[3], 1024, 512)
        A("activation", og[:], psf[3][:, :], AF.Silu, r=[psf[3]], w=[og])
        self.dma("sp", self.ogB[t0:t0 + 128, :], og[:], r=[og], w=["ogB"])
        tokmajor(psf[4], 1536, 512)
        ob = norm_rope(psf[4], 0, 8, 0, is_ctx, ti - NTC, 0, 0.125)
        for pr in range(4):
            self.tr(psb[0][:, pr * 128:(pr + 1) * 128], ob[:, pr * 128:(pr + 1) * 128], self.ident_b[:], r=[ob, self.ident_b], w=[psb[0]])
        st = stg.next()
        V("tensor_copy", st[:].rearrange("p a b -> p (a b)"), psb[0][:, 0:512], r=[psb[0]], w=[st])
        self.dma("sp", self.qTA.rearrange("h p t -> p h t")[:, :, t0:t0 + 128], st[:], r=[st], w=["qTA"])
        tokmajor(psf[5], 2048, 256)
        ob = norm_rope(psf[5], 0, 2, 64, is_ctx, ti - NTC, 8, 1.0)
        self.tr(psb[1][:, 0:128], ob[:, 0:128], self.ident_b[:], r=[ob, self.ident_b], w=[psb[1]])
        st = stg.next()
        V("tensor_copy", st[:, 0, :], psb[1][:, 0:128], r=[psb[1]], w=[st])
        self.dma("sp", self.kTA[0][:, t0:t0 + 128], st[:, 0, :], r=[st], w=["kTA"])
        vb = vbs.next()
        A("copy", vb[:, 0:128], psf[5][:, 128:256], r=[psf[5]], w=[vb])
        self.dma("sp", self.vA[t0:t0 + 128, 0:128], vb[:, 0:128], r=[vb], w=["vA"])
        tokmajor(psf[2], 0, 512)
        rb_ = obs.next()
        A("copy", rb_[:], psf[2][:, :], r=[psf[2]], w=[rb_])
        for cc in range(4):
            self.tr(psb[0][:, cc * 128:(cc + 1) * 128], rb_[:, cc * 128:(cc + 1) * 128], self.ident_b[:], r=[rb_, self.ident_b], w=[psb[0]])
        V("tensor_copy", stg2[:].rearrange("p a b -> p (a b)"), psb[0][:, 0:512], r=[psb[0]], w=[stg2])
        self.dma("sp", self.qkraw.rearrange("c p t -> p c t")[:, 0:4, t0:t0 + 128], stg2[:], r=[stg2], w=["qkraw"])


Builder.proj_odd = _proj_odd


def _retention(self, l):
    i = l // 2
    T, NT, NTC = self.T, self.NT, self.NTC
    self.new_phase()
    psf, psb = self.psf, self.psb
    V, A, G, mm = self.V, self.A, self.G, self.mm
    qT = self.sb([64, 4, T], BF16)
    kT = self.sb([64, 4, T], BF16)
    for h in range(4):
        self.dma("sp", qT[:, h, :], self.qkraw[h // 2][(h % 2) * 64:(h % 2 + 1) * 64, :], r=["qkraw"], w=[qT])
        self.dma("sp", kT[:, h, :], self.qkraw[2 + h // 2][(h % 2) * 64:(h % 2 + 1) * 64, :], r=["qkraw"], w=[kT])
    vp = self.sb([128, NT, 512], BF16)
    self.dma("sp", vp[:], self.vB.rearrange("(n p) f -> p n f", p=128), r=["vB"], w=[vp])
    ld = self.sb([128, 8], F32)
    self.dma("sp", ld[:], self.od_decay[i:i + 1, :].partition_broadcast(128), w=[ld])
    A("activation", ld[:], ld[:], AF.Exp, scale=-1.0, r=[ld], w=[ld])
    A("activation", ld[:], ld[:], AF.Ln, bias=1.0, r=[ld], w=[ld])
    V("tensor_scalar", ld[:], ld[:], -1.0, None, ALU.mult, r=[ld], w=[ld])
    lagp = self.sb([128, 128], F32)
    lagn = self.sb([128, 128], F32)
    V("tensor_scalar", lagp[:], self.iot[:], 0.0, None, ALU.max, r=[self.iot], w=[lagp])
    V("tensor_scalar", lagn[:], self.iot[:], -1.0, 0.0, ALU.mult, ALU.max, r=[self.iot], w=[lagn])
    rowf = self.sb([128, 128], F32)
    rowb = self.sb([128, 128], F32)
    G("iota", rowf[:], [[1, 128]], base=1, channel_multiplier=0, allow_small_or_imprecise_dtypes=True, w=[rowf])
    G("iota", rowb[:], [[-1, 128]], base=128, channel_multiplier=0, allow_small_or_imprecise_dtypes=True, w=[rowb])
    colf = self.sb([128, 1], F32)
    V("tensor_scalar", colf[:], self.pidx[:], -1.0, 127.0, ALU.mult, ALU.add, r=[self.pidx], w=[colf])
    Dm = self.sb([128, 2, 4, 128], F32)
    qd = self.sb([64, 2, 4, 128], F32)
    kd = self.sb([128, 2, 4], F32)
    cd = self.sb([128, 2, 4], F32)
    for d in range(2):
        for h in range(4):
            c = ld[:, d * 4 + h:d * 4 + h + 1]
            A("activation", Dm[:, d, h, :], (lagp if d == 0 else lagn)[:], AF.Exp, scale=c, r=[lagp, lagn, ld], w=[Dm])
            V("scalar_tensor_tensor", Dm[:, d, h, :], Dm[:, d, h, :], 0.125, (self.triU_f if d == 0 else self.triL_f)[:], ALU.mult, ALU.mult,
              r=[Dm, self.triU_f, self.triL_f], w=[Dm])
            A("activation", qd[:, d, h, :], (rowf if d == 0 else rowb)[0:64, :], AF.Exp, scale=ld[0:64, d * 4 + h:d * 4 + h + 1],
              r=[rowf, rowb, ld], w=[qd])
            A("activation", kd[:, d, h:h + 1], (colf if d == 0 else self.pidx)[:], AF.Exp, scale=c, r=[colf, self.pidx, ld], w=[kd])
    V("tensor_scalar", kd[:], kd[:], 0.125, None, ALU.mult, r=[kd], w=[kd])
    A("activation", cd[:].rearrange("p a b -> p (a b)"), ld[:], AF.Exp, scale=128.0, r=[ld], w=[cd])
    S32 = self.sb([64, 4, 128], F32)
    Sb = self.sb([64, 4, 128], BF16)
    At = self.sb([128, 4, 128], BF16)
    qeb = self.sb([64, 4, 128], BF16)
    kw = self.sb([128, 4, 64], BF16)
    hout = self.sb([128, 4, 128], F32)
    hbs = Rot([self.sb([128, 512], F32) for _ in range(2)])
    ogs = Rot([self.sb([128, 512], F32) for _ in range(2)])
    st4 = self.sb([128, 4], F32)
    sq = self.sb([128, 4, 128], F32)
    catb = Rot([self.sb([128, 512], BF16) for _ in range(2)])
    KQ, OP, UP, KTp = psf[0], psf[1], psf[2], psb[0]
    for direction in ("bwd", "fwd"):
        fwd = direction == "fwd"
        d = 0 if fwd else 1
        V("memset", S32[:], 0.0, w=[S32])
        V("memset", Sb[:], 0.0, w=[Sb])
        order = list(range(NT)) if fwd else (list(range(NTC - 1, -1, -1)) + list(range(NT - 1, NTC - 1, -1)))
        for c in order:
            tok0, tok1 = c * 128, (c + 1) * 128
            for h in range(4):
                mm(KQ[:, h * 128:(h + 1) * 128], kT[:, h, tok0:tok1], qT[:, h, tok0:tok1], r=[kT, qT], w=[KQ])
            V("tensor_tensor", At[:], KQ[:, :].rearrange("p (h t) -> p h t", t=128), Dm[:, d, :, :], ALU.mult, r=[KQ, Dm], w=[At])
            G("tensor_tensor", qeb[:], qT[:, :, tok0:tok1], qd[:, d, :, :], ALU.mult, r=[qT, qd], w=[qeb])
            for h in range(4):
                self.tr(KTp[:, h * 64:(h + 1) * 64], kT[:, h, tok0:tok1], self.ident_b[0:64, 0:64], r=[kT, self.ident_b], w=[KTp])
            V("tensor_tensor", kw[:], KTp[:, 0:256].rearrange("p (h d) -> p h d", d=64), kd[:, d, :].unsqueeze(2).broadcast_to([128, 4, 64]),
              ALU.mult, r=[KTp, kd], w=[kw])
            for h in range(4):
                o_ = OP[:, h * 128:(h + 1) * 128]
                mm(o_, At[:, h, :], vp[:, c, h * 128:(h + 1) * 128], start=(h == 0), stop=False, r=[At, vp], w=[OP], skip=True)
                mm(o_, qeb[:, h, :], Sb[:, h, :], start=False, stop=True, r=[qeb, Sb], w=[OP], skip=True)
            hflat = hout[:].rearrange("p h d -> p (h d)")
            if not fwd:
                A("copy", hflat, OP[:, :], r=[OP], w=[hout])
                self.dma("sp", self.hB[tok0:tok1, :], hflat, r=[hout], w=["hB"])
            else:
                hb, og = hbs.next(), ogs.next()
                self.dma("sp", hb[:], self.hB[tok0:tok1, :], r=["hB"], w=[hb])
                self.dma("sp", og[:], self.ogB[tok0:tok1, :], r=["ogB"], w=[og])
                V("tensor_tensor", hflat, OP[:, :], hb[:], ALU.add, r=[OP, hb], w=[hout])
                G("tensor_tensor", sq[:], hout[:], hout[:], ALU.mult, r=[hout], w=[sq])
                V("tensor_reduce", st4[:], sq[:], axis=AX.X, op=ALU.add, r=[sq], w=[st4])
                V("tensor_scalar", st4[:], st4[:], 1.0 / 128, EPS, ALU.mult, ALU.add, r=[st4], w=[st4])
                A("activation", st4[:], st4[:], AF.Sqrt, r=[st4], w=[st4])
                V("reciprocal", st4[:], st4[:], r=[st4], w=[st4])
                V("tensor_tensor", hout[:], hout[:], st4[:].unsqueeze(2).broadcast_to([128, 4, 128]), ALU.mult, r=[hout, st4], w=[hout])
                cbt = catb.next()
                V("tensor_tensor", cbt[:], hflat, og[:], ALU.mult, r=[hout, og], w=[cbt])
                self.dma("sp", self.cat[tok0:tok1, 0:512], cbt[:], r=[cbt], w=["cat"])
            for h in range(4):
                mm(UP[0:64, h * 128:(h + 1) * 128], kw[:, h, :], vp[:, c, h * 128:(h + 1) * 128], start=(h == 0), stop=True,
                   r=[kw, vp], w=[UP], skip=True)
            for h in range(4):
                V("scalar_tensor_tensor", S32[:, h, :], S32[:, h, :], cd[0:64, d, h:h + 1], UP[0:64, h * 128:(h + 1) * 128], ALU.mult, ALU.add,
                  r=[S32, cd, UP], w=[S32])
            A("copy", Sb[:], S32[:], r=[S32], w=[Sb])


Builder.retention = _retention


def _gqa(self, l):
    T, NT, NTC = self.T, self.NT, self.NTC
    self.new_phase()
    psf = self.psf
    V, A, G, mm = self.V, self.A, self.G, self.mm
    self.tr(psf[0][0:16, 0:128], self.nmax[:, 0:16], self.ident_f[:], r=[self.nmax, self.ident_f], w=[psf[0]])
    mx = self.sb([16, 1], F32)
    V("tensor_reduce", mx[:], psf[0][0:16, 0:128], axis=AX.X, op=ALU.max, r=[psf[0]], w=[mx])
    dg = self.sb([16, 16], F32)
    V("tensor_scalar", dg[:], self.ident_f[0:16, 0:16], mx[:, 0:1], None, ALU.mult, r=[mx, self.ident_f], w=[dg])
    mm(psf[1][:, 0:16], self.ones_f[0:16, 0:128], dg[:], r=[self.ones_f, dg], w=[psf[1]])
    mxb = self.sb([128, 16], F32)
    V("tensor_copy", mxb[:], psf[1][:, 0:16], r=[psf[1]], w=[mxb])
    negm = self.sb([128, 8], F32)
    for kv in range(2):
        V("tensor_scalar", negm[:, kv * 4:(kv + 1) * 4], mxb[:, kv * 4:(kv + 1) * 4], mxb[:, 8 + kv:9 + kv], None, ALU.mult, r=[mxb], w=[negm])
    A("activation", negm[:], negm[:], AF.Sqrt, r=[negm], w=[negm])
    V("tensor_scalar", negm[:], negm[:], -0.125, None, ALU.mult, r=[negm], w=[negm])
    kT = self.sb([128, T], BF16)
    self.dma("sp", kT[:], self.kTA[0], r=["kTA"], w=[kT])
    self.convert_expert_weights(l)
    vh = self.sb([128, NT, 2, 65], BF16)
    V("memset", vh[:, :, :, 64:65], 1.0, w=[vh])
    for n in range(NT):
        self.dma("sp", vh[:, n, :, 0:64], self.vA[n * 128:(n + 1) * 128, 0:128].rearrange("p (k d) -> p k d", d=64), r=["vA"], w=[vh])
    qTs = Rot([self.sb([128, T], BF16) for _ in range(2)])
    pTs = Rot([self.sb([128, 512], BF16) for _ in range(5)])
    st_rot = Rot([psf[4], psf[5], self.psb[0].bitcast(F32), self.psb[1].bitcast(F32)])
    srow = self.sb([65, 512], F32)
    rbs = Rot([self.sb([64, 512], F32) for _ in range(2)])
    outb = Rot([self.sb([64, 512], BF16) for _ in range(2)])
    bank = 0
    for j in range(8):
        kv = j // 4
        qT = qTs.next()
        self.dma("sp", qT[kv * 64:(kv + 1) * 64, :], self.qTA[j // 2][(j % 2) * 64:(j % 2 + 1) * 64, :], r=["qTA"], w=[qT])
        jobs = []
        for (q0, nq, ktiles) in self.qblocks():
            o_ps = psf[bank]
            b_ps = psf[2 + bank]
            bank = 1 - bank

            def post(j=j, q0=q0, nq=nq, o_ps=o_ps, b_ps=b_ps):
                A("copy", srow[64:65, 0:nq], o_ps[64:65, 0:nq], r=[o_ps], w=[srow])
                mm(b_ps[0:64, 0:nq], self.ones_f[64:65, 0:64], srow[64:65, 0:nq], r=[self.ones_f, srow], w=[b_ps])
                rb = rbs.next()
                V("reciprocal", rb[:, 0:nq], b_ps[0:64, 0:nq], r=[b_ps], w=[rb])
                ob = outb.next()
                V("tensor_tensor", ob[:, 0:nq], o_ps[0:64, 0:nq], rb[:, 0:nq], ALU.mult, r=[o_ps, rb], w=[ob])
                self.dma("sp", self.catT[4 + j // 2][(j % 2) * 64:(j % 2 + 1) * 64, q0:q0 + nq], ob[:, 0:nq], r=[ob], w=["catT"])

            jobs.append(dict(kT=kT, qT=qT, krow=(kv * 64, (kv + 1) * 64), v_fn=(lambda kt, kv=kv: vh[:, kt, kv, :]), vdep=vh, dv=65,
                             q0=q0, nq=nq, ktiles=ktiles, negm_col=negm[:, j:j + 1], ndep=negm, o_ps=o_ps, s_ps=None, sum_mode="col", post=post))
        self.run_attn_jobs(jobs, pTs, st_rot)


Builder.gqa = _gqa


def build_program(TC, TL, debug=()):
    B = Builder(TC, TL, debug=debug)
    B.declare_io()
    B.setup_persistent()
    B.phase_mods()
    for l in range(DEPTH):
        i = l // 2
        last = l == DEPTH - 1
        if l % 2 == 0:
            B.proj_even(l)
            B.attnA(l)
            B.mlstm(l)
            B.outproj(l, B.ev_w_out[i])
        else:
            B.proj_odd(l)
            B.retention(l)
            B.gqa(l)
            B.outproj(l, B.od_w_out[i])
        B.moe(l, last)
    B.new_phase()
    B.P.final_wait(list(B.outputs.keys()))
    B.P.emit()
    return B


def kernel(**inputs):
    inp = {k: np.asarray(v) for k, v in inputs.items()}
    BATCH, TL, _ = inp["x"].shape
    TC = inp["ctx"].shape[1]
    n_cores = 8
    B = build_program(TC, TL)
    sh = prep_shared(inp, TL)
    in_maps = []
    for core in range(n_cores):
        b = core % BATCH
        m = dict(sh)
        m.update(prep_core(inp, b))
        in_maps.append({k: v for k, v in m.items() if k in B.inputs})
    res = run_bass_kernel_spmd(B.nc, in_maps, core_ids=list(range(n_cores)))
    out = np.stack([np.asarray(res.results[b]["yout"], dtype=np.float32) for b in range(BATCH)], 0)
    return out
```
